# Optimizing a Trainium2 kernel written in Bass

```python
import math
import jax, jax.numpy as jnp
from jax import lax
import numpy as np

D_MODEL = 1024
BATCH = 4
SEQ = 8192
DEPTH = 2

GRID_W = 64
CTX_LEN = 256
EPS = 1e-6

HG_HEADS = 8
HG_DK = 64
HG_DV = 64
HG_W = HG_HEADS * HG_DV
HG_CHUNK = 64

MLA_HEADS = 8
MLA_Q_RANK = 256
MLA_KV_RANK = 128
MLA_NOPE = 64
MLA_ROPE = 32
MLA_V = 64
MLA_QK = MLA_NOPE + MLA_ROPE
MLA_W = MLA_HEADS * MLA_V
Q_BLOCK = 128
ROPE_BASE = 10000.0

LRU_W = 512
LRU_BLOCKS = 8
LRU_BD = LRU_W // LRU_BLOCKS
CONV_W = 4
LRU_C = 8.0

N_GROUPS = 4
EXP_PER_GROUP = 4
N_EXPERTS = N_GROUPS * EXP_PER_GROUP
D_EXPERT = 256
TOP_K_IN_GROUP = 2

IN_SPLITS = (HG_W, HG_W, HG_W, HG_W, HG_W, MLA_Q_RANK, MLA_KV_RANK, MLA_ROPE, LRU_W, LRU_W, D_MODEL, D_MODEL, D_MODEL)
IN_WIDTH = sum(IN_SPLITS)

kernel_name = 'hybrid_flow_backbone'


def _rmsnorm(t, g):
    tf = t.astype(jnp.float32)
    tf = tf * lax.rsqrt(jnp.mean(tf * tf, axis=-1, keepdims=True) + EPS)
    return (tf * g.astype(jnp.float32)).astype(t.dtype)


def _modulate(t, shift, scale):
    return t * (1.0 + scale) + shift


def _split_in(p):
    offsets = np.cumsum(IN_SPLITS)[:-1].tolist()
    return jnp.split(p, offsets, axis=-1)


def _axial_rope(rows):
    half = MLA_ROPE // 2
    pos_row = jnp.repeat(jnp.arange(rows, dtype=jnp.float32), GRID_W)
    pos_col = jnp.tile(jnp.arange(GRID_W, dtype=jnp.float32), rows)
    inv = ROPE_BASE ** (-jnp.arange(0, half, 2, dtype=jnp.float32) / half)
    ang = jnp.concatenate([pos_row[:, None] * inv, pos_col[:, None] * inv], axis=-1)
    return jnp.cos(ang), jnp.sin(ang)


def _apply_rope(t, cos, sin):
    half = MLA_ROPE // 2
    nope, rp = t[..., :MLA_NOPE], t[..., MLA_NOPE:]
    r1, r2 = rp[..., :half], rp[..., half:]
    cs = cos[None, :, None, :].astype(t.dtype)
    sn = sin[None, :, None, :].astype(t.dtype)
    return jnp.concatenate([nope, r1 * cs - r2 * sn, r2 * cs + r1 * sn], axis=-1)


def _gla_chunked(q, k, v, logf, s0):
    b_, h_, L, _ = q.shape
    n = L // HG_CHUNK

    def chunks(t):
        return t.reshape(b_, h_, n, HG_CHUNK, t.shape[-1]).transpose(2, 0, 1, 3, 4)

    mask = jnp.tril(jnp.ones((HG_CHUNK, HG_CHUNK), dtype=bool))[..., None]

    def step(S, inp):
        qb, kb, vb, fb = inp
        bcum = jnp.cumsum(fb, axis=2)
        diff = bcum[:, :, :, None, :] - bcum[:, :, None, :, :]
        decay = jnp.exp(jnp.where(mask, diff, -jnp.inf))
        A = jnp.einsum('bhtd,bhsd,bhtsd->bhts', qb, kb, decay)
        o = jnp.einsum('bhts,bhsv->bhtv', A, vb) + jnp.einsum('bhtd,bhdv->bhtv', qb * jnp.exp(bcum), S)
        btot = bcum[:, :, -1:, :]
        S_new = S * jnp.exp(btot[:, :, 0, :, None]) + jnp.einsum('bhsd,bhsv->bhdv', kb * jnp.exp(btot - bcum), vb)
        return S_new, o

    S_fin, oc = lax.scan(step, s0, tuple(chunks(t) for t in (q, k, v, logf)))
    o = oc.transpose(1, 2, 0, 3, 4).reshape(b_, h_, L, v.shape[-1])
    return o, S_fin


def _hgrn2_branch(px, pc, lb, gain, need_ctx):
    qx, fxf, fxb, ix, gx = px
    qc, fcf, fcb, ic, gc = pc
    dt = qx.dtype
    bsz = qx.shape[0]

    def heads(t):
        b_, L, _ = t.shape
        return t.astype(jnp.float32).reshape(b_, L, HG_HEADS, -1).transpose(0, 2, 1, 3)

    def decay(z, lbd):
        z = z.astype(jnp.float32)
        logf = jnp.logaddexp(jnp.log(lbd), jnp.log1p(-lbd) + jax.nn.log_sigmoid(z))
        key = (1.0 - lbd) * jax.nn.sigmoid(-z)
        return heads(logf), heads(key)

    qxh, vxh, qch, vch = heads(qx), heads(ix), heads(qc), heads(ic)
    s0 = jnp.zeros((bsz, HG_HEADS, HG_DK, HG_DV), jnp.float32)
    ox_dirs, oc_dirs = [], []
    for d, (zx, zc) in enumerate(((fxf, fcf), (fxb, fcb))):
        lfx, kx = decay(zx, lb[d])
        lfc, kc = decay(zc, lb[d])
        tx = (qxh, kx, vxh, lfx)
        tc = (qch, kc, vch, lfc)
        if d == 1:
            tx = tuple(jnp.flip(t, axis=2) for t in tx)
            tc = tuple(jnp.flip(t, axis=2) for t in tc)
        oc, sc = _gla_chunked(*tc, s0)
        ox, _ = _gla_chunked(*tx, sc)
        if d == 1:
            ox, oc = jnp.flip(ox, axis=2), jnp.flip(oc, axis=2)
        ox_dirs.append(ox)
        oc_dirs.append(oc)

    def readout(dirs, g):
        o = (dirs[0] + dirs[1]).transpose(0, 2, 1, 3)
        b_, L = o.shape[0], o.shape[1]
        o = _rmsnorm(o, gain).reshape(b_, L, HG_W)
        return (o * jax.nn.sigmoid(g.astype(jnp.float32))).astype(dt)

    out_x = readout(ox_dirs, gx)
    out_c = readout(oc_dirs, gc) if need_ctx else None
    return out_x, out_c


def _mla_qkv(dq, dkv, kr, q_norm, kv_norm, w_uq, w_ukv, gq, gk):
    b_, L, _ = dq.shape
    q = (_rmsnorm(dq, q_norm) @ w_uq).reshape(b_, L, MLA_HEADS, MLA_QK)
    kv = (_rmsnorm(dkv, kv_norm) @ w_ukv).reshape(b_, L, MLA_HEADS, MLA_NOPE + MLA_V)
    k_nope, v = kv[..., :MLA_NOPE], kv[..., MLA_NOPE:]
    k_rope = jnp.broadcast_to(kr[:, :, None, :], (b_, L, MLA_HEADS, MLA_ROPE))
    k = jnp.concatenate([k_nope, k_rope], axis=-1)
    return _rmsnorm(q, gq), _rmsnorm(k, gk), v


def _mla_branch(px, pc, cos, sin, q_norm, kv_norm, w_uq, w_ukv, gq, gk, need_ctx):
    dqx, dkvx, krx = px
    dqc, dkvc, krc = pc
    scale = MLA_QK ** -0.5
    qx, kx, vx = _mla_qkv(dqx, dkvx, krx, q_norm, kv_norm, w_uq, w_ukv, gq, gk)
    qx, kx = _apply_rope(qx, cos, sin), _apply_rope(kx, cos, sin)
    qc, kc, vc = _mla_qkv(dqc, dkvc, krc, q_norm, kv_norm, w_uq, w_ukv, gq, gk)
    k_all = jnp.concatenate([kx, kc], axis=1)
    v_all = jnp.concatenate([vx, vc], axis=1)
    b_, S = qx.shape[0], qx.shape[1]
    nb = S // Q_BLOCK
    qb = qx.reshape(b_, nb, Q_BLOCK, MLA_HEADS, MLA_QK).transpose(1, 0, 2, 3, 4)

    def attend(qblk):
        s = jnp.einsum('bqhd,bkhd->bhqk', qblk, k_all).astype(jnp.float32) * scale
        p = jax.nn.softmax(s, axis=-1).astype(v_all.dtype)
        return jnp.einsum('bhqk,bkhd->bqhd', p, v_all)

    ox = lax.map(attend, qb).transpose(1, 0, 2, 3, 4).reshape(b_, S, MLA_W)
    oc = None
    if need_ctx:
        s = jnp.einsum('bqhd,bkhd->bhqk', qc, kc).astype(jnp.float32) * scale
        p = jax.nn.softmax(s, axis=-1).astype(vc.dtype)
        oc = jnp.einsum('bhqk,bkhd->bqhd', p, vc).reshape(b_, qc.shape[1], MLA_W)
    return ox, oc


def _conv_centred(t, w, b):
    L = t.shape[1]
    left = CONV_W // 2
    tp = jnp.pad(t, ((0, 0), (left, CONV_W - 1 - left), (0, 0)))
    out = jnp.broadcast_to(b, t.shape).astype(t.dtype)
    for j in range(CONV_W):
        out = out + tp[:, j:j + L] * w[j]
    return out


def _rglru_gates(u, wa, ba, wx, bx, lam):
    b_, L, _ = u.shape
    ub = u.reshape(b_, L, LRU_BLOCKS, LRU_BD)
    r = jax.nn.sigmoid(jnp.einsum('blnd,nde->blne', ub, wa.astype(jnp.float32)).reshape(b_, L, LRU_W) + ba)
    ig = jax.nn.sigmoid(jnp.einsum('blnd,nde->blne', ub, wx.astype(jnp.float32)).reshape(b_, L, LRU_W) + bx)
    log_a = -LRU_C * r * jax.nn.softplus(-lam.astype(jnp.float32))
    a = jnp.exp(log_a)
    inp = jnp.sqrt(-jnp.expm1(2.0 * log_a)) * (ig * u)
    return a, inp


def _linear_scan(a, b, h0):
    def comb(l, r):
        return l[0] * r[0], r[0] * l[1] + r[1]
    A, H = lax.associative_scan(comb, (a, b), axis=1)
    return H + A * h0[:, None, :]


def _rglru_branch(px, pc, conv_w, conv_b, wa, ba, wx, bx, lam, need_ctx):
    xx, yx = px
    xc, yc = pc
    dt = xx.dtype
    ux = _conv_centred(xx, conv_w, conv_b).astype(jnp.float32)
    uc = _conv_centred(xc, conv_w, conv_b).astype(jnp.float32)
    hx_dirs, hc_dirs = [], []
    for d in range(2):
        ax, bxs = _rglru_gates(ux, wa[d], ba[d], wx[d], bx[d], lam[d])
        ac, bcs = _rglru_gates(uc, wa[d], ba[d], wx[d], bx[d], lam[d])
        if d == 1:
            ax, bxs, ac, bcs = (jnp.flip(t, axis=1) for t in (ax, bxs, ac, bcs))
        hc = _linear_scan(ac, bcs, jnp.zeros_like(uc[:, 0]))
        hx = _linear_scan(ax, bxs, hc[:, -1])
        if d == 1:
            hx, hc = jnp.flip(hx, axis=1), jnp.flip(hc, axis=1)
        hx_dirs.append(hx)
        hc_dirs.append(hc)
    out_x = ((hx_dirs[0] + hx_dirs[1]) * jax.nn.gelu(yx.astype(jnp.float32))).astype(dt)
    out_c = None
    if need_ctx:
        out_c = ((hc_dirs[0] + hc_dirs[1]) * jax.nn.gelu(yc.astype(jnp.float32))).astype(dt)
    return out_x, out_c


def _merge(o_hg, o_mla, o_lru, g_hg, g_mla, g_lru, w_br_hg, w_br_mla, w_br_lru, w_out):
    y = (jax.nn.sigmoid(g_hg) * (o_hg @ w_br_hg)
         + jax.nn.sigmoid(g_mla) * (o_mla @ w_br_mla)
         + jax.nn.sigmoid(g_lru) * (o_lru @ w_br_lru))
    return y @ w_out


def _hier_moe(h, w_rg, b_rg, w_re, b_re, w1, w3, w2):
    shp = h.shape
    t = h.reshape(-1, shp[-1])
    n = t.shape[0]
    lg = (t @ w_rg).astype(jnp.float32)
    g_sel = jnp.argmax(lg + b_rg.astype(jnp.float32), axis=-1)
    g_oh = jax.nn.one_hot(g_sel, N_GROUPS, dtype=jnp.float32)
    p_g = jnp.sum(jax.nn.softmax(lg, axis=-1) * g_oh, axis=-1, keepdims=True)
    le = (t @ w_re).astype(jnp.float32).reshape(n, N_GROUPS, EXP_PER_GROUP)
    le_g = jnp.einsum('ng,nge->ne', g_oh, le)
    bias_g = jnp.einsum('ng,ge->ne', g_oh, b_re.astype(jnp.float32).reshape(N_GROUPS, EXP_PER_GROUP))
    _, idx = lax.top_k(le_g + bias_g, TOP_K_IN_GROUP)
    w_sel = jax.nn.softmax(jnp.take_along_axis(le_g, idx, axis=1), axis=-1) * p_g
    e_sel = g_sel[:, None] * EXP_PER_GROUP + idx
    combine = jnp.einsum('nk,nke->ne', w_sel, jax.nn.one_hot(e_sel, N_EXPERTS, dtype=jnp.float32)).astype(t.dtype)
    out = jnp.zeros_like(t)
    for e in range(N_EXPERTS):
        hid = jax.nn.silu(t @ w1[e]) * (t @ w3[e])
        out = out + combine[:, e:e + 1] * (hid @ w2[e])
    return out.reshape(shp)


def setup_inputs(seed: int = 0) -> dict:
    key = jax.random.key(seed)
    ks = iter(jax.random.split(key, 48))

    def nrm(shape, scale):
        return jax.random.normal(next(ks), shape, jnp.float32) * scale

    def gain(shape):
        return 1.0 + nrm(shape, 0.02)

    L = DEPTH
    a0 = jax.random.uniform(next(ks), (L, 2, LRU_W), jnp.float32, 0.9, 0.999)
    sig = a0 ** (1.0 / LRU_C)
    return {
        'x': nrm((BATCH, SEQ, D_MODEL), 1.0),
        'c': nrm((BATCH, D_MODEL), 1.0),
        'ctx': nrm((BATCH, CTX_LEN, D_MODEL), 1.0),
        'c_ctx': nrm((D_MODEL,), 1.0),
        'w_mod': nrm((L, D_MODEL, 6 * D_MODEL), 0.5 * D_MODEL ** -0.5),
        'b_mod': nrm((L, 6 * D_MODEL), 0.02),
        'norm_mix': gain((L, D_MODEL)),
        'norm_ffn': gain((L, D_MODEL)),
        'w_in': nrm((L, D_MODEL, IN_WIDTH), D_MODEL ** -0.5),
        'hg_lb': nrm((L, 2, HG_W), 1.0),
        'hg_norm': gain((L, HG_DV)),
        'mla_q_norm': gain((L, MLA_Q_RANK)),
        'mla_kv_norm': gain((L, MLA_KV_RANK)),
        'mla_w_uq': nrm((L, MLA_Q_RANK, MLA_HEADS * MLA_QK), MLA_Q_RANK ** -0.5),
        'mla_w_ukv': nrm((L, MLA_KV_RANK, MLA_HEADS * (MLA_NOPE + MLA_V)), MLA_KV_RANK ** -0.5),
        'mla_qk_gain_q': gain((L, MLA_QK)),
        'mla_qk_gain_k': gain((L, MLA_QK)),
        'lru_conv_w': nrm((L, CONV_W, LRU_W), CONV_W ** -0.5),
        'lru_conv_b': nrm((L, LRU_W), 0.02),
        'lru_wa': nrm((L, 2, LRU_BLOCKS, LRU_BD, LRU_BD), LRU_BD ** -0.5),
        'lru_ba': nrm((L, 2, LRU_W), 0.02),
        'lru_wx': nrm((L, 2, LRU_BLOCKS, LRU_BD, LRU_BD), LRU_BD ** -0.5),
        'lru_bx': nrm((L, 2, LRU_W), 0.02),
        'lru_lambda': jnp.log(sig) - jnp.log1p(-sig),
        'w_br_hg': nrm((L, HG_W, D_MODEL), HG_W ** -0.5),
        'w_br_mla': nrm((L, MLA_W, D_MODEL), MLA_W ** -0.5),
        'w_br_lru': nrm((L, LRU_W, D_MODEL), LRU_W ** -0.5),
        'w_out': nrm((L, D_MODEL, D_MODEL), D_MODEL ** -0.5),
        'moe_w_rg': nrm((L, D_MODEL, N_GROUPS), D_MODEL ** -0.5),
        'moe_b_rg': nrm((L, N_GROUPS), 0.01),
        'moe_w_re': nrm((L, D_MODEL, N_EXPERTS), D_MODEL ** -0.5),
        'moe_b_re': nrm((L, N_EXPERTS), 0.01),
        'moe_w1': nrm((L, N_EXPERTS, D_MODEL, D_EXPERT), D_MODEL ** -0.5),
        'moe_w3': nrm((L, N_EXPERTS, D_MODEL, D_EXPERT), D_MODEL ** -0.5),
        'moe_w2': nrm((L, N_EXPERTS, D_EXPERT, D_MODEL), D_EXPERT ** -0.5),
    }


def reference(x, c, ctx, c_ctx, w_mod, b_mod, norm_mix, norm_ffn, w_in, hg_lb, hg_norm,
              mla_q_norm, mla_kv_norm, mla_w_uq, mla_w_ukv, mla_qk_gain_q, mla_qk_gain_k,
              lru_conv_w, lru_conv_b, lru_wa, lru_ba, lru_wx, lru_bx, lru_lambda,
              w_br_hg, w_br_mla, w_br_lru, w_out,
              moe_w_rg, moe_b_rg, moe_w_re, moe_b_re, moe_w1, moe_w3, moe_w2):
    rows = x.shape[1] // GRID_W
    cos, sin = _axial_rope(rows)
    lb_cs = jnp.cumsum(jax.nn.softmax(hg_lb.astype(jnp.float32), axis=0), axis=0)
    lb_all = lb_cs - lb_cs[0:1]
    xc = ctx
    for l in range(DEPTH):
        need_ctx = l < DEPTH - 1
        mx = jax.nn.silu(c) @ w_mod[l] + b_mod[l]
        mc = jax.nn.silu(c_ctx) @ w_mod[l] + b_mod[l]
        sh1x, sc1x, g1x, sh2x, sc2x, g2x = (m[:, None, :] for m in jnp.split(mx, 6, axis=-1))
        sh1c, sc1c, g1c, sh2c, sc2c, g2c = jnp.split(mc, 6, axis=-1)

        hx = _modulate(_rmsnorm(x, norm_mix[l]), sh1x, sc1x)
        hc = _modulate(_rmsnorm(xc, norm_mix[l]), sh1c, sc1c)
        px = _split_in(hx @ w_in[l])
        pc = _split_in(hc @ w_in[l])

        o_hg_x, o_hg_c = _hgrn2_branch(px[0:5], pc[0:5], lb_all[l], hg_norm[l], need_ctx)
        o_mla_x, o_mla_c = _mla_branch(px[5:8], pc[5:8], cos, sin, mla_q_norm[l], mla_kv_norm[l],
                                       mla_w_uq[l], mla_w_ukv[l], mla_qk_gain_q[l], mla_qk_gain_k[l], need_ctx)
        o_lru_x, o_lru_c = _rglru_branch(px[8:10], pc[8:10], lru_conv_w[l], lru_conv_b[l], lru_wa[l], lru_ba[l],
                                         lru_wx[l], lru_bx[l], lru_lambda[l], need_ctx)

        yx = _merge(o_hg_x, o_mla_x, o_lru_x, px[10], px[11], px[12], w_br_hg[l], w_br_mla[l], w_br_lru[l], w_out[l])
        x = x + g1x * yx
        if need_ctx:
            yc = _merge(o_hg_c, o_mla_c, o_lru_c, pc[10], pc[11], pc[12], w_br_hg[l], w_br_mla[l], w_br_lru[l], w_out[l])
            xc = xc + g1c * yc

        hx = _modulate(_rmsnorm(x, norm_ffn[l]), sh2x, sc2x)
        x = x + g2x * _hier_moe(hx, moe_w_rg[l], moe_b_rg[l], moe_w_re[l], moe_b_re[l], moe_w1[l], moe_w3[l], moe_w2[l])
        if need_ctx:
            hc = _modulate(_rmsnorm(xc, norm_ffn[l]), sh2c, sc2c)
            xc = xc + g2c * _hier_moe(hc, moe_w_rg[l], moe_b_rg[l], moe_w_re[l], moe_b_re[l], moe_w1[l], moe_w3[l], moe_w2[l])
    return x
```

```python
import numpy as np
from contextlib import ExitStack
import concourse.bass as bass
import concourse.mybir as mybir

F32 = mybir.dt.float32
BF16 = mybir.dt.bfloat16
AF = mybir.ActivationFunctionType
ALU = mybir.AluOpType
AX = mybir.AxisListType

NDSEM = 90
NO_SELF_WAIT = ("pe",)


class Buf:
    def __init__(self, t, name):
        self.t = t
        self.name = name
        self.lastw = None
        self.readers = {}
        self.dsem = None
        self.root = self

    def alias(self, ap, name=None):
        b = Buf(ap, name or self.name)
        b.root = self.root
        return b

    def __getitem__(self, k):
        return self.t[k]


class _Rec:
    def __init__(self):
        self.calls = []

    def __getattr__(self, name):
        def f(*a, **kw):
            self.calls.append((name, a, kw))
            return None
        return f


class Eng:
    def __init__(self, name, sem_id):
        self.name = name
        self.sem_id = sem_id
        self.count = 0
        self.ops = []
        self.waited = {}


class Prog:
    def __init__(self, nc):
        self.nc = nc
        self.es = ExitStack()
        self.sems = []
        self.semvals = []
        self.engs = {}
        for n in ("pe", "act", "dve", "pool", "sp"):
            sid = self._newsem("e_" + n)
            self.engs[n] = Eng(n, sid)
        self.dsems = [self._newsem("d%d" % i) for i in range(NDSEM)]
        self.dsem_next = 0
        self.stage_es = None
        self.stage_bufs = []
        self.nstage = 0

    def _newsem(self, name):
        h = self.es.enter_context(self.nc.semaphore(name))
        self.sems.append(h)
        self.semvals.append(0)
        return len(self.sems) - 1

    def begin_stage(self, name):
        self.stage_es = ExitStack()
        self.stage_bufs = []
        self.dsem_next = 0
        self.stage_name = name

    def _uid(self):
        self.uid = getattr(self, "uid", 0) + 1
        return self.uid

    def sb(self, name, shape, dtype):
        t = self.stage_es.enter_context(self.nc.sbuf_tensor("%s_%d_%d" % (name, self.nstage, self._uid()), list(shape), dtype))
        b = Buf(t, name)
        self.stage_bufs.append(b)
        return b

    def gsb(self, name, shape, dtype):
        t = self.es.enter_context(self.nc.sbuf_tensor("g_" + name, list(shape), dtype))
        return Buf(t, name)

    def ps(self, name, shape, dtype=F32):
        full = 512 if dtype == F32 else 1024
        t = self.stage_es.enter_context(self.nc.psum_tensor("%s_%d_%d" % (name, self.nstage, self._uid()), [128, full], dtype))
        shape = list(shape)
        n = 1
        for s_ in shape[1:]:
            n *= s_
        assert n <= full
        v = t[0:shape[0], 0:n]
        if len(shape) == 3:
            v = v.rearrange("p (a b) -> p a b", b=shape[2])
        elif len(shape) == 4:
            v = v.rearrange("p (a b c) -> p a b c", b=shape[2], c=shape[3])
        b = Buf(v, name)
        b.is_psum = True
        self.stage_bufs.append(b)
        return b

    def ps_bank(self, name):
        t = self.stage_es.enter_context(self.nc.psum_tensor("%s_%d_%d" % (name, self.nstage, self._uid()), [128, 512], F32))
        b = Buf(t[:, :], name)
        b.is_psum = True
        self.stage_bufs.append(b)
        return b

    def _dsem_for(self, b, qname="sp"):
        if b.dsem is None:
            b.dsem = {}
        if qname not in b.dsem:
            assert self.dsem_next < NDSEM, "out of dma sems"
            b.dsem[qname] = self.dsems[self.dsem_next]
            self.dsem_next += 1
        return b.dsem[qname]

    def _collect(self, eng, reads, writes):
        reads = [b.root for b in reads]
        writes = [b.root for b in writes]
        need = {}

        def add(sid, val):
            if val > need.get(sid, 0):
                need[sid] = val
        for b in reads:
            if b.lastw is not None:
                add(*b.lastw)
        for b in writes:
            if b.lastw is not None:
                add(*b.lastw)
            for sid, val in b.readers.items():
                add(sid, val)
        waits = []
        for sid, val in need.items():
            if sid == eng.sem_id and eng.name in NO_SELF_WAIT:
                continue
            if eng.waited.get(sid, 0) >= val:
                continue
            eng.waited[sid] = val
            waits.append((sid, val))
        return waits

    def _commit(self, token, reads, writes):
        reads = [b.root for b in reads]
        writes = [b.root for b in writes]
        for b in writes:
            b.lastw = token
            b.readers = {}
        for b in reads:
            if b in writes:
                continue
            sid, val = token
            if val > b.readers.get(sid, 0):
                b.readers[sid] = val

    def op(self, engname, fn, reads=(), writes=(), rg=None):
        reads = [b.root for b in reads]
        writes = [b.root for b in writes]
        writes = list(writes) + [b for b in reads if getattr(b, "is_psum", False) and b not in writes]
        eng = self.engs[engname]
        waits = self._collect(eng, reads, writes)
        if engname == "pe":
            rgs = frozenset(rg) if rg is not None else frozenset((0, 1, 2, 3))
            for b in writes:
                if getattr(b, "is_psum", False):
                    lp = getattr(b, "last_pe", None)
                    if lp is not None and not (lp[0] & rgs) and eng.waited.get(eng.sem_id, 0) < lp[1]:
                        eng.waited[eng.sem_id] = lp[1]
                        waits.append((eng.sem_id, lp[1]))
                    b.last_pe = (rgs, eng.count + 1)
        eng.count += 1
        self.semvals[eng.sem_id] = eng.count
        rec = _Rec()
        fn(rec)
        assert len(rec.calls) == 1
        eng.ops.append((waits, rec.calls[0], (eng.sem_id, 1)))
        self._commit((eng.sem_id, eng.count), reads, writes)

    def dma(self, qname, out, in_, reads=(), writes=(), owner=None, **kw):
        eng = self.engs[qname]
        waits = self._collect(eng, reads, writes)
        if owner is None:
            owner = writes[0] if writes else reads[0]
        sid = self._dsem_for(owner.root, qname)
        self.semvals[sid] += 16
        val = self.semvals[sid]

        eng.ops.append((waits, ("dma_start", (), dict(out=out, in_=in_, **kw)), (sid, 16)))
        self._commit((sid, val), reads, writes)

    def end_stage(self):
        tokens = []
        for e in self.engs.values():
            if e.count > 0:
                tokens.append((e.sem_id, e.count))
        for sid in self.dsems:
            if self.semvals[sid] > 0:
                tokens.append((sid, self.semvals[sid]))
        for e in self.engs.values():
            waits = []
            for sid, val in tokens:
                if sid == e.sem_id:
                    continue
                if e.waited.get(sid, 0) >= val:
                    continue
                e.waited[sid] = val
                waits.append((sid, val))
            if waits:
                e.ops.append((waits, None, None))
        nc = self.nc
        sems = self.sems
        with nc.Block() as block:
            def replay(e_handle, eng):
                for waits, fn, inc in eng.ops:
                    for sid, val in waits:
                        e_handle.wait_ge(sems[sid], val)
                    if fn is not None:
                        ins = getattr(e_handle, fn[0])(*fn[1], **fn[2])
                        ins.then_inc(sems[inc[0]], inc[1])
                eng.ops = []

            @block.tensor
            def _(t):
                replay(t, self.engs["pe"])

            @block.scalar
            def _(s):
                replay(s, self.engs["act"])

            @block.vector
            def _(v):
                replay(v, self.engs["dve"])

            @block.gpsimd
            def _(g):
                replay(g, self.engs["pool"])

            @block.sync
            def _(s):
                replay(s, self.engs["sp"])
        self.stage_es.close()
        self.stage_es = None
        self.nstage += 1

    def finish(self):
        self.es.close()


import numpy as np

NT = 8448
CT = 256
LT = 8192
EPS = 1e-6
BLOCKS = [(0, 256)] + [(256 + 512 * i, 512) for i in range(16)]
C_Q, C_FF, C_FB, C_I, C_G, C_DQ, C_DKV, C_KR, C_XX, C_YX, C_GHG, C_GMLA, C_GLRU = (
    0, 512, 1024, 1536, 2048, 2560, 2816, 2944, 2976, 3488, 4000, 5024, 6048)

INPUT_SHAPES = {
    'x': [8192, 1024], 'c': [1024], 'ctx': [256, 1024], 'c_ctx': [1024],
    'w_mod': [2, 1024, 6144], 'b_mod': [2, 6144], 'norm_mix': [2, 1024], 'norm_ffn': [2, 1024],
    'w_in': [2, 1024, 7072], 'hg_lb': [2, 2, 512], 'hg_norm': [2, 64],
    'mla_q_norm': [2, 256], 'mla_kv_norm': [2, 128], 'mla_w_uq': [2, 256, 768], 'mla_w_ukv': [2, 128, 1024],
    'mla_qk_gain_q': [2, 96], 'mla_qk_gain_k': [2, 96],
    'lru_conv_w': [2, 4, 512], 'lru_conv_b': [2, 512], 'lru_wa': [2, 2, 8, 64, 64], 'lru_ba': [2, 2, 512],
    'lru_wx': [2, 2, 8, 64, 64], 'lru_bx': [2, 2, 512], 'lru_lambda': [2, 2, 512],
    'w_br_hg': [2, 512, 1024], 'w_br_mla': [2, 512, 1024], 'w_br_lru': [2, 512, 1024], 'w_out': [2, 1024, 1024],
    'moe_w_rg': [2, 1024, 4], 'moe_b_rg': [2, 4], 'moe_w_re': [2, 1024, 16], 'moe_b_re': [2, 16],
    'moe_w1': [2, 16, 1024, 256], 'moe_w3': [2, 16, 1024, 256], 'moe_w2': [2, 16, 256, 1024],
    'k_ident': [128, 128], 'k_cos': [8192, 16], 'k_sin': [8192, 16], 'k_tri': [32, 2, 32],
    'k_sel': [16, 16, 128], 'k_selrow': [2, 2, 128],
}

SCRATCH = {
    'XCUR': ([NT, 1024], F32),
    'FQ': ([512, NT], BF16), 'FZ': ([1024, NT], F32), 'FXX': ([512, NT], F32), 'FYX': ([512, NT], BF16),
    'FG': ([3072, NT], BF16), 'TV': ([NT, 1024], BF16), 'TM': ([NT, 416], F32),
    'ODIR': ([2, NT, 512], F32), 'OHG': ([512, NT], BF16), 'OLRU': ([512, NT], BF16), 'OMLA': ([512, NT], BF16),
    'HF': ([512, NT], F32), 'W13B': ([16, 128, 8, 512], BF16), 'QT': ([8, 96, NT], BF16), 'KT': ([8, 96, NT], BF16), 'VX': ([NT, 8, 65], BF16),
}


class Dram:
    pass


def host_constants():
    k = {}
    k['k_ident'] = np.eye(128, dtype=np.float32)
    half = 16
    rows = 8192 // 64
    pos_row = np.repeat(np.arange(rows, dtype=np.float32), 64)
    pos_col = np.tile(np.arange(64, dtype=np.float32), rows)
    inv = (np.float32(10000.0) ** (-np.arange(0, half, 2, dtype=np.float32) / np.float32(half))).astype(np.float32)
    ang = np.concatenate([pos_row[:, None] * inv, pos_col[:, None] * inv], axis=-1).astype(np.float32)
    k['k_cos'] = np.cos(ang).astype(np.float32)
    k['k_sin'] = np.sin(ang).astype(np.float32)
    s = np.arange(32)[:, None]
    t = np.arange(32)[None, :]
    tri = np.zeros((32, 2, 32), np.float32)
    tri[:, 0, :] = (t >= s)
    tri[:, 1, :] = (t <= s)
    k['k_tri'] = tri
    sel = np.zeros((16, 16, 128), np.float32)
    for e in range(16):
        sel[e, e, :] = 1.0
    k['k_sel'] = sel
    sr = np.zeros((2, 2, 128), np.float32)
    sr[0, 0, :] = 1.0
    sr[1, 1, :] = 1.0
    k['k_selrow'] = sr
    return k


def declare(nc, debug=()):
    d = Dram()
    for name, shape in INPUT_SHAPES.items():
        setattr(d, name, nc.dram_tensor(name, list(shape), F32, kind="ExternalInput").ap())
    d.out = nc.dram_tensor("out", [8192, 1024], F32, kind="ExternalOutput").ap()
    for name, (shape, dt) in SCRATCH.items():
        kind = "ExternalOutput" if name in debug else "Internal"
        setattr(d, name, nc.dram_tensor(name, list(shape), dt, kind=kind).ap())
    return d


class Ring:
    def __init__(self, bufs):
        self.bufs = bufs
        self.i = 0

    def next(self):
        b = self.bufs[self.i % len(self.bufs)]
        self.i += 1
        return b


class G:
    pass


def alloc_globals(P):
    g = G()
    g.identf = P.gsb("identf", [128, 128], F32)
    g.identb = P.gsb("identb", [128, 128], BF16)
    g.gm = P.gsb("gm", [128, 2, 8, 2], F32)
    g.shf = P.gsb("shf", [128, 2, 8, 2], F32)
    g.gbc = P.gsb("gbc", [128, 2, 2, 1024], F32)
    return g


def bcast_rows(P, dst_fn, row_aps, widths, ps=None):
    tot = sum(widths)
    row = P.sb("bc_row", [1, tot], F32)
    ones = P.sb("bc_ones", [1, 128], F32)
    if ps is None:
        ps = P.ps("bc_ps", [128, tot])
    P.op("dve", lambda e: e.memset(ones[:], 1.0), writes=[ones])
    o = 0
    for ap, w in zip(row_aps, widths):
        P.dma("sp", row[0:1, o:o + w], ap, writes=[row])
        o += w
    P.op("pe", lambda e: e.matmul(ps[:, 0:tot], lhsT=ones[0:1, :], rhs=row[0:1, :], start=True, stop=True), reads=[ones, row], writes=[ps])
    dst_fn(ps)


def stage_init(P, d, g):
    P.begin_stage("init")
    misc = P.sb("misc", [1, 2], F32)
    P.dma("sp", g.identf[:], d.k_ident[:, :], writes=[g.identf])
    P.op("dve", lambda e: e.tensor_copy(out=g.identb[:], in_=g.identf[:]), reads=[g.identf], writes=[g.identb])
    P.dma("sp", d.XCUR[0:256, :], d.ctx[:, :], owner=misc)
    for i in range(4):
        P.dma("pool" if i % 2 else "sp", d.XCUR[256 + i * 2048:256 + (i + 1) * 2048, :], d.x[i * 2048:(i + 1) * 2048, :], owner=misc)
    P.end_stage()


def stage_P(P, l, d, g):
    P.begin_stage("P%d" % l)
    sc = P.sb("sc", [128, 8, 2], F32)
    P.dma("sp", sc[:, :, 0], d.c.rearrange("(k p) -> p k", p=128), writes=[sc], allow_slow_non_contiguous=True)
    P.dma("sp", sc[:, :, 1], d.c_ctx.rearrange("(k p) -> p k", p=128), writes=[sc], allow_slow_non_contiguous=True)
    P.op("act", lambda e: e.activation(out=sc[:], in_=sc[:], func=AF.Silu), reads=[sc], writes=[sc])
    bm = P.sb("bm", [2, 6144], F32)
    P.dma("sp", bm[0:1, :], d.b_mod[l:l + 1, :], writes=[bm])
    P.dma("sp", bm[1:2, :], d.b_mod[l:l + 1, :], writes=[bm])
    selrow = P.sb("selrow", [2, 2, 128], F32)
    P.dma("sp", selrow[:], d.k_selrow[:, :, :], writes=[selrow])
    nm = P.sb("nm", [128, 2, 8], F32)
    P.dma("sp", nm[:, 0, :], d.norm_mix[l].rearrange("(k p) -> p k", p=128), writes=[nm], allow_slow_non_contiguous=True)
    P.dma("sp", nm[:, 1, :], d.norm_ffn[l].rearrange("(k p) -> p k", p=128), writes=[nm], allow_slow_non_contiguous=True)
    modrow = P.sb("modrow", [2, 6144], F32)
    wmr = Ring([P.sb("wm%d" % i, [128, 3072], F32) for i in range(2)])
    psr = [P.ps("psr%d" % i, [2, 512]) for i in range(6)]
    for half in range(2):
        for k in range(8):
            wm = wmr.next()
            P.dma("sp" if k % 2 else "pool", wm[:], d.w_mod[l, k * 128:(k + 1) * 128, half * 3072:(half + 1) * 3072], writes=[wm])
            for j in range(6):
                P.op("pe", lambda e, j=j, k=k, wm=wm: e.matmul(psr[j][:], lhsT=sc[:, k, :], rhs=wm[:, j * 512:(j + 1) * 512],
                                                             start=(k == 0), stop=(k == 7)), reads=[sc, wm], writes=[psr[j]])
        for j in range(6):
            c0 = half * 3072 + j * 512
            P.op("dve", lambda e, j=j, c0=c0: e.tensor_tensor(out=modrow[:, c0:c0 + 512], in0=psr[j][:], in1=bm[:, c0:c0 + 512], op=ALU.add),
                 reads=[psr[j], bm], writes=[modrow])
    pT = P.ps("pT", [128, 32, 2])
    cols = [0, 1024, 3072, 4096]
    for gi, cbase in enumerate(cols):
        for k in range(8):
            P.op("pe", lambda e, gi=gi, k=k, cbase=cbase: e.transpose(pT[:, gi * 8 + k, :], modrow[0:2, cbase + k * 128:cbase + (k + 1) * 128], g.identf[0:2, 0:2]),
                 reads=[modrow, g.identf], writes=[pT])
    modfm = P.sb("modfm", [128, 32, 2], F32)
    P.op("dve", lambda e: e.tensor_copy(out=modfm[:], in_=pT[:]), reads=[pT], writes=[modfm])
    for ni in range(2):
        shg, scg = (0, 1) if ni == 0 else (2, 3)
        P.op("dve", lambda e, ni=ni, shg=shg: e.tensor_copy(out=g.shf[:, ni, :, :], in_=modfm[:, shg * 8:(shg + 1) * 8, :]), reads=[modfm], writes=[g.shf])
        P.op("dve", lambda e, ni=ni, scg=scg: e.tensor_scalar(out=g.gm[:, ni, :, :], in0=modfm[:, scg * 8:(scg + 1) * 8, :], scalar1=1.0, scalar2=None, op0=ALU.add),
             reads=[modfm], writes=[g.gm])
        P.op("dve", lambda e, ni=ni: e.tensor_tensor(out=g.gm[:, ni, :, :], in0=g.gm[:, ni, :, :], in1=nm[:, ni, :].unsqueeze(2).to_broadcast([128, 8, 2]), op=ALU.mult),
             reads=[g.gm, nm], writes=[g.gm])
    psb = Ring([P.ps("psb%d" % i, [128, 512]) for i in range(1)])
    for gi, cbase in enumerate([2048, 5120]):
        for cond in range(2):
            for h in range(2):
                pb = psb.next()
                P.op("pe", lambda e, pb=pb, cond=cond, cbase=cbase, h=h: e.matmul(pb[:], lhsT=selrow[0:2, cond, :], rhs=modrow[0:2, cbase + h * 512:cbase + (h + 1) * 512], start=True, stop=True),
                     reads=[selrow, modrow], writes=[pb])
                P.op("act", lambda e, pb=pb, gi=gi, cond=cond, h=h: e.copy(out=g.gbc[:, gi, cond, h * 512:(h + 1) * 512], in_=pb[:]), reads=[pb], writes=[g.gbc])
    P.end_stage()


def norm_transpose(P, g, xt, ss, rs, xs, pst, hT, j, ni, cond):
    P.op("pool", lambda e: e.memset(ss[:], 0.0), writes=[ss])
    P.op("act", lambda e: e.activation(out=xs[:], in_=xt[:], func=AF.Square, accum_out=ss[:]), reads=[xt, ss], writes=[xs, ss])
    P.op("dve", lambda e: e.tensor_scalar(out=rs[:], in0=ss[:], scalar1=1.0 / 1024.0, scalar2=EPS, op0=ALU.mult, op1=ALU.add), reads=[ss], writes=[rs])
    P.op("act", lambda e: e.activation(out=rs[:], in_=rs[:], func=AF.Ln), reads=[rs], writes=[rs])
    P.op("act", lambda e: e.activation(out=rs[:], in_=rs[:], func=AF.Exp, scale=-0.5), reads=[rs], writes=[rs])
    P.op("dve", lambda e: e.tensor_scalar(out=xs[:], in0=xt[:], scalar1=rs[:, 0:1], scalar2=None, op0=ALU.mult), reads=[xt, rs], writes=[xs])
    for k in range(8):
        P.op("pe", lambda e, k=k: e.transpose(pst[:, k, :], xs[:, k * 128:(k + 1) * 128], g.identb[:]), reads=[xs, g.identb], writes=[pst])
    for k in range(8):
        P.op("act", lambda e, k=k: e.activation(out=hT[:, k, j * 128:(j + 1) * 128], in_=pst[:, k, :], func=AF.Identity,
                                                scale=g.gm[:, ni, k, cond:cond + 1], bias=g.shf[:, ni, k, cond:cond + 1]),
             reads=[pst, g.gm, g.shf], writes=[hT])


def stage_A(P, l, d, g):
    P.begin_stage("A%d" % l)
    W = P.sb("W", [128, 8, 7072], BF16)
    for k in range(8):
        P.dma("pool", W[:, k, :], d.w_in[l, k * 128:(k + 1) * 128, :], writes=[W])
    xr = Ring([P.sb("xt%d" % i, [128, 1024], F32) for i in range(2)])
    xsr = Ring([P.sb("xs%d" % i, [128, 1024], BF16) for i in range(2)])
    ssr = Ring([P.sb("ss%d" % i, [128, 1], F32) for i in range(2)])
    rsr = Ring([P.sb("rs%d" % i, [128, 1], F32) for i in range(2)])
    hr = Ring([P.sb("hT%d" % i, [128, 8, 512], BF16) for i in range(2)])
    pstr = Ring([P.ps("pst%d" % i, [128, 8, 128], BF16) for i in range(2)])
    psf = Ring([P.ps("psf%d" % i, [128, 512]) for i in range(3)])
    pstm = Ring([P.ps("pstm%d" % i, [128, 512]) for i in range(2)])
    st32 = Ring([P.sb("st32_%d" % i, [128, 512], F32) for i in range(3)])
    st16 = Ring([P.sb("st16_%d" % i, [128, 512], BF16) for i in range(3)])
    sttv = Ring([P.sb("sttv%d" % i, [128, 1024], BF16) for i in range(2)])
    sttm = Ring([P.sb("sttm%d" % i, [128, 416], F32) for i in range(2)])
    fm_groups = [(d.FQ, 0, C_Q, 512, False), (d.FZ, 0, C_FF, 1024, True), (d.FXX, 0, C_XX, 512, True),
                 (d.FYX, 0, C_YX, 512, False), (d.FG, 0, C_GHG, 3072, False)]
    ev = 0

    def norm_steps(bi):
        t0, n = BLOCKS[bi]
        cond = 1 if t0 < CT else 0
        hT = hr.next()

        def mk(j):
            def f():
                xt = xr.next()
                P.dma("sp", xt[:], d.XCUR[t0 + j * 128:t0 + (j + 1) * 128, :], writes=[xt])
                norm_transpose(P, g, xt, ssr.next(), rsr.next(), xsr.next(), pstr.next(), hT, j, 0, cond)
            return f
        return hT, [mk(j) for j in range(n // 128)]

    hT, st0 = norm_steps(0)
    for f_ in st0:
        f_()
    for bi, (t0, n) in enumerate(BLOCKS):
        cond = 1 if t0 < CT else 0
        if bi + 1 < len(BLOCKS):
            hT_next, nsteps = norm_steps(bi + 1)
        else:
            hT_next, nsteps = None, []
        cnt = 0
        for (dst, r0, c0, nc_, is32) in fm_groups:
            for cc in range(nc_ // 128):
                cnt += 1
                if cnt % 8 == 0 and nsteps:
                    nsteps.pop(0)()
                ps = psf.next()
                for k in range(8):
                    P.op("pe", lambda e, ps=ps, k=k, c0=c0, cc=cc, hT=hT, n=n: e.matmul(ps[:, 0:n], lhsT=W[:, k, c0 + cc * 128:c0 + (cc + 1) * 128], rhs=hT[:, k, 0:n],
                                                                                      start=(k == 0), stop=(k == 7)), reads=[W, hT], writes=[ps])
                stg = st32.next() if is32 else st16.next()
                if ev % 2 == 0:
                    P.op("act", lambda e, ps=ps, stg=stg, n=n: e.copy(out=stg[:, 0:n], in_=ps[:, 0:n]), reads=[ps], writes=[stg])
                else:
                    P.op("dve", lambda e, ps=ps, stg=stg, n=n: e.tensor_copy(out=stg[:, 0:n], in_=ps[:, 0:n]), reads=[ps], writes=[stg])
                ev += 1
                P.dma("pool" if ev % 2 else "sp", dst[r0 + cc * 128:r0 + (cc + 1) * 128, t0:t0 + n], stg[:, 0:n], reads=[stg])
        for j in range(n // 128):
            tv = sttv.next()
            tm = sttm.next()
            for (c0, nc_, dstb, o0) in [(C_I, 512, tv, 0), (C_G, 512, tv, 512), (C_DQ, 416, tm, 0)]:
                ps = pstm.next()
                for k in range(8):
                    P.op("pe", lambda e, ps=ps, k=k, c0=c0, nc_=nc_, hT=hT, j=j: e.matmul(ps[:, 0:nc_], lhsT=hT[:, k, j * 128:(j + 1) * 128], rhs=W[:, k, c0:c0 + nc_],
                                                                                       start=(k == 0), stop=(k == 7)), reads=[W, hT], writes=[ps])
                if ev % 2 == 0:
                    P.op("act", lambda e, ps=ps, dstb=dstb, o0=o0, nc_=nc_: e.copy(out=dstb[:, o0:o0 + nc_], in_=ps[:, 0:nc_]), reads=[ps], writes=[dstb])
                else:
                    P.op("dve", lambda e, ps=ps, dstb=dstb, o0=o0, nc_=nc_: e.tensor_copy(out=dstb[:, o0:o0 + nc_], in_=ps[:, 0:nc_]), reads=[ps], writes=[dstb])
                ev += 1
            tt = t0 + j * 128
            P.dma("sp", d.TV[tt:tt + 128, :], tv[:], reads=[tv])
            P.dma("pool", d.TM[tt:tt + 128, :], tm[:], reads=[tm])
        for f_ in nsteps:
            f_()
        hT = hT_next
    P.end_stage()


def body_C(P, l, d, g, merged=False, dbgsel=None):
    NB = 256 if merged else 1024
    convw = P.sb("convw", [128, 4, 4], F32)
    convb = P.sb("convb", [128, 4], F32)
    bab = P.sb("bab", [128, 4, 2, 2], F32)
    clru = P.sb("clru", [128, 4, 2], F32)
    clru2 = P.sb("clru2", [128, 4, 2], F32)
    for j in range(4):
        P.dma("sp", convw[:, :, j], d.lru_conv_w[l, j].rearrange("(t p) -> p t", p=128), writes=[convw], allow_slow_non_contiguous=True)
    P.dma("sp", convb[:], d.lru_conv_b[l].rearrange("(t p) -> p t", p=128), writes=[convb], allow_slow_non_contiguous=True)
    for r_ in range(2):
        P.dma("sp", bab[:, :, 0, r_], d.lru_ba[l, r_].rearrange("(t p) -> p t", p=128), writes=[bab], allow_slow_non_contiguous=True)
        P.dma("sp", bab[:, :, 1, r_], d.lru_bx[l, r_].rearrange("(t p) -> p t", p=128), writes=[bab], allow_slow_non_contiguous=True)
        P.dma("sp", clru[:, :, r_], d.lru_lambda[l, r_].rearrange("(t p) -> p t", p=128), writes=[clru], allow_slow_non_contiguous=True)
    P.op("act", lambda e: e.activation(out=clru[:], in_=clru[:], func=AF.Exp, scale=-1.0), reads=[clru], writes=[clru])
    P.op("act", lambda e: e.activation(out=clru[:], in_=clru[:], func=AF.Ln, bias=1.0), reads=[clru], writes=[clru])
    P.op("dve", lambda e: e.tensor_scalar(out=clru2[:], in0=clru[:], scalar1=-16.0, scalar2=None, op0=ALU.mult), reads=[clru], writes=[clru2])
    P.op("dve", lambda e: e.tensor_scalar(out=clru[:], in0=clru[:], scalar1=-8.0, scalar2=None, op0=ALU.mult), reads=[clru], writes=[clru])
    wbd = P.sb("wbd", [128, 16, 128], F32)
    P.op("pool", lambda e: e.memset(wbd[:], 0.0), writes=[wbd])
    for dr in range(2):
        for gi, wsrc in enumerate([d.lru_wa, d.lru_wx]):
            for ti in range(4):
                for hf in range(2):
                    idx = (dr * 2 + gi) * 4 + ti
                    P.dma("sp", wbd[hf * 64:(hf + 1) * 64, idx, hf * 64:(hf + 1) * 64], wsrc[l, dr, ti * 2 + hf, :, :], writes=[wbd])
    xpr = Ring([P.sb("xp%d" % i, [128, NB + 3], F32) for i in range(2)])
    ur = Ring([P.sb("u%d" % i, [128, NB], F32) for i in range(2)])
    rr = Ring([P.sb("r%d" % i, [128, NB], F32) for i in range(2)])
    igr = Ring([P.sb("ig%d" % i, [128, NB], F32) for i in range(2)])
    ar = Ring([P.sb("a%d" % i, [128, NB], F32) for i in range(2)])
    a2r = Ring([P.sb("a2%d" % i, [128, NB], F32) for i in range(2)])
    hr = Ring([P.sb("h%d" % i, [128, NB], F32) for i in range(2)])
    hfr = Ring([P.sb("hf%d" % i, [128, NB], F32) for i in range(2)])
    yr = Ring([P.sb("y%d" % i, [128, NB], BF16) for i in range(2)])
    gyr = Ring([P.sb("gy%d" % i, [128, NB], F32) for i in range(2)])
    obr = Ring([P.sb("ob%d" % i, [128, NB], BF16) for i in range(2)])
    psg = Ring([P.ps("psg%d" % i, [128, 512]) for i in range(2 if merged else 6)])
    segs = [(0, CT), (CT, NT)]
    nlb = LT // NB
    blocks_f = [(0, 256, 0)] + [(256 + NB * i, NB, 1) for i in range(nlb)]
    blocks_b = [(0, 256, 0)] + [(256 + NB * i, NB, 1) for i in reversed(range(nlb))]
    yield
    items = []
    for ti in range(4):
        for dr in range(2):
            for bi, (t0, n, sg) in enumerate(blocks_f if dr == 0 else blocks_b):
                items.append((ti, dr, bi, t0, n, sg))

    def phase1(it):
        ti, dr, bi, t0, n, sg = it
        rows = slice(ti * 128, (ti + 1) * 128)
        s0, s1 = segs[sg]
        xp = xpr.next()
        lo = max(t0 - 2, s0)
        hi = min(t0 + n + 1, s1)
        P.op("pool", lambda e: e.memset(xp[:], 0.0), writes=[xp])
        P.dma("sp", xp[:, lo - (t0 - 2):hi - (t0 - 2)], d.FXX[rows, lo:hi], writes=[xp])
        u = ur.next()
        P.op("dve", lambda e: e.tensor_scalar(out=u[:, 0:n], in0=xp[:, 0:n], scalar1=convw[:, ti, 0:1], scalar2=convb[:, ti:ti + 1], op0=ALU.mult, op1=ALU.add),
             reads=[xp, convw, convb], writes=[u])
        for j in range(1, 4):
            P.op("dve", lambda e: e.scalar_tensor_tensor(out=u[:, 0:n], in0=xp[:, j:j + n], scalar=convw[:, ti, j:j + 1], in1=u[:, 0:n], op0=ALU.mult, op1=ALU.add),
                 reads=[xp, convw, u], writes=[u])
        r = rr.next()
        ig = igr.next()
        for gi, dst in enumerate([r, ig]):
            idx = (dr * 2 + gi) * 4 + ti
            for c0 in range(0, n, 512):
                cn = min(512, n - c0)
                ps = psg.next()
                P.op("pe", lambda e: e.matmul(ps[:, 0:cn], lhsT=wbd[:, idx, :], rhs=u[:, c0:c0 + cn], start=True, stop=True), reads=[wbd, u], writes=[ps])
                P.op("act", lambda e: e.activation(out=dst[:, c0:c0 + cn], in_=ps[:, 0:cn], func=AF.Sigmoid, bias=bab[:, ti, gi, dr:dr + 1]), reads=[ps, bab], writes=[dst])
        a = ar.next()
        a2 = a2r.next()
        P.op("act", lambda e: e.activation(out=a[:, 0:n], in_=r[:, 0:n], func=AF.Exp, scale=clru[:, ti, dr:dr + 1]), reads=[r, clru], writes=[a])
        P.op("act", lambda e: e.activation(out=a2[:, 0:n], in_=r[:, 0:n], func=AF.Exp, scale=clru2[:, ti, dr:dr + 1]), reads=[r, clru2], writes=[a2])
        P.op("dve", lambda e: e.tensor_scalar(out=a2[:, 0:n], in0=a2[:, 0:n], scalar1=-1.0, scalar2=1.0, op0=ALU.mult, op1=ALU.add), reads=[a2], writes=[a2])
        P.op("act", lambda e: e.activation(out=a2[:, 0:n], in_=a2[:, 0:n], func=AF.Ln), reads=[a2], writes=[a2])
        P.op("act", lambda e: e.activation(out=a2[:, 0:n], in_=a2[:, 0:n], func=AF.Exp, scale=0.5), reads=[a2], writes=[a2])
        P.op("dve", lambda e: e.tensor_tensor(out=ig[:, 0:n], in0=ig[:, 0:n], in1=u[:, 0:n], op=ALU.mult), reads=[ig, u], writes=[ig])
        P.op("dve", lambda e: e.tensor_tensor(out=ig[:, 0:n], in0=ig[:, 0:n], in1=a2[:, 0:n], op=ALU.mult), reads=[ig, a2], writes=[ig])
        ctx_ = dict(a=a, ig=ig)
        if dr == 1:
            hf = hfr.next()
            y = yr.next()
            P.dma("sp", hf[:, 0:n], d.HF[rows, t0:t0 + n], reads=[hf_tok[(ti, t0)]], writes=[hf])
            P.dma("sp", y[:, 0:n], d.FYX[rows, t0:t0 + n], writes=[y])
            gy = gyr.next()
            P.op("act", lambda e: e.activation(out=gy[:, 0:n], in_=y[:, 0:n], func=AF.Gelu_apprx_tanh), reads=[y], writes=[gy])
            ctx_.update(hf=hf, gy=gy)
        return ctx_

    prev = {}
    hf_tok = {}

    def phase2(it, cx):
        ti, dr, bi, t0, n, sg = it
        rows = slice(ti * 128, (ti + 1) * 128)
        a, ig = cx["a"], cx["ig"]
        prev_h = prev.get((ti, dr)) if bi > 0 else None
        h = hr.next()
        rd = [a, ig] + ([prev_h[0]] if prev_h else [])
        if dr == 0:
            init = 0.0 if prev_h is None else prev_h[0][:, prev_h[1] - 1:prev_h[1]]
            P.op("dve", lambda e: e.tensor_tensor_scan(out=h[:, 0:n], data0=a[:, 0:n], data1=ig[:, 0:n], initial=init, op0=ALU.mult, op1=ALU.add), reads=rd, writes=[h])
            hf_tok[(ti, t0)] = Buf(None, "hftok")
            P.dma("pool", d.HF[rows, t0:t0 + n], h[:, 0:n], reads=[h], writes=[hf_tok[(ti, t0)]], owner=h)
        else:
            init = 0.0 if prev_h is None else prev_h[0][:, 0:1]
            P.op("dve", lambda e: e.tensor_tensor_scan(out=h[:, 0:n][:, ::-1], data0=a[:, 0:n][:, ::-1], data1=ig[:, 0:n][:, ::-1], initial=init, op0=ALU.mult, op1=ALU.add),
                 reads=rd, writes=[h])
            hf, gy = cx["hf"], cx["gy"]
            P.op("dve", lambda e: e.tensor_tensor(out=hf[:, 0:n], in0=hf[:, 0:n], in1=h[:, 0:n], op=ALU.add), reads=[hf, h], writes=[hf])
            ob = obr.next()
            P.op("dve", lambda e: e.tensor_tensor(out=ob[:, 0:n], in0=hf[:, 0:n], in1=gy[:, 0:n], op=ALU.mult), reads=[hf, gy], writes=[ob])
            P.dma("pool", d.OLRU[rows, t0:t0 + n], ob[:, 0:n], reads=[ob])
        prev[(ti, dr)] = (h, n)

    cxs = {0: phase1(items[0])}
    for k in range(len(items)):
        if k + 1 < len(items):
            cxs[k + 1] = phase1(items[k + 1])
        phase2(items[k], cxs.pop(k))
        yield


def stage_C(P, l, d, g, dbgsel=None):
    P.begin_stage("C%d" % l)
    for _ in body_C(P, l, d, g, False, dbgsel):
        pass
    P.end_stage()


def stage_F(P, l, d, g, last):
    P.begin_stage("F%d" % l)
    wbr = P.sb("wbr", [128, 3, 4, 1024], BF16)
    wout = P.sb("wout", [128, 8, 1024], BF16)
    for bi, wsrc in enumerate([d.w_br_hg, d.w_br_mla, d.w_br_lru]):
        P.dma("pool", wbr[:, bi, :, :], wsrc[l].rearrange("(k p) n -> p k n", p=128), writes=[wbr])
    P.dma("pool", wout[:], d.w_out[l].rearrange("(k p) n -> p k n", p=128), writes=[wout])
    obr = Ring([P.sb("ob%d" % i, [128, 3, 4, 512], BF16) for i in range(2)])
    gtr = Ring([P.sb("gt%d" % i, [128, 24, 512], BF16) for i in range(2)])
    sgr = Ring([P.sb("sg%d" % i, [128, 512], F32) for i in range(3)])
    accr = Ring([P.sb("acc%d" % i, [128, 512], F32) for i in range(2)])
    tmr = Ring([P.sb("tm%d" % i, [128, 512], F32) for i in range(3)])
    ytr = Ring([P.sb("yT%d" % i, [128, 8, 512], BF16) for i in range(2)])
    xr = Ring([P.sb("xt%d" % i, [128, 1024], F32) for i in range(3)])
    psm = Ring([P.ps("psm%d" % i, [128, 512]) for i in range(4)])
    pso = Ring([P.ps("pso%d" % i, [128, 512]) for i in range(3)])
    srcs = [d.OHG, d.OMLA, d.OLRU]
    for (t0, n) in BLOCKS:
        cond = 1 if t0 < CT else 0
        if last and cond == 1:
            continue
        ob = obr.next()
        gt = gtr.next()
        for bi in range(3):
            P.dma("sp", ob[:, bi, :, 0:n], srcs[bi][:, t0:t0 + n].rearrange("(k p) t -> p k t", p=128), writes=[ob])
        P.dma("sp", gt[:, :, 0:n], d.FG[:, t0:t0 + n].rearrange("(k p) t -> p k t", p=128), writes=[gt])
        yT = ytr.next()
        for nn in range(8):
            acc = accr.next()
            for bi in range(3):
                ps = psm.next()
                for k in range(4):
                    P.op("pe", lambda e, ps=ps, bi=bi, k=k, nn=nn, ob=ob, n=n: e.matmul(ps[:, 0:n], lhsT=wbr[:, bi, k, nn * 128:(nn + 1) * 128], rhs=ob[:, bi, k, 0:n], start=(k == 0), stop=(k == 3)),
                         reads=[wbr, ob], writes=[ps])
                sg = sgr.next()
                P.op("act", lambda e, sg=sg, gt=gt, bi=bi, nn=nn, n=n: e.activation(out=sg[:, 0:n], in_=gt[:, bi * 8 + nn, 0:n], func=AF.Sigmoid), reads=[gt], writes=[sg])
                if bi == 0:
                    P.op("dve", lambda e, acc=acc, ps=ps, sg=sg, n=n: e.tensor_tensor(out=acc[:, 0:n], in0=ps[:, 0:n], in1=sg[:, 0:n], op=ALU.mult), reads=[ps, sg], writes=[acc])
                else:
                    tm = tmr.next()
                    P.op("dve", lambda e, tm=tm, ps=ps, sg=sg, n=n: e.tensor_tensor(out=tm[:, 0:n], in0=ps[:, 0:n], in1=sg[:, 0:n], op=ALU.mult), reads=[ps, sg], writes=[tm])
                    if bi == 1:
                        P.op("dve", lambda e, acc=acc, tm=tm, n=n: e.tensor_tensor(out=acc[:, 0:n], in0=acc[:, 0:n], in1=tm[:, 0:n], op=ALU.add), reads=[acc, tm], writes=[acc])
                    else:
                        P.op("dve", lambda e, acc=acc, tm=tm, n=n, yT=yT, nn=nn: e.tensor_tensor(out=yT[:, nn, 0:n], in0=acc[:, 0:n], in1=tm[:, 0:n], op=ALU.add), reads=[acc, tm], writes=[yT])
        for j in range(n // 128):
            tt = t0 + j * 128
            xt = xr.next()
            P.dma("sp", xt[:], d.XCUR[tt:tt + 128, :], writes=[xt])
            for hf in range(2):
                ps = pso.next()
                for k in range(8):
                    P.op("pe", lambda e, ps=ps, k=k, j=j, hf=hf, yT=yT: e.matmul(ps[:], lhsT=yT[:, k, j * 128:(j + 1) * 128], rhs=wout[:, k, hf * 512:(hf + 1) * 512], start=(k == 0), stop=(k == 7)),
                         reads=[yT, wout], writes=[ps])
                tm = tmr.next()
                P.op("dve", lambda e, tm=tm, ps=ps, hf=hf, cond=cond: e.tensor_tensor(out=tm[:], in0=ps[:], in1=g.gbc[:, 0, cond, hf * 512:(hf + 1) * 512], op=ALU.mult), reads=[ps, g.gbc], writes=[tm])
                P.op("dve", lambda e, xt=xt, tm=tm, hf=hf: e.tensor_tensor(out=xt[:, hf * 512:(hf + 1) * 512], in0=xt[:, hf * 512:(hf + 1) * 512], in1=tm[:], op=ALU.add), reads=[xt, tm], writes=[xt])
            P.dma("pool", d.XCUR[tt:tt + 128, :], xt[:], reads=[xt])
    P.end_stage()


def stage_G0(P, l, d, g):
    P.begin_stage("G0_%d" % l)
    wr = Ring([P.sb("wc%d" % i, [128, 8, 512], BF16) for i in range(3)])
    for e_ in range(16):
        w = wr.next()
        P.dma("pool", w[:, :, 0:256], d.moe_w1[l, e_].rearrange("(k p) n -> p k n", p=128), writes=[w])
        P.dma("pool", w[:, :, 256:512], d.moe_w3[l, e_].rearrange("(k p) n -> p k n", p=128), writes=[w])
        P.dma("sp", d.W13B[e_], w[:], reads=[w])
    P.end_stage()


def stage_G(P, l, d, g, last):
    P.begin_stage("G%d" % l)
    w2all = P.sb("w2all", [128, 32, 1024], BF16)
    for e_ in range(16):
        P.dma("pool", w2all[:, 2 * e_:2 * e_ + 2, :], d.moe_w2[l, e_].rearrange("(h p) n -> p h n", p=128), writes=[w2all])
    wrt = P.sb("wrt", [128, 8, 20], BF16)
    P.dma("pool", wrt[:, :, 0:4], d.moe_w_rg[l].rearrange("(k p) n -> p k n", p=128), writes=[wrt])
    P.dma("pool", wrt[:, :, 4:20], d.moe_w_re[l].rearrange("(k p) n -> p k n", p=128), writes=[wrt])
    brow = P.sb("brow", [128, 20], F32)
    psmisc = P.ps("psmisc", [128, 512])
    bcast_rows(P, lambda ps: P.op("dve", lambda e: e.tensor_copy(out=brow[:], in_=ps[:, 0:20]), reads=[ps], writes=[brow]),
               [d.moe_b_rg[l:l + 1, :], d.moe_b_re[l:l + 1, :]], [4, 16], ps=psmisc)
    sel = P.sb("sel", [16, 16, 128], F32)
    P.dma("sp", sel[:], d.k_sel[:, :, :], writes=[sel])
    w13r = Ring([P.sb("w13_%d" % i, [128, 8, 512], BF16) for i in range(2)])
    xts = [[P.sb("xt%d_%d" % (i, j), [128, 1024], F32) for j in range(4)] for i in range(2)]
    xsr = Ring([P.sb("xs%d" % i, [128, 1024], BF16) for i in range(1)])
    ssr = Ring([P.sb("ss%d" % i, [128, 1], F32) for i in range(2)])
    rsr = Ring([P.sb("rs%d" % i, [128, 1], F32) for i in range(2)])
    hr = Ring([P.sb("hT%d" % i, [128, 8, 512], BF16) for i in range(2)])
    hidr = Ring([P.sb("hid%d" % i, [128, 32, 512], BF16) for i in range(1)])
    sar = Ring([P.sb("sa%d" % i, [128, 512], F32) for i in range(2)])
    tmr = Ring([P.sb("tm%d" % i, [128, 512], F32) for i in range(2)])
    bcsr = Ring([P.sb("bcs%d" % i, [128, 512], F32) for i in range(2)])

    def small(name, shape):
        return P.sb(name, shape, F32)
    lgle = small("lgle", [128, 4, 20]); lgb = small("lgb", [128, 4, 4]); mg = small("mg", [128, 4]); goh = small("goh", [128, 4, 4])
    e0 = small("e0", [128, 4, 4]); s0 = small("s0", [128, 4]); pg = small("pg", [128, 4]); t4 = small("t4", [128, 4, 4])
    leb = small("leb", [128, 4, 16]); pen = small("pen", [128, 4, 16]); val = small("val", [128, 4, 16]); oh1 = small("oh1", [128, 4, 16])
    oh2 = small("oh2", [128, 4, 16]); m1 = small("m1", [128, 4]); t16 = small("t16", [128, 4, 16]); l1 = small("l1", [128, 4]); l2 = small("l2", [128, 4])
    w1p = small("w1p", [128, 4]); w2p = small("w2p", [128, 4]); comb = small("comb", [128, 4, 16])
    psa = Ring([P.ps("psa%d" % i, [128, 512]) for i in range(2)])
    psb = Ring([P.ps("psb%d" % i, [128, 512]) for i in range(2)])
    psbc = P.ps("psbc", [128, 512])
    pst = P.ps("pst", [128, 8, 128], BF16)
    pso = P.ps("pso", [128, 512])
    blocks = [(t0, n) for (t0, n) in BLOCKS if not (last and t0 < CT)]
    combTr = Ring([P.sb("combT%d" % i, [16, 512], F32) for i in range(2)])

    class Cx:
        pass

    def make_steps(bi):
        t0, n = blocks[bi]
        cx = Cx()
        cx.t0, cx.n, cx.cond, cx.nt = t0, n, (1 if t0 < CT else 0), n // 128
        cx.xt4 = xts[bi % 2]
        cx.hT = hr.next()
        cx.combT = combTr.next()
        cond, nt, xt4, hT, combT = cx.cond, cx.nt, cx.xt4, cx.hT, cx.combT
        steps = []

        def mk_norm(j):
            def f():
                xt = xt4[j]
                P.dma("sp", xt[:], d.XCUR[t0 + j * 128:t0 + (j + 1) * 128, :], writes=[xt])
                norm_transpose(P, g, xt, ssr.next(), rsr.next(), xsr.next(), pst, hT, j, 1, cond)
            return f
        for j in range(nt):
            steps.append(mk_norm(j))
        while len(steps) < 4:
            steps.append(lambda: None)

        def router_a():
            for j in range(nt):
                for k in range(8):
                    P.op("pe", lambda e, j=j, k=k, hT=hT: e.matmul(psmisc[:, j * 20:(j + 1) * 20], lhsT=hT[:, k, j * 128:(j + 1) * 128], rhs=wrt[:, k, :], start=(k == 0), stop=(k == 7)),
                         reads=[hT, wrt], writes=[psmisc])
            V = lambda t: t[:, 0:nt]
            P.op("dve", lambda e: e.tensor_copy(out=lgle[:, 0:nt, :], in_=psmisc[:, 0:nt * 20].rearrange("p (j c) -> p j c", c=20)), reads=[psmisc], writes=[lgle])
            lg = lgle[:, 0:nt, 0:4]
            le = lgle[:, 0:nt, 4:20]
            D_ = lambda fn, r, w: P.op("dve", fn, reads=r, writes=w)
            D_(lambda e: e.tensor_tensor(out=lgb[:, 0:nt, :], in0=lg, in1=brow[:, 0:4].unsqueeze(1).to_broadcast([128, nt, 4]), op=ALU.add), [lgle, brow], [lgb])
            D_(lambda e: e.reduce_max(out=mg[:, 0:nt], in_=lgb[:, 0:nt, :], axis=AX.X), [lgb], [mg])
            D_(lambda e: e.tensor_tensor(out=goh[:, 0:nt, :], in0=lgb[:, 0:nt, :], in1=mg[:, 0:nt].unsqueeze(2).to_broadcast([128, nt, 4]), op=ALU.is_ge), [lgb, mg], [goh])
            D_(lambda e: e.reduce_max(out=mg[:, 0:nt], in_=lg, axis=AX.X), [lgle], [mg])
            D_(lambda e: e.tensor_tensor(out=e0[:, 0:nt, :], in0=lg, in1=mg[:, 0:nt].unsqueeze(2).to_broadcast([128, nt, 4]), op=ALU.subtract), [lgle, mg], [e0])
            P.op("act", lambda e: e.activation(out=e0[:, 0:nt, :], in_=e0[:, 0:nt, :], func=AF.Exp), reads=[e0], writes=[e0])
            D_(lambda e: e.reduce_sum(out=s0[:, 0:nt], in_=e0[:, 0:nt, :], axis=AX.X), [e0], [s0])
            D_(lambda e: e.tensor_tensor(out=t4[:, 0:nt, :], in0=e0[:, 0:nt, :], in1=goh[:, 0:nt, :], op=ALU.mult), [e0, goh], [t4])
            D_(lambda e: e.reduce_sum(out=pg[:, 0:nt], in_=t4[:, 0:nt, :], axis=AX.X), [t4], [pg])
            D_(lambda e: e.reciprocal(out=s0[:, 0:nt], in_=s0[:, 0:nt]), [s0], [s0])
            D_(lambda e: e.tensor_tensor(out=pg[:, 0:nt], in0=pg[:, 0:nt], in1=s0[:, 0:nt], op=ALU.mult), [pg, s0], [pg])
            D_(lambda e: e.tensor_tensor(out=leb[:, 0:nt, :], in0=le, in1=brow[:, 4:20].unsqueeze(1).to_broadcast([128, nt, 16]), op=ALU.add), [lgle, brow], [leb])
            mask = goh[:, 0:nt, :].unsqueeze(3).to_broadcast([128, nt, 4, 4])
            r4 = lambda t: t[:, 0:nt, :].rearrange("p j (a b) -> p j a b", b=4)
            D_(lambda e: e.tensor_tensor(out=r4(val), in0=r4(leb), in1=mask, op=ALU.mult), [leb, goh], [val])
            D_(lambda e: e.tensor_scalar(out=r4(pen), in0=mask, scalar1=1e30, scalar2=-1e30, op0=ALU.mult, op1=ALU.add), [goh], [pen])
            D_(lambda e: e.tensor_tensor(out=val[:, 0:nt, :], in0=val[:, 0:nt, :], in1=pen[:, 0:nt, :], op=ALU.add), [val, pen], [val])
            D_(lambda e: e.reduce_max(out=m1[:, 0:nt], in_=val[:, 0:nt, :], axis=AX.X), [val], [m1])
            D_(lambda e: e.tensor_tensor(out=oh1[:, 0:nt, :], in0=val[:, 0:nt, :], in1=m1[:, 0:nt].unsqueeze(2).to_broadcast([128, nt, 16]), op=ALU.is_ge), [val, m1], [oh1])
            D_(lambda e: e.scalar_tensor_tensor(out=val[:, 0:nt, :], in0=oh1[:, 0:nt, :], scalar=-1e30, in1=val[:, 0:nt, :], op0=ALU.mult, op1=ALU.add), [oh1, val], [val])
            D_(lambda e: e.reduce_max(out=m1[:, 0:nt], in_=val[:, 0:nt, :], axis=AX.X), [val], [m1])
            D_(lambda e: e.tensor_tensor(out=oh2[:, 0:nt, :], in0=val[:, 0:nt, :], in1=m1[:, 0:nt].unsqueeze(2).to_broadcast([128, nt, 16]), op=ALU.is_ge), [val, m1], [oh2])
            D_(lambda e: e.tensor_tensor(out=t16[:, 0:nt, :], in0=le, in1=oh1[:, 0:nt, :], op=ALU.mult), [lgle, oh1], [t16])
            D_(lambda e: e.reduce_sum(out=l1[:, 0:nt], in_=t16[:, 0:nt, :], axis=AX.X), [t16], [l1])
            D_(lambda e: e.tensor_tensor(out=t16[:, 0:nt, :], in0=le, in1=oh2[:, 0:nt, :], op=ALU.mult), [lgle, oh2], [t16])
            D_(lambda e: e.reduce_sum(out=l2[:, 0:nt], in_=t16[:, 0:nt, :], axis=AX.X), [t16], [l2])
            D_(lambda e: e.tensor_tensor(out=l1[:, 0:nt], in0=l1[:, 0:nt], in1=l2[:, 0:nt], op=ALU.subtract), [l1, l2], [l1])
            P.op("act", lambda e: e.activation(out=l1[:, 0:nt], in_=l1[:, 0:nt], func=AF.Sigmoid), reads=[l1], writes=[l1])
            D_(lambda e: e.tensor_tensor(out=w1p[:, 0:nt], in0=l1[:, 0:nt], in1=pg[:, 0:nt], op=ALU.mult), [l1, pg], [w1p])
            D_(lambda e: e.tensor_tensor(out=w2p[:, 0:nt], in0=pg[:, 0:nt], in1=w1p[:, 0:nt], op=ALU.subtract), [pg, w1p], [w2p])
            D_(lambda e: e.tensor_tensor(out=comb[:, 0:nt, :], in0=oh1[:, 0:nt, :], in1=w1p[:, 0:nt].unsqueeze(2).to_broadcast([128, nt, 16]), op=ALU.mult), [oh1, w1p], [comb])
            D_(lambda e: e.tensor_tensor(out=t16[:, 0:nt, :], in0=oh2[:, 0:nt, :], in1=w2p[:, 0:nt].unsqueeze(2).to_broadcast([128, nt, 16]), op=ALU.mult), [oh2, w2p], [t16])
            D_(lambda e: e.tensor_tensor(out=comb[:, 0:nt, :], in0=comb[:, 0:nt, :], in1=t16[:, 0:nt, :], op=ALU.add), [comb, t16], [comb])

        def router_b():
            for j in range(nt):
                P.op("pe", lambda e, j=j: e.transpose(psmisc[0:16, j * 128:(j + 1) * 128], comb[:, j, :], g.identf[:]), reads=[comb, g.identf], writes=[psmisc])
            P.op("dve", lambda e: e.tensor_copy(out=combT[:, 0:n], in_=psmisc[0:16, 0:n]), reads=[psmisc], writes=[combT])

        steps.append(router_a)
        steps.append(router_b)
        return cx, steps

    def phase2(cx, nxt_steps):
        t0, n, cond, nt, xt4, hT, combT = cx.t0, cx.n, cx.cond, cx.nt, cx.xt4, cx.hT, cx.combT
        sched = {1: 0, 3: 1, 5: 2, 7: 3, 9: 4, 12: 5}
        hid = hidr.next()
        for e_ in range(16):
            if nxt_steps is not None and e_ in sched:
                nxt_steps[sched[e_]]()
            w13 = w13r.next()
            P.dma("sp", w13[:], d.W13B[e_], writes=[w13])
            P.op("pe", lambda e, e_=e_: e.matmul(psbc[:, 0:n], lhsT=sel[0:16, e_, :], rhs=combT[0:16, 0:n], start=True, stop=True), reads=[sel, combT], writes=[psbc])
            bcs = bcsr.next()
            P.op("act", lambda e, bcs=bcs: e.copy(out=bcs[:, 0:n], in_=psbc[:, 0:n]), reads=[psbc], writes=[bcs])
            for hh in range(2):
                pa = psa.next()
                pb = psb.next()
                for k in range(8):
                    P.op("pe", lambda e, pa=pa, k=k, hh=hh, w13=w13, hT=hT: e.matmul(pa[:, 0:n], lhsT=w13[:, k, hh * 128:(hh + 1) * 128], rhs=hT[:, k, 0:n], start=(k == 0), stop=(k == 7)),
                         reads=[w13, hT], writes=[pa])
                for k in range(8):
                    P.op("pe", lambda e, pb=pb, k=k, hh=hh, w13=w13, hT=hT: e.matmul(pb[:, 0:n], lhsT=w13[:, k, 256 + hh * 128:256 + (hh + 1) * 128], rhs=hT[:, k, 0:n], start=(k == 0), stop=(k == 7)),
                         reads=[w13, hT], writes=[pb])
                sa = sar.next()
                P.op("act", lambda e, sa=sa, pa=pa: e.activation(out=sa[:, 0:n], in_=pa[:, 0:n], func=AF.Silu), reads=[pa], writes=[sa])
                tm = tmr.next()
                P.op("dve", lambda e, tm=tm, sa=sa, pb=pb: e.tensor_tensor(out=tm[:, 0:n], in0=pb[:, 0:n], in1=sa[:, 0:n], op=ALU.mult), reads=[pb, sa], writes=[tm])
                P.op("pool", lambda e, tm=tm, bcs=bcs, e_=e_, hh=hh, hid=hid: e.tensor_tensor(out=hid[:, 2 * e_ + hh, 0:n], in0=tm[:, 0:n], in1=bcs[:, 0:n], op=ALU.mult), reads=[tm, bcs], writes=[hid])
        for j in range(nt):
            xt = xt4[j]
            tt = t0 + j * 128
            for hf in range(2):
                for kc in range(32):
                    P.op("pe", lambda e, kc=kc, j=j, hf=hf, hid=hid: e.matmul(pso[:], lhsT=hid[:, kc, j * 128:(j + 1) * 128], rhs=w2all[:, kc, hf * 512:(hf + 1) * 512], start=(kc == 0), stop=(kc == 31)),
                         reads=[hid, w2all], writes=[pso])
                tm = tmr.next()
                P.op("dve", lambda e, tm=tm, hf=hf, cond=cond: e.tensor_tensor(out=tm[:], in0=pso[:], in1=g.gbc[:, 1, cond, hf * 512:(hf + 1) * 512], op=ALU.mult), reads=[pso, g.gbc], writes=[tm])
                P.op("pool", lambda e, xt=xt, tm=tm, hf=hf: e.tensor_tensor(out=xt[:, hf * 512:(hf + 1) * 512], in0=xt[:, hf * 512:(hf + 1) * 512], in1=tm[:], op=ALU.add), reads=[xt, tm], writes=[xt])
            if last:
                P.dma("pool", d.out[tt - CT:tt - CT + 128, :], xt[:], reads=[xt])
            else:
                P.dma("pool", d.XCUR[tt:tt + 128, :], xt[:], reads=[xt])

    cx, steps = make_steps(0)
    for st in steps:
        st()
    for bi in range(len(blocks)):
        if bi + 1 < len(blocks):
            ncx, nsteps = make_steps(bi + 1)
        else:
            ncx, nsteps = None, None
        phase2(cx, nsteps)
        cx = ncx
    P.end_stage()


def stage_D(P, l, d, g):
    P.begin_stage("D%d" % l)
    wuq = P.sb("wuq", [128, 2, 768], BF16)
    wukv = P.sb("wukv", [128, 1024], BF16)
    P.dma("pool", wuq[:], d.mla_w_uq[l].rearrange("(k p) n -> p k n", p=128), writes=[wuq])
    P.dma("pool", wukv[:], d.mla_w_ukv[l], writes=[wukv])
    nrm = P.sb("nrm", [128, 3], F32)
    P.dma("sp", nrm[:, 0:2], d.mla_q_norm[l].rearrange("(k p) -> p k", p=128), writes=[nrm], allow_slow_non_contiguous=True)
    P.dma("sp", nrm[:, 2:3], d.mla_kv_norm[l].rearrange("(k p) -> p k", p=128), writes=[nrm], allow_slow_non_contiguous=True)
    gqk = P.sb("gqk", [128, 16, 96], F32)
    def _dst(ps):
        P.op("dve", lambda e: e.tensor_copy(out=gqk[:, 0:8, :], in_=ps[:, 0:96].unsqueeze(1).to_broadcast([128, 8, 96])), reads=[ps], writes=[gqk])
        P.op("dve", lambda e: e.tensor_copy(out=gqk[:, 8:16, :], in_=ps[:, 96:192].unsqueeze(1).to_broadcast([128, 8, 96])), reads=[ps], writes=[gqk])
    bcast_rows(P, _dst, [d.mla_qk_gain_q[l:l + 1, :], d.mla_qk_gain_k[l:l + 1, :]], [96, 96])
    invn = P.sb("invn", [128, 2], F32)
    P.op("dve", lambda e: e.memset(invn[:, 0:1], 1.0 / 256.0), writes=[invn])
    P.op("dve", lambda e: e.memset(invn[:, 1:2], 1.0 / 128.0), writes=[invn])
    epst = P.sb("epst", [128, 1], F32)
    P.op("dve", lambda e: e.memset(epst[:], EPS), writes=[epst])
    tmr = Ring([P.sb("tm%d" % i, [128, 416], F32) for i in range(2)])
    junk = P.sb("junk", [128, 256], F32)
    ssr = Ring([P.sb("ss%d" % i, [128, 2], F32) for i in range(2)])
    dsr = Ring([P.sb("ds%d" % i, [128, 384], BF16) for i in range(2)])
    dTr = Ring([P.sb("dT%d" % i, [128, 3, 128], BF16) for i in range(2)])
    qkr = Ring([P.sb("qk%d" % i, [128, 16, 96], F32) for i in range(2)])
    sqr = Ring([P.sb("sq%d" % i, [128, 16, 96], F32) for i in range(1)])
    sshr = Ring([P.sb("ssh%d" % i, [128, 16], F32) for i in range(2)])
    qkbr = Ring([P.sb("qkb%d" % i, [128, 16, 96], BF16) for i in range(2)])
    vxr = Ring([P.sb("vx%d" % i, [128, 8, 65], BF16) for i in range(2)])
    for b in vxr.bufs:
        P.op("dve", lambda e, b=b: e.memset(b[:], 1.0), writes=[b])
    csr = Ring([P.sb("cs%d" % i, [128, 2, 16], F32) for i in range(2)])
    rtr = Ring([P.sb("rt%d" % i, [128, 4, 16, 16], F32) for i in range(2)])
    qTr = Ring([P.sb("qT%d" % i, [96, 8, 512], BF16) for i in range(2)])
    kTr = Ring([P.sb("kT%d" % i, [96, 8, 512], BF16) for i in range(2)])
    pst = P.ps("pst", [128, 3, 128], BF16)
    psq = [P.ps("psq%d" % i, [128, 384]) for i in range(2)]
    pskv = [P.ps("pskv%d" % i, [128, 512]) for i in range(2)]
    psT = [P.ps("psT%d" % i, [96, 8, 128], BF16) for i in range(2)]
    import os
    nblk = int(os.environ.get("MK_D_NBLK", "99"))
    skip = os.environ.get("MK_D_SKIP", "").split(",")
    tiles = []
    for (t0, n) in BLOCKS[:nblk]:
        for j in range(n // 128):
            tiles.append((t0, n, j))

    def phase1(tile):
        t0, n, j = tile
        tt = t0 + j * 128
        tm = tmr.next()
        P.dma("sp", tm[:], d.TM[tt:tt + 128, :], writes=[tm])
        ss = ssr.next()
        P.op("pool", lambda e, ss=ss: e.memset(ss[:], 0.0), writes=[ss])
        P.op("act", lambda e, tm=tm, ss=ss: e.activation(out=junk[:, 0:256], in_=tm[:, 0:256], func=AF.Square, accum_out=ss[:, 0:1]), reads=[tm, ss], writes=[junk, ss])
        P.op("act", lambda e, tm=tm, ss=ss: e.activation(out=junk[:, 0:128], in_=tm[:, 256:384], func=AF.Square, accum_out=ss[:, 1:2]), reads=[tm, ss], writes=[junk, ss])
        P.op("dve", lambda e, ss=ss: e.tensor_tensor(out=ss[:], in0=ss[:], in1=invn[:], op=ALU.mult), reads=[ss, invn], writes=[ss])
        P.op("act", lambda e, ss=ss: e.activation(out=ss[:], in_=ss[:], func=AF.Ln, bias=epst[:, 0:1]), reads=[ss, epst], writes=[ss])
        P.op("act", lambda e, ss=ss: e.activation(out=ss[:], in_=ss[:], func=AF.Exp, scale=-0.5), reads=[ss], writes=[ss])
        ds = dsr.next()
        P.op("dve", lambda e, ds=ds, tm=tm, ss=ss: e.tensor_scalar(out=ds[:, 0:256], in0=tm[:, 0:256], scalar1=ss[:, 0:1], scalar2=None, op0=ALU.mult), reads=[tm, ss], writes=[ds])
        P.op("dve", lambda e, ds=ds, tm=tm, ss=ss: e.tensor_scalar(out=ds[:, 256:384], in0=tm[:, 256:384], scalar1=ss[:, 1:2], scalar2=None, op0=ALU.mult), reads=[tm, ss], writes=[ds])
        for k in range(3):
            P.op("pe", lambda e, k=k, ds=ds: e.transpose(pst[:, k, :], ds[:, k * 128:(k + 1) * 128], g.identb[:]), reads=[ds, g.identb], writes=[pst])
        dT = dTr.next()
        for k in range(3):
            P.op("act", lambda e, k=k, dT=dT: e.activation(out=dT[:, k, :], in_=pst[:, k, :], func=AF.Copy, scale=nrm[:, k:k + 1]), reads=[pst, nrm], writes=[dT])
        return tm, dT

    blkbuf = {}

    def phase2(tile, tm, dT):
        t0, n, j = tile
        tt = t0 + j * 128
        is_ctx = t0 < CT
        if j == 0:
            blkbuf["qT"] = qTr.next()
            blkbuf["kT"] = kTr.next()
        qT, kT = blkbuf["qT"], blkbuf["kT"]
        for hf in range(2):
            for k in range(2):
                P.op("pe", lambda e, hf=hf, k=k, dT=dT: e.matmul(psq[hf][:], lhsT=dT[:, k, :], rhs=wuq[:, k, hf * 384:(hf + 1) * 384], start=(k == 0), stop=(k == 1)),
                     reads=[dT, wuq], writes=[psq[hf]])
            P.op("pe", lambda e, hf=hf, dT=dT: e.matmul(pskv[hf][:], lhsT=dT[:, 2, :], rhs=wukv[:, hf * 512:(hf + 1) * 512], start=True, stop=True),
                 reads=[dT, wukv], writes=[pskv[hf]])
        qk = qkr.next()
        vx = vxr.next()
        for hf in range(2):
            P.op("act", lambda e, hf=hf, qk=qk: e.copy(out=qk[:, 4 * hf:4 * hf + 4, :], in_=psq[hf][:].rearrange("p (h c) -> p h c", c=96)), reads=[psq[hf]], writes=[qk])
            kvv = pskv[hf][:].rearrange("p (h c) -> p h c", c=128)
            P.op("dve", lambda e, hf=hf, qk=qk, kvv=kvv: e.tensor_copy(out=qk[:, 8 + 4 * hf:8 + 4 * hf + 4, 0:64], in_=kvv[:, :, 0:64]), reads=[pskv[hf]], writes=[qk])
            P.op("act", lambda e, hf=hf, vx=vx, kvv=kvv: e.copy(out=vx[:, 4 * hf:4 * hf + 4, 0:64], in_=kvv[:, :, 64:128]), reads=[pskv[hf]], writes=[vx])
        P.op("dve", lambda e, qk=qk, tm=tm: e.tensor_copy(out=qk[:, 8:16, 64:96], in_=tm[:, 384:416].unsqueeze(1).to_broadcast([128, 8, 32])), reads=[tm], writes=[qk])
        P.dma("pool", d.VX[tt:tt + 128, :, :], vx[:], reads=[vx])
        sq = sqr.next()
        ssh = sshr.next()
        P.op("dve", lambda e, sq=sq, qk=qk: e.tensor_tensor(out=sq[:], in0=qk[:], in1=qk[:], op=ALU.mult), reads=[qk], writes=[sq])
        P.op("dve", lambda e, sq=sq, ssh=ssh: e.reduce_sum(out=ssh[:], in_=sq[:], axis=AX.X), reads=[sq], writes=[ssh])
        P.op("dve", lambda e, ssh=ssh: e.tensor_scalar(out=ssh[:], in0=ssh[:], scalar1=1.0 / 96.0, scalar2=EPS, op0=ALU.mult, op1=ALU.add), reads=[ssh], writes=[ssh])
        P.op("act", lambda e, ssh=ssh: e.activation(out=ssh[:], in_=ssh[:], func=AF.Ln), reads=[ssh], writes=[ssh])
        P.op("act", lambda e, ssh=ssh: e.activation(out=ssh[:], in_=ssh[:], func=AF.Exp, scale=-0.5), reads=[ssh], writes=[ssh])
        P.op("dve", lambda e, qk=qk, ssh=ssh: e.tensor_tensor(out=qk[:], in0=qk[:], in1=ssh[:].unsqueeze(2).to_broadcast([128, 16, 96]), op=ALU.mult), reads=[qk, ssh], writes=[qk])
        qkb = qkbr.next()
        if is_ctx or "rope" in skip:
            P.op("dve", lambda e, qk=qk, qkb=qkb: e.tensor_tensor(out=qkb[:], in0=qk[:], in1=gqk[:], op=ALU.mult), reads=[qk, gqk], writes=[qkb])
        else:
            P.op("dve", lambda e, qk=qk: e.tensor_tensor(out=qk[:], in0=qk[:], in1=gqk[:], op=ALU.mult), reads=[qk, gqk], writes=[qk])
            cs = csr.next()
            P.dma("sp", cs[:, 0, :], d.k_cos[tt - CT:tt - CT + 128, :], writes=[cs])
            P.dma("sp", cs[:, 1, :], d.k_sin[tt - CT:tt - CT + 128, :], writes=[cs])
            rt = rtr.next()
            cosb = cs[:, 0, :].unsqueeze(1).to_broadcast([128, 16, 16])
            sinb = cs[:, 1, :].unsqueeze(1).to_broadcast([128, 16, 16])
            r1 = qk[:, :, 64:80]
            r2 = qk[:, :, 80:96]
            P.op("dve", lambda e, rt=rt, r1=r1, cosb=cosb: e.tensor_tensor(out=rt[:, 0, :, :], in0=r1, in1=cosb, op=ALU.mult), reads=[qk, cs], writes=[rt])
            P.op("dve", lambda e, rt=rt, r2=r2, sinb=sinb: e.tensor_tensor(out=rt[:, 1, :, :], in0=r2, in1=sinb, op=ALU.mult), reads=[qk, cs], writes=[rt])
            P.op("dve", lambda e, rt=rt, r2=r2, cosb=cosb: e.tensor_tensor(out=rt[:, 2, :, :], in0=r2, in1=cosb, op=ALU.mult), reads=[qk, cs], writes=[rt])
            P.op("dve", lambda e, rt=rt, r1=r1, sinb=sinb: e.tensor_tensor(out=rt[:, 3, :, :], in0=r1, in1=sinb, op=ALU.mult), reads=[qk, cs], writes=[rt])
            P.op("act", lambda e, qk=qk, qkb=qkb: e.copy(out=qkb[:, :, 0:64], in_=qk[:, :, 0:64]), reads=[qk], writes=[qkb])
            P.op("dve", lambda e, rt=rt, qkb=qkb: e.tensor_tensor(out=qkb[:, :, 64:80], in0=rt[:, 0, :, :], in1=rt[:, 1, :, :], op=ALU.subtract), reads=[rt], writes=[qkb])
            P.op("dve", lambda e, rt=rt, qkb=qkb: e.tensor_tensor(out=qkb[:, :, 80:96], in0=rt[:, 2, :, :], in1=rt[:, 3, :, :], op=ALU.add), reads=[rt], writes=[qkb])
        for i in range(16):
            P.op("pe", lambda e, i=i, qkb=qkb: e.transpose(psT[i // 8][:, i % 8, :], qkb[:, i, :], g.identb[:]), reads=[qkb, g.identb], writes=[psT[i // 8]])
        P.op("act", lambda e, qT=qT, j=j: e.copy(out=qT[:, :, j * 128:(j + 1) * 128], in_=psT[0][:]), reads=[psT[0]], writes=[qT])
        P.op("dve", lambda e, kT=kT, j=j: e.tensor_copy(out=kT[:, :, j * 128:(j + 1) * 128], in_=psT[1][:]), reads=[psT[1]], writes=[kT])
        if j == n // 128 - 1:
            if "stores" not in skip:
                P.dma("sp", d.QT[:, :, t0:t0 + n].rearrange("h c t -> c h t"), qT[:, :, 0:n], reads=[qT])
                P.dma("pool", d.KT[:, :, t0:t0 + n].rearrange("h c t -> c h t"), kT[:, :, 0:n], reads=[kT])


    cur = phase1(tiles[0])
    for ti_ in range(len(tiles)):
        nxt = phase1(tiles[ti_ + 1]) if ti_ + 1 < len(tiles) else None
        phase2(tiles[ti_], *cur)
        cur = nxt
    P.end_stage()


def body_E(P, l, d, g, last, merged=False):
    scale = 96.0 ** -0.5
    onesf = P.sb("onesf", [128, 64], F32)
    P.op("dve", lambda e: e.memset(onesf[:], 1.0), writes=[onesf])
    kTr = Ring([P.sb("kT%d" % i, [96, NT], BF16) for i in range(1 if merged else 2)])
    qbr = Ring([P.sb("qb%d" % i, [96, 512], BF16) for i in range(3)])
    vxr = Ring([P.sb("vx%d" % i, [128, 66, 65], BF16) for i in range(1 if merged else 2)])
    ptr = Ring([P.sb("pt%d" % i, [128, 512], BF16) for i in range(4)])
    osr = Ring([P.sb("os%d" % i, [65, 512], F32) for i in range(2)])
    rcr = Ring([P.sb("rc%d" % i, [64, 512], F32) for i in range(2)])
    obr = Ring([P.sb("ob%d" % i, [64, 512], BF16) for i in range(2)])
    pss = Ring([P.ps("pss%d" % i, [128, 512]) for i in range(3 if merged else 4)])
    pso = Ring([P.ps("pso%d" % i, [128, 512]) for i in range(1 if merged else 2)])
    psb_own = None if merged else P.ps("psb", [64, 512])
    yield
    for h in range(8):
        kT = kTr.next()
        vx = vxr.next()
        P.dma("sp", kT[:], d.KT[h], writes=[kT])
        P.dma("pool", vx[:], d.VX[:, h, :].rearrange("(kt p) c -> p kt c", p=128), writes=[vx])
        for (t0, n) in BLOCKS:
            is_ctx = t0 < CT
            if is_ctx and last:
                continue
            kts = [0, 1] if is_ctx else list(range(66))
            po = pso.next()
            qb = qbr.next()
            P.dma("sp", qb[:, 0:n], d.QT[h, :, t0:t0 + n], writes=[qb])
            nk = len(kts)
            LOOK = 2
            pss_q = []

            def issue_s(ki):
                kt = kts[ki]
                ps = pss.next()
                P.op("pe", lambda e: e.matmul(ps[:, 0:n], lhsT=kT[:, kt * 128:(kt + 1) * 128], rhs=qb[:, 0:n], start=True, stop=True),
                     reads=[kT, qb], writes=[ps])
                pss_q.append(ps)
            for ki in range(min(LOOK, nk)):
                issue_s(ki)
            for ki, kt in enumerate(kts):
                ps = pss_q[ki]
                pt = ptr.next()
                P.op("act", lambda e: e.activation(out=pt[:, 0:n], in_=ps[:, 0:n], func=AF.Exp, scale=scale), reads=[ps], writes=[pt])
                if ki + LOOK < nk:
                    issue_s(ki + LOOK)
                P.op("pe", lambda e: e.matmul(po[0:65, 0:n], lhsT=vx[:, kt, :], rhs=pt[:, 0:n], start=(ki == 0), stop=(ki == nk - 1)),
                     reads=[vx, pt], writes=[po])
            psb = psb_own if psb_own is not None else pss.next()
            osb = osr.next()
            P.op("dve", lambda e, osb=osb, po=po, n=n: e.tensor_copy(out=osb[:, 0:n], in_=po[0:65, 0:n]), reads=[po], writes=[osb])
            P.op("pe", lambda e, osb=osb, n=n: e.matmul(psb[0:64, 0:n], lhsT=onesf[64:65, 0:64], rhs=osb[64:65, 0:n], start=True, stop=True), reads=[onesf, osb], writes=[psb])
            rc = rcr.next()
            P.op("dve", lambda e, rc=rc, n=n: e.reciprocal(out=rc[:, 0:n], in_=psb[0:64, 0:n]), reads=[psb], writes=[rc])
            ob = obr.next()
            P.op("pool", lambda e, ob=ob, osb=osb, rc=rc, n=n: e.tensor_tensor(out=ob[:, 0:n], in0=osb[0:64, 0:n], in1=rc[:, 0:n], op=ALU.mult), reads=[osb, rc], writes=[ob])
            P.dma("pool", d.OMLA[h * 64:(h + 1) * 64, t0:t0 + n], ob[:, 0:n], reads=[ob])
            yield


def stage_E(P, l, d, g, last):
    P.begin_stage("E%d" % l)
    for _ in body_E(P, l, d, g, last):
        pass
    P.end_stage()


def body_B(P, l, d, g, merged=False):
    NB = 256
    NCH = NB // 32
    tri = P.sb("tri", [32, 2, 32], F32)
    P.dma("sp", tri[:], d.k_tri[:, :, :], writes=[tri])
    lb = P.sb("lb", [128, 4, 2], F32)
    oml = P.sb("oml", [128, 4, 2], F32)
    if l > 0:
        lb0 = P.sb("lb0", [128, 4, 2], F32)
        for r_ in range(2):
            P.dma("sp", lb[:, :, r_], d.hg_lb[l, r_].rearrange("(t p) -> p t", p=128), writes=[lb], allow_slow_non_contiguous=True)
            P.dma("sp", lb0[:, :, r_], d.hg_lb[0, r_].rearrange("(t p) -> p t", p=128), writes=[lb0], allow_slow_non_contiguous=True)
        P.op("dve", lambda e: e.tensor_tensor(out=lb[:], in0=lb[:], in1=lb0[:], op=ALU.subtract), reads=[lb, lb0], writes=[lb])
        P.op("act", lambda e: e.activation(out=lb[:], in_=lb[:], func=AF.Sigmoid), reads=[lb], writes=[lb])
        P.op("dve", lambda e: e.tensor_scalar(out=oml[:], in0=lb[:], scalar1=-1.0, scalar2=1.0, op0=ALU.mult, op1=ALU.add), reads=[lb], writes=[oml])
    masks = []
    for dr in range(2):
        m = P.sb("mask%d" % dr, [128, 4 * NB], F32)
        P.op("pool", lambda e: e.memset(m[:], 1.0), writes=[m])
        mv = m[:].rearrange("p (c j) -> p c j", j=32)
        pos = 0 if dr == 0 else 31
        P.op("pool", lambda e: e.memset(mv[:, :, pos:pos + 1], 0.0), writes=[m])
        masks.append(m)

    class St:
        pass
    sts = []
    if merged is True:
        bankKA = P.ps_bank("psKA")
        sh_psK = bankKA.alias(bankKA.t[0:32, 0:256].bitcast(BF16), "psK")
        sh_psA = bankKA.alias(bankKA.t[0:32, 256:512], "psA")
        sh_psO = P.ps("psO", [32, 512])
        sh_psS = P.ps("psS", [128, 4, 128])
    cp = "dve"
    for dr in range(2):
        s = St()
        s.zt = P.sb("zt%d" % dr, [128, 4, NB], F32)
        s.kk = P.sb("kk%d" % dr, [128, 4, NB], F32)
        s.b = P.sb("b%d" % dr, [128, 4, NB], F32)
        s.eb = P.sb("eb%d" % dr, [128, 4, NB], F32)
        s.ebt_r = Ring([P.sb("ebt%d_%d" % (dr, i), [128, 4, NCH], F32) for i in range(2)])
        s.qt_r = Ring([P.sb("qt%d_%d" % (dr, i), [128, 4, NB], BF16) for i in range(2)])
        s.Qt_r = Ring([P.sb("Qt%d_%d" % (dr, i), [128, 4, NB], BF16) for i in range(2)])
        s.Kt_r = Ring([P.sb("Kt%d_%d" % (dr, i), [128, 4, NB], BF16) for i in range(2)])
        s.Kh_r = Ring([P.sb("Kh%d_%d" % (dr, i), [128, 4, NB], BF16) for i in range(2)])
        s.V_r = Ring([P.sb("V%d_%d" % (dr, i), [32, NCH, 512], BF16) for i in range(2)])
        s.ost = Ring([P.sb("ost%d_%d" % (dr, i), [32, NCH, 512], F32) for i in range(1 if merged else 2)])
        s.Sp = [P.sb("S%d_%d" % (dr, i), [128, 4, 64], F32) for i in range(2)]
        s.Sbp = [P.sb("Sb%d_%d" % (dr, i), [128, 4, 64], BF16) for i in range(2)]
        s.step = [0]
        s.tmpS = P.sb("tmpS%d" % dr, [128, 4, 64], F32)
        s.khat = Ring([P.sb("khat%d_%d" % (dr, i), [32, 512], BF16) for i in range(2)])
        s.AT = Ring([P.sb("AT%d_%d" % (dr, i), [32, 8, 32], BF16) for i in range(2)])
        if merged is True:
            s.psK, s.psA, s.psO, s.psS = sh_psK, sh_psA, sh_psO, sh_psS
        elif merged == "ka":
            bk = P.ps_bank("psKA%d" % dr)
            s.psK = bk.alias(bk.t[0:32, 0:256].bitcast(BF16), "psK")
            s.psA = bk.alias(bk.t[0:32, 256:512], "psA")
            s.psO = P.ps("psO%d" % dr, [32, 512])
            s.psS = P.ps("psS%d" % dr, [128, 4, 128])
        else:
            s.psK = P.ps("psK%d" % dr, [32, 512], BF16)
            s.psA = P.ps("psA%d" % dr, [32, 256])
            s.psO = P.ps("psO%d" % dr, [32, 512])
            s.psS = P.ps("psS%d" % dr, [128, 4, 128])
        for i_ in range(2):
            P.op("dve", lambda e: e.memset(s.Sp[i_][:], 0.0), writes=[s.Sp[i_]])
            P.op("dve", lambda e: e.memset(s.Sbp[i_][:], 0.0), writes=[s.Sbp[i_]])
        sts.append(s)

    blocks_f = [(i * NB, NB) for i in range(NT // NB)]
    blocks_b = [(0, NB)] + [(i * NB, NB) for i in reversed(range(1, NT // NB))]

    def prep_steps(dr, t0, n):
        s = sts[dr]

        class Cx:
            pass
        cx = Cx()
        cx.ebt, cx.qt, cx.Qt, cx.Kt, cx.Kh, cx.V = s.ebt_r.next(), s.qt_r.next(), s.Qt_r.next(), s.Kt_r.next(), s.Kh_r.next(), s.V_r.next()
        cx.t0, cx.n = t0, n
        s_ = s
        s = St()
        s.__dict__.update(s_.__dict__)
        s.__dict__.update(cx.__dict__)
        zrow = dr * 512
        fl = lambda t: t[:].rearrange("p a n -> p (a n)")

        def st0():
            P.dma("sp", s.zt[:], d.FZ[zrow:zrow + 512, t0:t0 + n].rearrange("(a p) t -> p a t", p=128), writes=[s.zt])
            P.dma("sp", s.qt[:], d.FQ[:, t0:t0 + n].rearrange("(a p) t -> p a t", p=128), writes=[s.qt])
            P.dma("pool", s.V[:], d.TV[t0:t0 + n, 0:512].rearrange("(c p) f -> p c f", p=32), writes=[s.V])

        def st1():
            P.op("act", lambda e: e.activation(out=fl(s.zt), in_=fl(s.zt), func=AF.Exp, scale=-1.0), reads=[s.zt], writes=[s.zt])

        def st2():
            P.op("dve", lambda e: e.tensor_scalar(out=fl(s.zt), in0=fl(s.zt), scalar1=1.0, scalar2=None, op0=ALU.add), reads=[s.zt], writes=[s.zt])
            P.op("dve", lambda e: e.reciprocal(out=fl(s.zt), in_=fl(s.zt)), reads=[s.zt], writes=[s.zt])
            if l > 0:
                P.op("dve", lambda e: e.tensor_tensor(out=s.zt[:], in0=s.zt[:], in1=oml[:, :, dr].unsqueeze(2).to_broadcast([128, 4, n]), op=ALU.mult), reads=[s.zt, oml], writes=[s.zt])
                P.op("dve", lambda e: e.tensor_tensor(out=s.zt[:], in0=s.zt[:], in1=lb[:, :, dr].unsqueeze(2).to_broadcast([128, 4, n]), op=ALU.add), reads=[s.zt, lb], writes=[s.zt])
            P.op("dve", lambda e: e.tensor_scalar(out=fl(s.kk), in0=fl(s.zt), scalar1=-1.0, scalar2=1.0, op0=ALU.mult, op1=ALU.add), reads=[s.zt], writes=[s.kk])

        def st3():
            P.op("act", lambda e: e.activation(out=fl(s.zt), in_=fl(s.zt), func=AF.Ln), reads=[s.zt], writes=[s.zt])

        def st4():
            if dr == 0:
                P.op("dve", lambda e: e.tensor_tensor_scan(out=fl(s.b), data0=masks[0][:], data1=fl(s.zt), initial=0.0, op0=ALU.mult, op1=ALU.add), reads=[masks[0], s.zt], writes=[s.b])
            else:
                P.op("dve", lambda e: e.tensor_tensor_scan(out=fl(s.b)[:, ::-1], data0=masks[1][:][:, ::-1], data1=fl(s.zt)[:, ::-1], initial=0.0, op0=ALU.mult, op1=ALU.add),
                     reads=[masks[1], s.zt], writes=[s.b])

        def st5():
            P.op("act", lambda e: e.activation(out=fl(s.eb), in_=fl(s.b), func=AF.Exp), reads=[s.b], writes=[s.eb])
            P.op("act", lambda e: e.activation(out=fl(s.b), in_=fl(s.b), func=AF.Exp, scale=-1.0), reads=[s.b], writes=[s.b])

        def st6():
            P.op("dve", lambda e: e.tensor_tensor(out=fl(s.Qt), in0=fl(s.qt), in1=fl(s.eb), op=ALU.mult), reads=[s.qt, s.eb], writes=[s.Qt])
            P.op("dve", lambda e: e.tensor_tensor(out=fl(s.Kt), in0=fl(s.kk), in1=fl(s.b), op=ALU.mult), reads=[s.kk, s.b], writes=[s.Kt])
            lastpos = 31 if dr == 0 else 0
            eb4 = s.eb[:].rearrange("p a (c j) -> p a c j", j=32)
            P.op("dve", lambda e: e.tensor_copy(out=s.ebt[:], in_=eb4[:, :, :, lastpos]), reads=[s.eb], writes=[s.ebt])
            P.op("dve", lambda e: e.tensor_tensor(out=s.Kh[:].rearrange("p a (c j) -> p a c j", j=32), in0=s.Kt[:].rearrange("p a (c j) -> p a c j", j=32),
                                                  in1=s.ebt[:].unsqueeze(3).to_broadcast([128, 4, NCH, 32]), op=ALU.mult), reads=[s.Kt, s.ebt], writes=[s.Kh])
        return cx, [st0, st1, st2, st3, st4, st5, st6]

    def prep(dr, t0, n):
        cx, steps = prep_steps(dr, t0, n)
        for f_ in steps:
            f_()
        return cx

    def chunks(dr, cx, nxt):
        s_ = sts[dr]
        s = St()
        s.__dict__.update(s_.__dict__)
        s.__dict__.update(cx.__dict__)
        t0, n = cx.t0, cx.n
        ost = s_.ost.next()
        order = list(range(NCH) if dr == 0 else reversed(range(NCH)))
        res = [None]
        nsteps = []
        if nxt is not None:
            res[0], nsteps = prep_steps(dr, *nxt)
        for ci, c in enumerate(order):
            if ci < len(nsteps):
                nsteps[ci]()
            o = c * 32
            khat = s.khat.next()
            for a in range(4):
                P.op("pe", lambda e: e.transpose(s.psK[0:32, a * 128:(a + 1) * 128], s.Kh[:, a, o:o + 32], g.identb[:]), reads=[s.Kh, g.identb], writes=[s.psK])
            P.op("act", lambda e: e.copy(out=khat[:], in_=s.psK[:]), reads=[s.psK], writes=[khat])
            for h in (0, 2, 4, 6, 1, 3, 5, 7):
                a, pr = h // 2, (h % 2) * 64
                P.op("pe", lambda e: e.matmul(s.psA[0:32, h * 32:(h + 1) * 32], lhsT=s.Kt[pr:pr + 64, a, o:o + 32], rhs=s.Qt[pr:pr + 64, a, o:o + 32], start=True, stop=True),
                     reads=[s.Kt, s.Qt], writes=[s.psA], rg=(pr // 32, pr // 32 + 1))
            AT = s.AT.next()
            P.op("dve", lambda e: e.tensor_tensor(out=AT[:], in0=s.psA[:].rearrange("p (h t) -> p h t", t=32), in1=tri[:, dr, :].unsqueeze(1).to_broadcast([32, 8, 32]), op=ALU.mult),
                 reads=[s.psA, tri], writes=[AT])
            k_ = s_.step[0]
            s_.step[0] += 1
            Sprev, Snew = s.Sp[k_ % 2], s.Sp[(k_ + 1) % 2]
            Sbprev, Sbnew = s.Sbp[k_ % 2], s.Sbp[(k_ + 1) % 2]
            for a in range(4):
                P.op("pe", lambda e: e.matmul(s.psS[:, a, :], lhsT=khat[0:32, a * 128:(a + 1) * 128], rhs=s.V[0:32, c, a * 128:(a + 1) * 128], start=True, stop=True),
                     reads=[khat, s.V], writes=[s.psS], rg=(0,))
            for hf in range(2):
                ph = slice(hf * 64, (hf + 1) * 64)
                P.op("dve", lambda e: e.tensor_tensor(out=s.tmpS[ph, :, :], in0=Sprev[ph, :, :], in1=s.ebt[ph, :, c].unsqueeze(2).to_broadcast([64, 4, 64]), op=ALU.mult),
                     reads=[Sprev, s.ebt], writes=[s.tmpS])
                P.op("dve", lambda e: e.tensor_tensor(out=Snew[ph, :, :], in0=s.tmpS[ph, :, :], in1=s.psS[ph, :, hf * 64:(hf + 1) * 64], op=ALU.add),
                     reads=[s.tmpS, s.psS], writes=[Snew])
            P.op("act", lambda e: e.copy(out=Sbnew[:], in_=Snew[:]), reads=[Snew], writes=[Sbnew])
            for hi, h in enumerate((0, 2, 4, 6, 1, 3, 5, 7)):
                a, pr = h // 2, (h % 2) * 64
                P.op("pe", lambda e: e.matmul(s.psO[0:32, h * 64:(h + 1) * 64], lhsT=s.Qt[pr:pr + 64, a, o:o + 32], rhs=Sbprev[pr:pr + 64, a, :], start=(hi == 0), stop=False, skip_group_check=True),
                     reads=[s.Qt, Sbprev], writes=[s.psO], rg=(pr // 32, pr // 32 + 1))
            for h in range(8):
                P.op("pe", lambda e: e.matmul(s.psO[0:32, h * 64:(h + 1) * 64], lhsT=AT[0:32, h, :], rhs=s.V[0:32, c, h * 64:(h + 1) * 64], start=False, stop=True, skip_group_check=True),
                     reads=[AT, s.V], writes=[s.psO], rg=(0,))
            P.op("act", lambda e: e.copy(out=ost[:, c, :], in_=s.psO[:]), reads=[s.psO], writes=[ost])
            yield
        P.dma("pool", d.ODIR[dr, t0:t0 + n, :].rearrange("(c p) f -> p c f", p=32), ost[:], reads=[ost])
        return res[0]

    def dirgen(dr, blocks):
        cx = prep(dr, *blocks[0])
        for i in range(len(blocks)):
            nxt = blocks[i + 1] if i + 1 < len(blocks) else None
            cx = yield from chunks(dr, cx, nxt)

    yield
    gf, gb = dirgen(0, blocks_f), dirgen(1, blocks_b)
    alive = [True, True]
    while any(alive):
        for gi, gen in enumerate((gf, gb)):
            if alive[gi]:
                try:
                    next(gen)
                except StopIteration:
                    alive[gi] = False
        yield


def stage_B(P, l, d, g):
    P.begin_stage("B%d" % l)
    for _ in body_B(P, l, d, g):
        pass
    P.end_stage()


def stage_BC(P, l, d, g):
    P.begin_stage("BC%d" % l)
    gb = body_B(P, l, d, g, "ka")
    gc = body_C(P, l, d, g, True)
    next(gb)
    next(gc)
    alive = [True, True]
    while any(alive):
        for gi, gen in enumerate((gb, gc)):
            if alive[gi]:
                try:
                    next(gen)
                except StopIteration:
                    alive[gi] = False
    P.end_stage()


def stage_BCE(P, l, d, g, last):
    P.begin_stage("BCE%d" % l)
    gens = [body_E(P, l, d, g, last, True), body_B(P, l, d, g, True), body_C(P, l, d, g, True)]
    ne = 8 * (16 if last else 17)
    totals = [ne + 1, 66 * 10 + 1, 4 * 2 * 33 + 1]
    done = [0, 0, 0]
    alive = [True, True, True]
    for gi, gen in enumerate(gens):
        next(gen)
        done[gi] += 1
    while any(alive):
        cand = [gi for gi in range(3) if alive[gi]]
        gi = min(cand, key=lambda i: done[i] / totals[i])
        try:
            next(gens[gi])
            done[gi] += 1
        except StopIteration:
            alive[gi] = False
    P.end_stage()


def stage_B2(P, l, d, g, last):
    P.begin_stage("B2_%d" % l)
    gn = P.sb("gn", [128, 64], F32)
    bcast_rows(P, lambda ps: P.op("dve", lambda e: e.tensor_copy(out=gn[:], in_=ps[:]), reads=[ps], writes=[gn]), [d.hg_norm[l:l + 1, :]], [64])
    o0r = Ring([P.sb("o0_%d" % i, [128, 512], F32) for i in range(2)])
    o1r = Ring([P.sb("o1_%d" % i, [128, 512], F32) for i in range(2)])
    gtr = Ring([P.sb("gt%d" % i, [128, 512], BF16) for i in range(2)])
    sgr = Ring([P.sb("sg%d" % i, [128, 512], F32) for i in range(2)])
    sqr = Ring([P.sb("sq%d" % i, [128, 512], F32) for i in range(2)])
    ssr = Ring([P.sb("ss%d" % i, [128, 8], F32) for i in range(2)])
    obr = Ring([P.sb("ob%d" % i, [128, 512], BF16) for i in range(2)])
    oTr = Ring([P.sb("oT%d" % i, [128, 4, 512], BF16) for i in range(2)])
    pst = Ring([P.ps("pst%d" % i, [128, 4, 128], BF16) for i in range(2)])
    for (t0, n) in BLOCKS:
        if last and t0 < CT:
            continue
        oT = oTr.next()
        for j in range(n // 128):
            tt = t0 + j * 128
            o0 = o0r.next(); o1 = o1r.next(); gt = gtr.next()
            P.dma("sp", o0[:], d.ODIR[0, tt:tt + 128, :], writes=[o0])
            P.dma("sp", o1[:], d.ODIR[1, tt:tt + 128, :], writes=[o1])
            P.dma("sp", gt[:], d.TV[tt:tt + 128, 512:1024], writes=[gt])
            sg = sgr.next(); sq = sqr.next(); ss = ssr.next(); ob = obr.next()
            P.op("act", lambda e: e.activation(out=sg[:], in_=gt[:], func=AF.Sigmoid), reads=[gt], writes=[sg])
            P.op("dve", lambda e: e.tensor_tensor(out=o0[:], in0=o0[:], in1=o1[:], op=ALU.add), reads=[o0, o1], writes=[o0])
            P.op("dve", lambda e: e.tensor_tensor(out=sq[:], in0=o0[:], in1=o0[:], op=ALU.mult), reads=[o0], writes=[sq])
            P.op("dve", lambda e: e.reduce_sum(out=ss[:], in_=sq[:].rearrange("p (h v) -> p h v", v=64), axis=AX.X), reads=[sq], writes=[ss])
            P.op("dve", lambda e: e.tensor_scalar(out=ss[:], in0=ss[:], scalar1=1.0 / 64.0, scalar2=EPS, op0=ALU.mult, op1=ALU.add), reads=[ss], writes=[ss])
            P.op("act", lambda e: e.activation(out=ss[:], in_=ss[:], func=AF.Ln), reads=[ss], writes=[ss])
            P.op("act", lambda e: e.activation(out=ss[:], in_=ss[:], func=AF.Exp, scale=-0.5), reads=[ss], writes=[ss])
            v3 = lambda t: t[:].rearrange("p (h v) -> p h v", v=64)
            P.op("dve", lambda e: e.tensor_tensor(out=v3(o0), in0=v3(o0), in1=ss[:].unsqueeze(2).to_broadcast([128, 8, 64]), op=ALU.mult), reads=[o0, ss], writes=[o0])
            P.op("dve", lambda e: e.tensor_tensor(out=v3(o0), in0=v3(o0), in1=gn[:].unsqueeze(1).to_broadcast([128, 8, 64]), op=ALU.mult), reads=[o0, gn], writes=[o0])
            P.op("dve", lambda e: e.tensor_tensor(out=ob[:], in0=o0[:], in1=sg[:], op=ALU.mult), reads=[o0, sg], writes=[ob])
            ps = pst.next()
            for a in range(4):
                P.op("pe", lambda e: e.transpose(ps[:, a, :], ob[:, a * 128:(a + 1) * 128], g.identb[:]), reads=[ob, g.identb], writes=[ps])
            P.op("act", lambda e: e.copy(out=oT[:, :, j * 128:(j + 1) * 128], in_=ps[:]), reads=[ps], writes=[oT])
        P.dma("pool", d.OHG[:, t0:t0 + n].rearrange("(a p) t -> p a t", p=128), oT[:, :, 0:n], reads=[oT])
    P.end_stage()


from concourse.bass_utils import run_bass_kernel_spmd


def build_program():
    nc = bass.Bass("TRN2", target_bir_lowering=False)
    d = declare(nc)
    P = Prog(nc)
    g = alloc_globals(P)
    stage_init(P, d, g)
    for l in range(2):
        last = (l == 1)
        stage_P(P, l, d, g)
        stage_A(P, l, d, g)
        stage_B(P, l, d, g)
        stage_B2(P, l, d, g, last)
        stage_C(P, l, d, g)
        stage_D(P, l, d, g)
        stage_E(P, l, d, g, last)
        stage_F(P, l, d, g, last)
        stage_G0(P, l, d, g)
        stage_G(P, l, d, g, last)
    P.finish()
    return nc


def kernel(**inputs):
    nc = build_program()
    consts = host_constants()
    shared = {k: np.ascontiguousarray(np.asarray(v, dtype=np.float32)) for k, v in inputs.items() if k not in ("x", "c", "ctx")}
    shared.update(consts)
    zeros = {k: np.zeros_like(v) for k, v in shared.items()}
    zx = np.zeros(tuple(inputs["x"].shape[1:]), np.float32)
    zc = np.zeros(tuple(inputs["c"].shape[1:]), np.float32)
    zctx = np.zeros(tuple(inputs["ctx"].shape[1:]), np.float32)
    active = {0: 0, 1: 1, 4: 2, 5: 3}
    in_maps = []
    for core in range(8):
        if core in active:
            b = active[core]
            m = dict(shared)
            m["x"] = np.ascontiguousarray(np.asarray(inputs["x"][b], dtype=np.float32))
            m["c"] = np.ascontiguousarray(np.asarray(inputs["c"][b], dtype=np.float32))
            m["ctx"] = np.ascontiguousarray(np.asarray(inputs["ctx"][b], dtype=np.float32))
        else:
            m = dict(zeros)
            m["x"], m["c"], m["ctx"] = zx, zc, zctx
        in_maps.append(m)
    res = run_bass_kernel_spmd(nc, in_maps, core_ids=list(range(8)))
    cores = [0, 1, 4, 5]
    out = np.stack([np.asarray(res.results[c]["out"], dtype=np.float32) for c in cores], axis=0)
    return out
```

```python
import numpy as np
from contextlib import ExitStack
import concourse.bass as bass
import concourse.mybir as mybir

F32 = mybir.dt.float32
BF16 = mybir.dt.bfloat16
AF = mybir.ActivationFunctionType
ALU = mybir.AluOpType
AX = mybir.AxisListType

NDSEM = 90
NO_SELF_WAIT = ("pe",)


class Buf:
    def __init__(self, t, name):
        self.t = t
        self.name = name
        self.lastw = None
        self.readers = {}
        self.dsem = None
        self.root = self

    def alias(self, ap, name=None):
        b = Buf(ap, name or self.name)
        b.root = self.root
        return b

    def __getitem__(self, k):
        return self.t[k]


class _Rec:
    def __init__(self):
        self.calls = []

    def __getattr__(self, name):
        def f(*a, **kw):
            self.calls.append((name, a, kw))
            return None
        return f


class Eng:
    def __init__(self, name, sem_id):
        self.name = name
        self.sem_id = sem_id
        self.count = 0
        self.ops = []
        self.waited = {}


class Prog:
    def __init__(self, nc):
        self.nc = nc
        self.es = ExitStack()
        self.sems = []
        self.semvals = []
        self.engs = {}
        for n in ("pe", "act", "dve", "pool", "sp"):
            sid = self._newsem("e_" + n)
            self.engs[n] = Eng(n, sid)
        self.dsems = [self._newsem("d%d" % i) for i in range(NDSEM)]
        self.dsem_next = 0
        self.stage_es = None
        self.stage_bufs = []
        self.nstage = 0

    def _newsem(self, name):
        h = self.es.enter_context(self.nc.semaphore(name))
        self.sems.append(h)
        self.semvals.append(0)
        return len(self.sems) - 1

    def begin_stage(self, name):
        self.stage_es = ExitStack()
        self.stage_bufs = []
        self.dsem_next = 0
        self.stage_name = name

    def _uid(self):
        self.uid = getattr(self, "uid", 0) + 1
        return self.uid

    def sb(self, name, shape, dtype):
        t = self.stage_es.enter_context(self.nc.sbuf_tensor("%s_%d_%d" % (name, self.nstage, self._uid()), list(shape), dtype))
        b = Buf(t, name)
        self.stage_bufs.append(b)
        return b

    def gsb(self, name, shape, dtype):
        t = self.es.enter_context(self.nc.sbuf_tensor("g_" + name, list(shape), dtype))
        return Buf(t, name)

    def ps(self, name, shape, dtype=F32):
        full = 512 if dtype == F32 else 1024
        t = self.stage_es.enter_context(self.nc.psum_tensor("%s_%d_%d" % (name, self.nstage, self._uid()), [128, full], dtype))
        shape = list(shape)
        n = 1
        for s_ in shape[1:]:
            n *= s_
        assert n <= full
        v = t[0:shape[0], 0:n]
        if len(shape) == 3:
            v = v.rearrange("p (a b) -> p a b", b=shape[2])
        elif len(shape) == 4:
            v = v.rearrange("p (a b c) -> p a b c", b=shape[2], c=shape[3])
        b = Buf(v, name)
        b.is_psum = True
        self.stage_bufs.append(b)
        return b

    def ps_bank(self, name):
        t = self.stage_es.enter_context(self.nc.psum_tensor("%s_%d_%d" % (name, self.nstage, self._uid()), [128, 512], F32))
        b = Buf(t[:, :], name)
        b.is_psum = True
        self.stage_bufs.append(b)
        return b

    def _dsem_for(self, b, qname="sp"):
        if b.dsem is None:
            b.dsem = {}
        if qname not in b.dsem:
            assert self.dsem_next < NDSEM, "out of dma sems"
            b.dsem[qname] = self.dsems[self.dsem_next]
            self.dsem_next += 1
        return b.dsem[qname]

    def _collect(self, eng, reads, writes):
        reads = [b.root for b in reads]
        writes = [b.root for b in writes]
        need = {}

        def add(sid, val):
            if val > need.get(sid, 0):
                need[sid] = val
        for b in reads:
            if b.lastw is not None:
                add(*b.lastw)
        for b in writes:
            if b.lastw is not None:
                add(*b.lastw)
            for sid, val in b.readers.items():
                add(sid, val)
        waits = []
        for sid, val in need.items():
            if sid == eng.sem_id and eng.name in NO_SELF_WAIT:
                continue
            if eng.waited.get(sid, 0) >= val:
                continue
            eng.waited[sid] = val
            waits.append((sid, val))
        return waits

    def _commit(self, token, reads, writes):
        reads = [b.root for b in reads]
        writes = [b.root for b in writes]
        for b in writes:
            b.lastw = token
            b.readers = {}
        for b in reads:
            if b in writes:
                continue
            sid, val = token
            if val > b.readers.get(sid, 0):
                b.readers[sid] = val

    def op(self, engname, fn, reads=(), writes=(), rg=None):
        reads = [b.root for b in reads]
        writes = [b.root for b in writes]
        writes = list(writes) + [b for b in reads if getattr(b, "is_psum", False) and b not in writes]
        eng = self.engs[engname]
        waits = self._collect(eng, reads, writes)
        if engname == "pe":
            rgs = frozenset(rg) if rg is not None else frozenset((0, 1, 2, 3))
            for b in writes:
                if getattr(b, "is_psum", False):
                    lp = getattr(b, "last_pe", None)
                    if lp is not None and not (lp[0] & rgs) and eng.waited.get(eng.sem_id, 0) < lp[1]:
                        eng.waited[eng.sem_id] = lp[1]
                        waits.append((eng.sem_id, lp[1]))
                    b.last_pe = (rgs, eng.count + 1)
        eng.count += 1
        self.semvals[eng.sem_id] = eng.count
        rec = _Rec()
        fn(rec)
        assert len(rec.calls) == 1
        eng.ops.append((waits, rec.calls[0], (eng.sem_id, 1)))
        self._commit((eng.sem_id, eng.count), reads, writes)

    def dma(self, qname, out, in_, reads=(), writes=(), owner=None, **kw):
        eng = self.engs[qname]
        waits = self._collect(eng, reads, writes)
        if owner is None:
            owner = writes[0] if writes else reads[0]
        sid = self._dsem_for(owner.root, qname)
        self.semvals[sid] += 16
        val = self.semvals[sid]

        eng.ops.append((waits, ("dma_start", (), dict(out=out, in_=in_, **kw)), (sid, 16)))
        self._commit((sid, val), reads, writes)

    def end_stage(self):
        tokens = []
        for e in self.engs.values():
            if e.count > 0:
                tokens.append((e.sem_id, e.count))
        for sid in self.dsems:
            if self.semvals[sid] > 0:
                tokens.append((sid, self.semvals[sid]))
        for e in self.engs.values():
            waits = []
            for sid, val in tokens:
                if sid == e.sem_id:
                    continue
                if e.waited.get(sid, 0) >= val:
                    continue
                e.waited[sid] = val
                waits.append((sid, val))
            if waits:
                e.ops.append((waits, None, None))
        nc = self.nc
        sems = self.sems
        with nc.Block() as block:
            def replay(e_handle, eng):
                for waits, fn, inc in eng.ops:
                    for sid, val in waits:
                        e_handle.wait_ge(sems[sid], val)
                    if fn is not None:
                        ins = getattr(e_handle, fn[0])(*fn[1], **fn[2])
                        ins.then_inc(sems[inc[0]], inc[1])
                eng.ops = []

            @block.tensor
            def _(t):
                replay(t, self.engs["pe"])

            @block.scalar
            def _(s):
                replay(s, self.engs["act"])

            @block.vector
            def _(v):
                replay(v, self.engs["dve"])

            @block.gpsimd
            def _(g):
                replay(g, self.engs["pool"])

            @block.sync
            def _(s):
                replay(s, self.engs["sp"])
        self.stage_es.close()
        self.stage_es = None
        self.nstage += 1

    def finish(self):
        self.es.close()


import numpy as np

NT = 8448
CT = 256
LT = 8192
EPS = 1e-6
BLOCKS = [(0, 256)] + [(256 + 512 * i, 512) for i in range(16)]
C_Q, C_FF, C_FB, C_I, C_G, C_DQ, C_DKV, C_KR, C_XX, C_YX, C_GHG, C_GMLA, C_GLRU = (
    0, 512, 1024, 1536, 2048, 2560, 2816, 2944, 2976, 3488, 4000, 5024, 6048)

INPUT_SHAPES = {
    'x': [8192, 1024], 'c': [1024], 'ctx': [256, 1024], 'c_ctx': [1024],
    'w_mod': [2, 1024, 6144], 'b_mod': [2, 6144], 'norm_mix': [2, 1024], 'norm_ffn': [2, 1024],
    'w_in': [2, 1024, 7072], 'hg_lb': [2, 2, 512], 'hg_norm': [2, 64],
    'mla_q_norm': [2, 256], 'mla_kv_norm': [2, 128], 'mla_w_uq': [2, 256, 768], 'mla_w_ukv': [2, 128, 1024],
    'mla_qk_gain_q': [2, 96], 'mla_qk_gain_k': [2, 96],
    'lru_conv_w': [2, 4, 512], 'lru_conv_b': [2, 512], 'lru_wa': [2, 2, 8, 64, 64], 'lru_ba': [2, 2, 512],
    'lru_wx': [2, 2, 8, 64, 64], 'lru_bx': [2, 2, 512], 'lru_lambda': [2, 2, 512],
    'w_br_hg': [2, 512, 1024], 'w_br_mla': [2, 512, 1024], 'w_br_lru': [2, 512, 1024], 'w_out': [2, 1024, 1024],
    'moe_w_rg': [2, 1024, 4], 'moe_b_rg': [2, 4], 'moe_w_re': [2, 1024, 16], 'moe_b_re': [2, 16],
    'moe_w1': [2, 16, 1024, 256], 'moe_w3': [2, 16, 1024, 256], 'moe_w2': [2, 16, 256, 1024],
    'k_ident': [128, 128], 'k_cos': [8192, 16], 'k_sin': [8192, 16], 'k_tri': [32, 2, 32],
    'k_sel': [16, 16, 128], 'k_selrow': [2, 2, 128],
}

SCRATCH = {
    'XCUR': ([NT, 1024], F32),
    'FQ': ([512, NT], BF16), 'FZ': ([1024, NT], F32), 'FXX': ([512, NT], F32), 'FYX': ([512, NT], BF16),
    'FG': ([3072, NT], BF16), 'TV': ([NT, 1024], BF16), 'TM': ([NT, 416], F32),
    'ODIR': ([2, NT, 512], F32), 'OHG': ([512, NT], BF16), 'OLRU': ([512, NT], BF16), 'OMLA': ([512, NT], BF16),
    'HF': ([512, NT], F32), 'W13B': ([16, 128, 8, 512], BF16), 'QT': ([8, 96, NT], BF16), 'KT': ([8, 96, NT], BF16), 'VX': ([NT, 8, 65], BF16),
}


class Dram:
    pass


def host_constants():
    k = {}
    k['k_ident'] = np.eye(128, dtype=np.float32)
    half = 16
    rows = 8192 // 64
    pos_row = np.repeat(np.arange(rows, dtype=np.float32), 64)
    pos_col = np.tile(np.arange(64, dtype=np.float32), rows)
    inv = (np.float32(10000.0) ** (-np.arange(0, half, 2, dtype=np.float32) / np.float32(half))).astype(np.float32)
    ang = np.concatenate([pos_row[:, None] * inv, pos_col[:, None] * inv], axis=-1).astype(np.float32)
    k['k_cos'] = np.cos(ang).astype(np.float32)
    k['k_sin'] = np.sin(ang).astype(np.float32)
    s = np.arange(32)[:, None]
    t = np.arange(32)[None, :]
    tri = np.zeros((32, 2, 32), np.float32)
    tri[:, 0, :] = (t >= s)
    tri[:, 1, :] = (t <= s)
    k['k_tri'] = tri
    sel = np.zeros((16, 16, 128), np.float32)
    for e in range(16):
        sel[e, e, :] = 1.0
    k['k_sel'] = sel
    sr = np.zeros((2, 2, 128), np.float32)
    sr[0, 0, :] = 1.0
    sr[1, 1, :] = 1.0
    k['k_selrow'] = sr
    return k


def declare(nc, debug=()):
    d = Dram()
    for name, shape in INPUT_SHAPES.items():
        setattr(d, name, nc.dram_tensor(name, list(shape), F32, kind="ExternalInput").ap())
    d.out = nc.dram_tensor("out", [8192, 1024], F32, kind="ExternalOutput").ap()
    for name, (shape, dt) in SCRATCH.items():
        kind = "ExternalOutput" if name in debug else "Internal"
        setattr(d, name, nc.dram_tensor(name, list(shape), dt, kind=kind).ap())
    return d


class Ring:
    def __init__(self, bufs):
        self.bufs = bufs
        self.i = 0

    def next(self):
        b = self.bufs[self.i % len(self.bufs)]
        self.i += 1
        return b


class G:
    pass


def alloc_globals(P):
    g = G()
    g.identf = P.gsb("identf", [128, 128], F32)
    g.identb = P.gsb("identb", [128, 128], BF16)
    g.gm = P.gsb("gm", [128, 2, 8, 2], F32)
    g.shf = P.gsb("shf", [128, 2, 8, 2], F32)
    g.gbc = P.gsb("gbc", [128, 2, 2, 1024], F32)
    return g


def bcast_rows(P, dst_fn, row_aps, widths, ps=None):
    tot = sum(widths)
    row = P.sb("bc_row", [1, tot], F32)
    ones = P.sb("bc_ones", [1, 128], F32)
    if ps is None:
        ps = P.ps("bc_ps", [128, tot])
    P.op("dve", lambda e: e.memset(ones[:], 1.0), writes=[ones])
    o = 0
    for ap, w in zip(row_aps, widths):
        P.dma("sp", row[0:1, o:o + w], ap, writes=[row])
        o += w
    P.op("pe", lambda e: e.matmul(ps[:, 0:tot], lhsT=ones[0:1, :], rhs=row[0:1, :], start=True, stop=True), reads=[ones, row], writes=[ps])
    dst_fn(ps)


def stage_init(P, d, g):
    P.begin_stage("init")
    misc = P.sb("misc", [1, 2], F32)
    P.dma("sp", g.identf[:], d.k_ident[:, :], writes=[g.identf])
    P.op("dve", lambda e: e.tensor_copy(out=g.identb[:], in_=g.identf[:]), reads=[g.identf], writes=[g.identb])
    P.dma("sp", d.XCUR[0:256, :], d.ctx[:, :], owner=misc)
    for i in range(4):
        P.dma("pool" if i % 2 else "sp", d.XCUR[256 + i * 2048:256 + (i + 1) * 2048, :], d.x[i * 2048:(i + 1) * 2048, :], owner=misc)
    P.end_stage()


def stage_P(P, l, d, g):
    P.begin_stage("P%d" % l)
    sc = P.sb("sc", [128, 8, 2], F32)
    P.dma("sp", sc[:, :, 0], d.c.rearrange("(k p) -> p k", p=128), writes=[sc], allow_slow_non_contiguous=True)
    P.dma("sp", sc[:, :, 1], d.c_ctx.rearrange("(k p) -> p k", p=128), writes=[sc], allow_slow_non_contiguous=True)
    P.op("act", lambda e: e.activation(out=sc[:], in_=sc[:], func=AF.Silu), reads=[sc], writes=[sc])
    bm = P.sb("bm", [2, 6144], F32)
    P.dma("sp", bm[0:1, :], d.b_mod[l:l + 1, :], writes=[bm])
    P.dma("sp", bm[1:2, :], d.b_mod[l:l + 1, :], writes=[bm])
    selrow = P.sb("selrow", [2, 2, 128], F32)
    P.dma("sp", selrow[:], d.k_selrow[:, :, :], writes=[selrow])
    nm = P.sb("nm", [128, 2, 8], F32)
    P.dma("sp", nm[:, 0, :], d.norm_mix[l].rearrange("(k p) -> p k", p=128), writes=[nm], allow_slow_non_contiguous=True)
    P.dma("sp", nm[:, 1, :], d.norm_ffn[l].rearrange("(k p) -> p k", p=128), writes=[nm], allow_slow_non_contiguous=True)
    modrow = P.sb("modrow", [2, 6144], F32)
    wmr = Ring([P.sb("wm%d" % i, [128, 3072], F32) for i in range(2)])
    psr = [P.ps("psr%d" % i, [2, 512]) for i in range(6)]
    for half in range(2):
        for k in range(8):
            wm = wmr.next()
            P.dma("sp" if k % 2 else "pool", wm[:], d.w_mod[l, k * 128:(k + 1) * 128, half * 3072:(half + 1) * 3072], writes=[wm])
            for j in range(6):
                P.op("pe", lambda e, j=j, k=k, wm=wm: e.matmul(psr[j][:], lhsT=sc[:, k, :], rhs=wm[:, j * 512:(j + 1) * 512],
                                                             start=(k == 0), stop=(k == 7)), reads=[sc, wm], writes=[psr[j]])
        for j in range(6):
            c0 = half * 3072 + j * 512
            P.op("dve", lambda e, j=j, c0=c0: e.tensor_tensor(out=modrow[:, c0:c0 + 512], in0=psr[j][:], in1=bm[:, c0:c0 + 512], op=ALU.add),
                 reads=[psr[j], bm], writes=[modrow])
    pT = P.ps("pT", [128, 32, 2])
    cols = [0, 1024, 3072, 4096]
    for gi, cbase in enumerate(cols):
        for k in range(8):
            P.op("pe", lambda e, gi=gi, k=k, cbase=cbase: e.transpose(pT[:, gi * 8 + k, :], modrow[0:2, cbase + k * 128:cbase + (k + 1) * 128], g.identf[0:2, 0:2]),
                 reads=[modrow, g.identf], writes=[pT])
    modfm = P.sb("modfm", [128, 32, 2], F32)
    P.op("dve", lambda e: e.tensor_copy(out=modfm[:], in_=pT[:]), reads=[pT], writes=[modfm])
    for ni in range(2):
        shg, scg = (0, 1) if ni == 0 else (2, 3)
        P.op("dve", lambda e, ni=ni, shg=shg: e.tensor_copy(out=g.shf[:, ni, :, :], in_=modfm[:, shg * 8:(shg + 1) * 8, :]), reads=[modfm], writes=[g.shf])
        P.op("dve", lambda e, ni=ni, scg=scg: e.tensor_scalar(out=g.gm[:, ni, :, :], in0=modfm[:, scg * 8:(scg + 1) * 8, :], scalar1=1.0, scalar2=None, op0=ALU.add),
             reads=[modfm], writes=[g.gm])
        P.op("dve", lambda e, ni=ni: e.tensor_tensor(out=g.gm[:, ni, :, :], in0=g.gm[:, ni, :, :], in1=nm[:, ni, :].unsqueeze(2).to_broadcast([128, 8, 2]), op=ALU.mult),
             reads=[g.gm, nm], writes=[g.gm])
    psb = Ring([P.ps("psb%d" % i, [128, 512]) for i in range(1)])
    for gi, cbase in enumerate([2048, 5120]):
        for cond in range(2):
            for h in range(2):
                pb = psb.next()
                P.op("pe", lambda e, pb=pb, cond=cond, cbase=cbase, h=h: e.matmul(pb[:], lhsT=selrow[0:2, cond, :], rhs=modrow[0:2, cbase + h * 512:cbase + (h + 1) * 512], start=True, stop=True),
                     reads=[selrow, modrow], writes=[pb])
                P.op("act", lambda e, pb=pb, gi=gi, cond=cond, h=h: e.copy(out=g.gbc[:, gi, cond, h * 512:(h + 1) * 512], in_=pb[:]), reads=[pb], writes=[g.gbc])
    P.end_stage()


def norm_transpose(P, g, xt, ss, rs, xs, pst, hT, j, ni, cond):
    P.op("pool", lambda e: e.memset(ss[:], 0.0), writes=[ss])
    P.op("act", lambda e: e.activation(out=xs[:], in_=xt[:], func=AF.Square, accum_out=ss[:]), reads=[xt, ss], writes=[xs, ss])
    P.op("dve", lambda e: e.tensor_scalar(out=rs[:], in0=ss[:], scalar1=1.0 / 1024.0, scalar2=EPS, op0=ALU.mult, op1=ALU.add), reads=[ss], writes=[rs])
    P.op("act", lambda e: e.activation(out=rs[:], in_=rs[:], func=AF.Ln), reads=[rs], writes=[rs])
    P.op("act", lambda e: e.activation(out=rs[:], in_=rs[:], func=AF.Exp, scale=-0.5), reads=[rs], writes=[rs])
    P.op("dve", lambda e: e.tensor_scalar(out=xs[:], in0=xt[:], scalar1=rs[:, 0:1], scalar2=None, op0=ALU.mult), reads=[xt, rs], writes=[xs])
    for k in range(8):
        P.op("pe", lambda e, k=k: e.transpose(pst[:, k, :], xs[:, k * 128:(k + 1) * 128], g.identb[:]), reads=[xs, g.identb], writes=[pst])
    for k in range(8):
        P.op("act", lambda e, k=k: e.activation(out=hT[:, k, j * 128:(j + 1) * 128], in_=pst[:, k, :], func=AF.Identity,
                                                scale=g.gm[:, ni, k, cond:cond + 1], bias=g.shf[:, ni, k, cond:cond + 1]),
             reads=[pst, g.gm, g.shf], writes=[hT])


def stage_A(P, l, d, g):
    P.begin_stage("A%d" % l)
    W = P.sb("W", [128, 8, 7072], BF16)
    for k in range(8):
        P.dma("pool", W[:, k, :], d.w_in[l, k * 128:(k + 1) * 128, :], writes=[W])
    xr = Ring([P.sb("xt%d" % i, [128, 1024], F32) for i in range(2)])
    xsr = Ring([P.sb("xs%d" % i, [128, 1024], BF16) for i in range(2)])
    ssr = Ring([P.sb("ss%d" % i, [128, 1], F32) for i in range(2)])
    rsr = Ring([P.sb("rs%d" % i, [128, 1], F32) for i in range(2)])
    hr = Ring([P.sb("hT%d" % i, [128, 8, 512], BF16) for i in range(2)])
    pstr = Ring([P.ps("pst%d" % i, [128, 8, 128], BF16) for i in range(2)])
    psf = Ring([P.ps("psf%d" % i, [128, 512]) for i in range(3)])
    pstm = Ring([P.ps("pstm%d" % i, [128, 512]) for i in range(2)])
    st32 = Ring([P.sb("st32_%d" % i, [128, 512], F32) for i in range(3)])
    st16 = Ring([P.sb("st16_%d" % i, [128, 512], BF16) for i in range(3)])
    sttv = Ring([P.sb("sttv%d" % i, [128, 1024], BF16) for i in range(2)])
    sttm = Ring([P.sb("sttm%d" % i, [128, 416], F32) for i in range(2)])
    fm_groups = [(d.FQ, 0, C_Q, 512, False), (d.FZ, 0, C_FF, 1024, True), (d.FXX, 0, C_XX, 512, True),
                 (d.FYX, 0, C_YX, 512, False), (d.FG, 0, C_GHG, 3072, False)]
    ev = 0

    def norm_steps(bi):
        t0, n = BLOCKS[bi]
        cond = 1 if t0 < CT else 0
        hT = hr.next()

        def mk(j):
            def f():
                xt = xr.next()
                P.dma("sp", xt[:], d.XCUR[t0 + j * 128:t0 + (j + 1) * 128, :], writes=[xt])
                norm_transpose(P, g, xt, ssr.next(), rsr.next(), xsr.next(), pstr.next(), hT, j, 0, cond)
            return f
        return hT, [mk(j) for j in range(n // 128)]

    hT, st0 = norm_steps(0)
    for f_ in st0:
        f_()
    for bi, (t0, n) in enumerate(BLOCKS):
        cond = 1 if t0 < CT else 0
        if bi + 1 < len(BLOCKS):
            hT_next, nsteps = norm_steps(bi + 1)
        else:
            hT_next, nsteps = None, []
        cnt = 0
        for (dst, r0, c0, nc_, is32) in fm_groups:
            for cc in range(nc_ // 128):
                cnt += 1
                if cnt % 8 == 0 and nsteps:
                    nsteps.pop(0)()
                ps = psf.next()
                for k in range(8):
                    P.op("pe", lambda e, ps=ps, k=k, c0=c0, cc=cc, hT=hT, n=n: e.matmul(ps[:, 0:n], lhsT=W[:, k, c0 + cc * 128:c0 + (cc + 1) * 128], rhs=hT[:, k, 0:n],
                                                                                      start=(k == 0), stop=(k == 7)), reads=[W, hT], writes=[ps])
                stg = st32.next() if is32 else st16.next()
                if ev % 2 == 0:
                    P.op("act", lambda e, ps=ps, stg=stg, n=n: e.copy(out=stg[:, 0:n], in_=ps[:, 0:n]), reads=[ps], writes=[stg])
                else:
                    P.op("dve", lambda e, ps=ps, stg=stg, n=n: e.tensor_copy(out=stg[:, 0:n], in_=ps[:, 0:n]), reads=[ps], writes=[stg])
                ev += 1
                P.dma("pool" if ev % 2 else "sp", dst[r0 + cc * 128:r0 + (cc + 1) * 128, t0:t0 + n], stg[:, 0:n], reads=[stg])
        for j in range(n // 128):
            tv = sttv.next()
            tm = sttm.next()
            for (c0, nc_, dstb, o0) in [(C_I, 512, tv, 0), (C_G, 512, tv, 512), (C_DQ, 416, tm, 0)]:
                ps = pstm.next()
                for k in range(8):
                    P.op("pe", lambda e, ps=ps, k=k, c0=c0, nc_=nc_, hT=hT, j=j: e.matmul(ps[:, 0:nc_], lhsT=hT[:, k, j * 128:(j + 1) * 128], rhs=W[:, k, c0:c0 + nc_],
                                                                                       start=(k == 0), stop=(k == 7)), reads=[W, hT], writes=[ps])
                if ev % 2 == 0:
                    P.op("act", lambda e, ps=ps, dstb=dstb, o0=o0, nc_=nc_: e.copy(out=dstb[:, o0:o0 + nc_], in_=ps[:, 0:nc_]), reads=[ps], writes=[dstb])
                else:
                    P.op("dve", lambda e, ps=ps, dstb=dstb, o0=o0, nc_=nc_: e.tensor_copy(out=dstb[:, o0:o0 + nc_], in_=ps[:, 0:nc_]), reads=[ps], writes=[dstb])
                ev += 1
            tt = t0 + j * 128
            P.dma("sp", d.TV[tt:tt + 128, :], tv[:], reads=[tv])
            P.dma("pool", d.TM[tt:tt + 128, :], tm[:], reads=[tm])
        for f_ in nsteps:
            f_()
        hT = hT_next
    P.end_stage()


def body_C(P, l, d, g, merged=False, dbgsel=None):
    NB = 256 if merged else 1024
    convw = P.sb("convw", [128, 4, 4], F32)
    convb = P.sb("convb", [128, 4], F32)
    bab = P.sb("bab", [128, 4, 2, 2], F32)
    clru = P.sb("clru", [128, 4, 2], F32)
    clru2 = P.sb("clru2", [128, 4, 2], F32)
    for j in range(4):
        P.dma("sp", convw[:, :, j], d.lru_conv_w[l, j].rearrange("(t p) -> p t", p=128), writes=[convw], allow_slow_non_contiguous=True)
    P.dma("sp", convb[:], d.lru_conv_b[l].rearrange("(t p) -> p t", p=128), writes=[convb], allow_slow_non_contiguous=True)
    for r_ in range(2):
        P.dma("sp", bab[:, :, 0, r_], d.lru_ba[l, r_].rearrange("(t p) -> p t", p=128), writes=[bab], allow_slow_non_contiguous=True)
        P.dma("sp", bab[:, :, 1, r_], d.lru_bx[l, r_].rearrange("(t p) -> p t", p=128), writes=[bab], allow_slow_non_contiguous=True)
        P.dma("sp", clru[:, :, r_], d.lru_lambda[l, r_].rearrange("(t p) -> p t", p=128), writes=[clru], allow_slow_non_contiguous=True)
    P.op("act", lambda e: e.activation(out=clru[:], in_=clru[:], func=AF.Exp, scale=-1.0), reads=[clru], writes=[clru])
    P.op("act", lambda e: e.activation(out=clru[:], in_=clru[:], func=AF.Ln, bias=1.0), reads=[clru], writes=[clru])
    P.op("dve", lambda e: e.tensor_scalar(out=clru2[:], in0=clru[:], scalar1=-16.0, scalar2=None, op0=ALU.mult), reads=[clru], writes=[clru2])
    P.op("dve", lambda e: e.tensor_scalar(out=clru[:], in0=clru[:], scalar1=-8.0, scalar2=None, op0=ALU.mult), reads=[clru], writes=[clru])
    wbd = P.sb("wbd", [128, 16, 128], F32)
    P.op("pool", lambda e: e.memset(wbd[:], 0.0), writes=[wbd])
    for dr in range(2):
        for gi, wsrc in enumerate([d.lru_wa, d.lru_wx]):
            for ti in range(4):
                for hf in range(2):
                    idx = (dr * 2 + gi) * 4 + ti
                    P.dma("sp", wbd[hf * 64:(hf + 1) * 64, idx, hf * 64:(hf + 1) * 64], wsrc[l, dr, ti * 2 + hf, :, :], writes=[wbd])
    xpr = Ring([P.sb("xp%d" % i, [128, NB + 3], F32) for i in range(2)])
    ur = Ring([P.sb("u%d" % i, [128, NB], F32) for i in range(2)])
    rr = Ring([P.sb("r%d" % i, [128, NB], F32) for i in range(2)])
    igr = Ring([P.sb("ig%d" % i, [128, NB], F32) for i in range(2)])
    ar = Ring([P.sb("a%d" % i, [128, NB], F32) for i in range(2)])
    a2r = Ring([P.sb("a2%d" % i, [128, NB], F32) for i in range(2)])
    hr = Ring([P.sb("h%d" % i, [128, NB], F32) for i in range(2)])
    hfr = Ring([P.sb("hf%d" % i, [128, NB], F32) for i in range(2)])
    yr = Ring([P.sb("y%d" % i, [128, NB], BF16) for i in range(2)])
    gyr = Ring([P.sb("gy%d" % i, [128, NB], F32) for i in range(2)])
    obr = Ring([P.sb("ob%d" % i, [128, NB], BF16) for i in range(2)])
    psg = Ring([P.ps("psg%d" % i, [128, 512]) for i in range(2 if merged else 6)])
    segs = [(0, CT), (CT, NT)]
    nlb = LT // NB
    blocks_f = [(0, 256, 0)] + [(256 + NB * i, NB, 1) for i in range(nlb)]
    blocks_b = [(0, 256, 0)] + [(256 + NB * i, NB, 1) for i in reversed(range(nlb))]
    yield
    items = []
    for ti in range(4):
        for dr in range(2):
            for bi, (t0, n, sg) in enumerate(blocks_f if dr == 0 else blocks_b):
                items.append((ti, dr, bi, t0, n, sg))

    def phase1(it):
        ti, dr, bi, t0, n, sg = it
        rows = slice(ti * 128, (ti + 1) * 128)
        s0, s1 = segs[sg]
        xp = xpr.next()
        lo = max(t0 - 2, s0)
        hi = min(t0 + n + 1, s1)
        P.op("pool", lambda e: e.memset(xp[:], 0.0), writes=[xp])
        P.dma("sp", xp[:, lo - (t0 - 2):hi - (t0 - 2)], d.FXX[rows, lo:hi], writes=[xp])
        u = ur.next()
        P.op("dve", lambda e: e.tensor_scalar(out=u[:, 0:n], in0=xp[:, 0:n], scalar1=convw[:, ti, 0:1], scalar2=convb[:, ti:ti + 1], op0=ALU.mult, op1=ALU.add),
             reads=[xp, convw, convb], writes=[u])
        for j in range(1, 4):
            P.op("dve", lambda e: e.scalar_tensor_tensor(out=u[:, 0:n], in0=xp[:, j:j + n], scalar=convw[:, ti, j:j + 1], in1=u[:, 0:n], op0=ALU.mult, op1=ALU.add),
                 reads=[xp, convw, u], writes=[u])
        r = rr.next()
        ig = igr.next()
        for gi, dst in enumerate([r, ig]):
            idx = (dr * 2 + gi) * 4 + ti
            for c0 in range(0, n, 512):
                cn = min(512, n - c0)
                ps = psg.next()
                P.op("pe", lambda e: e.matmul(ps[:, 0:cn], lhsT=wbd[:, idx, :], rhs=u[:, c0:c0 + cn], start=True, stop=True), reads=[wbd, u], writes=[ps])
                P.op("act", lambda e: e.activation(out=dst[:, c0:c0 + cn], in_=ps[:, 0:cn], func=AF.Sigmoid, bias=bab[:, ti, gi, dr:dr + 1]), reads=[ps, bab], writes=[dst])
        a = ar.next()
        a2 = a2r.next()
        P.op("act", lambda e: e.activation(out=a[:, 0:n], in_=r[:, 0:n], func=AF.Exp, scale=clru[:, ti, dr:dr + 1]), reads=[r, clru], writes=[a])
        P.op("act", lambda e: e.activation(out=a2[:, 0:n], in_=r[:, 0:n], func=AF.Exp, scale=clru2[:, ti, dr:dr + 1]), reads=[r, clru2], writes=[a2])
        P.op("pool", lambda e: e.tensor_scalar(out=a2[:, 0:n], in0=a2[:, 0:n], scalar1=-1.0, scalar2=1.0, op0=ALU.mult, op1=ALU.add), reads=[a2], writes=[a2])
        P.op("act", lambda e: e.activation(out=a2[:, 0:n], in_=a2[:, 0:n], func=AF.Ln), reads=[a2], writes=[a2])
        P.op("act", lambda e: e.activation(out=a2[:, 0:n], in_=a2[:, 0:n], func=AF.Exp, scale=0.5), reads=[a2], writes=[a2])
        P.op("pool", lambda e: e.tensor_tensor(out=ig[:, 0:n], in0=ig[:, 0:n], in1=u[:, 0:n], op=ALU.mult), reads=[ig, u], writes=[ig])
        P.op("pool", lambda e: e.tensor_tensor(out=ig[:, 0:n], in0=ig[:, 0:n], in1=a2[:, 0:n], op=ALU.mult), reads=[ig, a2], writes=[ig])
        ctx_ = dict(a=a, ig=ig)
        if dr == 1:
            hf = hfr.next()
            y = yr.next()
            P.dma("sp", hf[:, 0:n], d.HF[rows, t0:t0 + n], reads=[hf_tok[(ti, t0)]], writes=[hf])
            P.dma("sp", y[:, 0:n], d.FYX[rows, t0:t0 + n], writes=[y])
            gy = gyr.next()
            P.op("act", lambda e: e.activation(out=gy[:, 0:n], in_=y[:, 0:n], func=AF.Gelu_apprx_tanh), reads=[y], writes=[gy])
            ctx_.update(hf=hf, gy=gy)
        return ctx_

    prev = {}
    hf_tok = {}

    def phase2(it, cx):
        ti, dr, bi, t0, n, sg = it
        rows = slice(ti * 128, (ti + 1) * 128)
        a, ig = cx["a"], cx["ig"]
        prev_h = prev.get((ti, dr)) if bi > 0 else None
        h = hr.next()
        rd = [a, ig] + ([prev_h[0]] if prev_h else [])
        if dr == 0:
            init = 0.0 if prev_h is None else prev_h[0][:, prev_h[1] - 1:prev_h[1]]
            P.op("dve", lambda e: e.tensor_tensor_scan(out=h[:, 0:n], data0=a[:, 0:n], data1=ig[:, 0:n], initial=init, op0=ALU.mult, op1=ALU.add), reads=rd, writes=[h])
            hf_tok[(ti, t0)] = Buf(None, "hftok")
            P.dma("pool", d.HF[rows, t0:t0 + n], h[:, 0:n], reads=[h], writes=[hf_tok[(ti, t0)]], owner=h)
        else:
            init = 0.0 if prev_h is None else prev_h[0][:, 0:1]
            P.op("dve", lambda e: e.tensor_tensor_scan(out=h[:, 0:n][:, ::-1], data0=a[:, 0:n][:, ::-1], data1=ig[:, 0:n][:, ::-1], initial=init, op0=ALU.mult, op1=ALU.add),
                 reads=rd, writes=[h])
            hf, gy = cx["hf"], cx["gy"]
            P.op("dve", lambda e: e.tensor_tensor(out=hf[:, 0:n], in0=hf[:, 0:n], in1=h[:, 0:n], op=ALU.add), reads=[hf, h], writes=[hf])
            ob = obr.next()
            P.op("dve", lambda e: e.tensor_tensor(out=ob[:, 0:n], in0=hf[:, 0:n], in1=gy[:, 0:n], op=ALU.mult), reads=[hf, gy], writes=[ob])
            P.dma("pool", d.OLRU[rows, t0:t0 + n], ob[:, 0:n], reads=[ob])
        prev[(ti, dr)] = (h, n)

    cxs = {0: phase1(items[0])}
    for k in range(len(items)):
        if k + 1 < len(items):
            cxs[k + 1] = phase1(items[k + 1])
        phase2(items[k], cxs.pop(k))
        yield


def stage_C(P, l, d, g, dbgsel=None):
    P.begin_stage("C%d" % l)
    for _ in body_C(P, l, d, g, False, dbgsel):
        pass
    P.end_stage()


def stage_F(P, l, d, g, last):
    P.begin_stage("F%d" % l)
    wbr = P.sb("wbr", [128, 3, 4, 1024], BF16)
    wout = P.sb("wout", [128, 8, 1024], BF16)
    for bi, wsrc in enumerate([d.w_br_hg, d.w_br_mla, d.w_br_lru]):
        P.dma("pool", wbr[:, bi, :, :], wsrc[l].rearrange("(k p) n -> p k n", p=128), writes=[wbr])
    P.dma("pool", wout[:], d.w_out[l].rearrange("(k p) n -> p k n", p=128), writes=[wout])
    obr = Ring([P.sb("ob%d" % i, [128, 3, 4, 512], BF16) for i in range(2)])
    gtr = Ring([P.sb("gt%d" % i, [128, 24, 512], BF16) for i in range(2)])
    sgr = Ring([P.sb("sg%d" % i, [128, 512], F32) for i in range(3)])
    accr = Ring([P.sb("acc%d" % i, [128, 512], F32) for i in range(2)])
    tmr = Ring([P.sb("tm%d" % i, [128, 512], F32) for i in range(3)])
    ytr = Ring([P.sb("yT%d" % i, [128, 8, 512], BF16) for i in range(2)])
    xr = Ring([P.sb("xt%d" % i, [128, 1024], F32) for i in range(3)])
    psm = Ring([P.ps("psm%d" % i, [128, 512]) for i in range(4)])
    pso = Ring([P.ps("pso%d" % i, [128, 512]) for i in range(3)])
    srcs = [d.OHG, d.OMLA, d.OLRU]
    for (t0, n) in BLOCKS:
        cond = 1 if t0 < CT else 0
        if last and cond == 1:
            continue
        ob = obr.next()
        gt = gtr.next()
        for bi in range(3):
            P.dma("sp", ob[:, bi, :, 0:n], srcs[bi][:, t0:t0 + n].rearrange("(k p) t -> p k t", p=128), writes=[ob])
        P.dma("sp", gt[:, :, 0:n], d.FG[:, t0:t0 + n].rearrange("(k p) t -> p k t", p=128), writes=[gt])
        yT = ytr.next()
        for nn in range(8):
            acc = accr.next()
            for bi in range(3):
                ps = psm.next()
                for k in range(4):
                    P.op("pe", lambda e, ps=ps, bi=bi, k=k, nn=nn, ob=ob, n=n: e.matmul(ps[:, 0:n], lhsT=wbr[:, bi, k, nn * 128:(nn + 1) * 128], rhs=ob[:, bi, k, 0:n], start=(k == 0), stop=(k == 3)),
                         reads=[wbr, ob], writes=[ps])
                sg = sgr.next()
                P.op("act", lambda e, sg=sg, gt=gt, bi=bi, nn=nn, n=n: e.activation(out=sg[:, 0:n], in_=gt[:, bi * 8 + nn, 0:n], func=AF.Sigmoid), reads=[gt], writes=[sg])
                if bi == 0:
                    P.op("dve", lambda e, acc=acc, ps=ps, sg=sg, n=n: e.tensor_tensor(out=acc[:, 0:n], in0=ps[:, 0:n], in1=sg[:, 0:n], op=ALU.mult), reads=[ps, sg], writes=[acc])
                else:
                    tm = tmr.next()
                    P.op("dve", lambda e, tm=tm, ps=ps, sg=sg, n=n: e.tensor_tensor(out=tm[:, 0:n], in0=ps[:, 0:n], in1=sg[:, 0:n], op=ALU.mult), reads=[ps, sg], writes=[tm])
                    if bi == 1:
                        P.op("pool", lambda e, acc=acc, tm=tm, n=n: e.tensor_tensor(out=acc[:, 0:n], in0=acc[:, 0:n], in1=tm[:, 0:n], op=ALU.add), reads=[acc, tm], writes=[acc])
                    else:
                        P.op("pool", lambda e, acc=acc, tm=tm, n=n, yT=yT, nn=nn: e.tensor_tensor(out=yT[:, nn, 0:n], in0=acc[:, 0:n], in1=tm[:, 0:n], op=ALU.add), reads=[acc, tm], writes=[yT])
        for j in range(n // 128):
            tt = t0 + j * 128
            xt = xr.next()
            P.dma("sp", xt[:], d.XCUR[tt:tt + 128, :], writes=[xt])
            for hf in range(2):
                ps = pso.next()
                for k in range(8):
                    P.op("pe", lambda e, ps=ps, k=k, j=j, hf=hf, yT=yT: e.matmul(ps[:], lhsT=yT[:, k, j * 128:(j + 1) * 128], rhs=wout[:, k, hf * 512:(hf + 1) * 512], start=(k == 0), stop=(k == 7)),
                         reads=[yT, wout], writes=[ps])
                tm = tmr.next()
                P.op("dve", lambda e, tm=tm, ps=ps, hf=hf, cond=cond: e.tensor_tensor(out=tm[:], in0=ps[:], in1=g.gbc[:, 0, cond, hf * 512:(hf + 1) * 512], op=ALU.mult), reads=[ps, g.gbc], writes=[tm])
                P.op("pool", lambda e, xt=xt, tm=tm, hf=hf: e.tensor_tensor(out=xt[:, hf * 512:(hf + 1) * 512], in0=xt[:, hf * 512:(hf + 1) * 512], in1=tm[:], op=ALU.add), reads=[xt, tm], writes=[xt])
            P.dma("pool", d.XCUR[tt:tt + 128, :], xt[:], reads=[xt])
    P.end_stage()


def stage_G0(P, l, d, g):
    P.begin_stage("G0_%d" % l)
    wr = Ring([P.sb("wc%d" % i, [128, 8, 512], BF16) for i in range(3)])
    for e_ in range(16):
        w = wr.next()
        P.dma("pool", w[:, :, 0:256], d.moe_w1[l, e_].rearrange("(k p) n -> p k n", p=128), writes=[w])
        P.dma("pool", w[:, :, 256:512], d.moe_w3[l, e_].rearrange("(k p) n -> p k n", p=128), writes=[w])
        P.dma("sp", d.W13B[e_], w[:], reads=[w])
    P.end_stage()


def stage_G(P, l, d, g, last):
    P.begin_stage("G%d" % l)
    w2all = P.sb("w2all", [128, 32, 1024], BF16)
    for e_ in range(16):
        P.dma("pool", w2all[:, 2 * e_:2 * e_ + 2, :], d.moe_w2[l, e_].rearrange("(h p) n -> p h n", p=128), writes=[w2all])
    wrt = P.sb("wrt", [128, 8, 20], BF16)
    P.dma("pool", wrt[:, :, 0:4], d.moe_w_rg[l].rearrange("(k p) n -> p k n", p=128), writes=[wrt])
    P.dma("pool", wrt[:, :, 4:20], d.moe_w_re[l].rearrange("(k p) n -> p k n", p=128), writes=[wrt])
    brow = P.sb("brow", [128, 20], F32)
    psmisc = P.ps("psmisc", [128, 512])
    bcast_rows(P, lambda ps: P.op("dve", lambda e: e.tensor_copy(out=brow[:], in_=ps[:, 0:20]), reads=[ps], writes=[brow]),
               [d.moe_b_rg[l:l + 1, :], d.moe_b_re[l:l + 1, :]], [4, 16], ps=psmisc)
    sel = P.sb("sel", [16, 16, 128], F32)
    P.dma("sp", sel[:], d.k_sel[:, :, :], writes=[sel])
    w13r = Ring([P.sb("w13_%d" % i, [128, 8, 512], BF16) for i in range(2)])
    xts = [[P.sb("xt%d_%d" % (i, j), [128, 1024], F32) for j in range(4)] for i in range(2)]
    xsr = Ring([P.sb("xs%d" % i, [128, 1024], BF16) for i in range(1)])
    ssr = Ring([P.sb("ss%d" % i, [128, 1], F32) for i in range(2)])
    rsr = Ring([P.sb("rs%d" % i, [128, 1], F32) for i in range(2)])
    hr = Ring([P.sb("hT%d" % i, [128, 8, 512], BF16) for i in range(2)])
    hidr = Ring([P.sb("hid%d" % i, [128, 32, 512], BF16) for i in range(1)])
    sar = Ring([P.sb("sa%d" % i, [128, 512], F32) for i in range(2)])
    tmr = Ring([P.sb("tm%d" % i, [128, 512], F32) for i in range(2)])
    bcsr = Ring([P.sb("bcs%d" % i, [128, 512], F32) for i in range(2)])

    def small(name, shape):
        return P.sb(name, shape, F32)
    lgle = small("lgle", [128, 4, 20]); lgb = small("lgb", [128, 4, 4]); mg = small("mg", [128, 4]); goh = small("goh", [128, 4, 4])
    e0 = small("e0", [128, 4, 4]); s0 = small("s0", [128, 4]); pg = small("pg", [128, 4]); t4 = small("t4", [128, 4, 4])
    leb = small("leb", [128, 4, 16]); pen = small("pen", [128, 4, 16]); val = small("val", [128, 4, 16]); oh1 = small("oh1", [128, 4, 16])
    oh2 = small("oh2", [128, 4, 16]); m1 = small("m1", [128, 4]); t16 = small("t16", [128, 4, 16]); l1 = small("l1", [128, 4]); l2 = small("l2", [128, 4])
    w1p = small("w1p", [128, 4]); w2p = small("w2p", [128, 4]); comb = small("comb", [128, 4, 16])
    psa = Ring([P.ps("psa%d" % i, [128, 512]) for i in range(2)])
    psb = Ring([P.ps("psb%d" % i, [128, 512]) for i in range(2)])
    psbc = P.ps("psbc", [128, 512])
    pst = P.ps("pst", [128, 8, 128], BF16)
    pso = P.ps("pso", [128, 512])
    blocks = [(t0, n) for (t0, n) in BLOCKS if not (last and t0 < CT)]
    combTr = Ring([P.sb("combT%d" % i, [16, 512], F32) for i in range(2)])

    class Cx:
        pass

    def make_steps(bi):
        t0, n = blocks[bi]
        cx = Cx()
        cx.t0, cx.n, cx.cond, cx.nt = t0, n, (1 if t0 < CT else 0), n // 128
        cx.xt4 = xts[bi % 2]
        cx.hT = hr.next()
        cx.combT = combTr.next()
        cond, nt, xt4, hT, combT = cx.cond, cx.nt, cx.xt4, cx.hT, cx.combT
        steps = []

        def mk_norm(j):
            def f():
                xt = xt4[j]
                P.dma("sp", xt[:], d.XCUR[t0 + j * 128:t0 + (j + 1) * 128, :], writes=[xt])
                norm_transpose(P, g, xt, ssr.next(), rsr.next(), xsr.next(), pst, hT, j, 1, cond)
            return f
        for j in range(nt):
            steps.append(mk_norm(j))
        while len(steps) < 4:
            steps.append(lambda: None)

        def router_a():
            for j in range(nt):
                for k in range(8):
                    P.op("pe", lambda e, j=j, k=k, hT=hT: e.matmul(psmisc[:, j * 20:(j + 1) * 20], lhsT=hT[:, k, j * 128:(j + 1) * 128], rhs=wrt[:, k, :], start=(k == 0), stop=(k == 7)),
                         reads=[hT, wrt], writes=[psmisc])
            V = lambda t: t[:, 0:nt]
            P.op("dve", lambda e: e.tensor_copy(out=lgle[:, 0:nt, :], in_=psmisc[:, 0:nt * 20].rearrange("p (j c) -> p j c", c=20)), reads=[psmisc], writes=[lgle])
            lg = lgle[:, 0:nt, 0:4]
            le = lgle[:, 0:nt, 4:20]
            D_ = lambda fn, r, w: P.op("dve", fn, reads=r, writes=w)
            D_(lambda e: e.tensor_tensor(out=lgb[:, 0:nt, :], in0=lg, in1=brow[:, 0:4].unsqueeze(1).to_broadcast([128, nt, 4]), op=ALU.add), [lgle, brow], [lgb])
            D_(lambda e: e.reduce_max(out=mg[:, 0:nt], in_=lgb[:, 0:nt, :], axis=AX.X), [lgb], [mg])
            D_(lambda e: e.tensor_tensor(out=goh[:, 0:nt, :], in0=lgb[:, 0:nt, :], in1=mg[:, 0:nt].unsqueeze(2).to_broadcast([128, nt, 4]), op=ALU.is_ge), [lgb, mg], [goh])
            D_(lambda e: e.reduce_max(out=mg[:, 0:nt], in_=lg, axis=AX.X), [lgle], [mg])
            D_(lambda e: e.tensor_tensor(out=e0[:, 0:nt, :], in0=lg, in1=mg[:, 0:nt].unsqueeze(2).to_broadcast([128, nt, 4]), op=ALU.subtract), [lgle, mg], [e0])
            P.op("act", lambda e: e.activation(out=e0[:, 0:nt, :], in_=e0[:, 0:nt, :], func=AF.Exp), reads=[e0], writes=[e0])
            D_(lambda e: e.reduce_sum(out=s0[:, 0:nt], in_=e0[:, 0:nt, :], axis=AX.X), [e0], [s0])
            D_(lambda e: e.tensor_tensor(out=t4[:, 0:nt, :], in0=e0[:, 0:nt, :], in1=goh[:, 0:nt, :], op=ALU.mult), [e0, goh], [t4])
            D_(lambda e: e.reduce_sum(out=pg[:, 0:nt], in_=t4[:, 0:nt, :], axis=AX.X), [t4], [pg])
            D_(lambda e: e.reciprocal(out=s0[:, 0:nt], in_=s0[:, 0:nt]), [s0], [s0])
            D_(lambda e: e.tensor_tensor(out=pg[:, 0:nt], in0=pg[:, 0:nt], in1=s0[:, 0:nt], op=ALU.mult), [pg, s0], [pg])
            D_(lambda e: e.tensor_tensor(out=leb[:, 0:nt, :], in0=le, in1=brow[:, 4:20].unsqueeze(1).to_broadcast([128, nt, 16]), op=ALU.add), [lgle, brow], [leb])
            mask = goh[:, 0:nt, :].unsqueeze(3).to_broadcast([128, nt, 4, 4])
            r4 = lambda t: t[:, 0:nt, :].rearrange("p j (a b) -> p j a b", b=4)
            D_(lambda e: e.tensor_tensor(out=r4(val), in0=r4(leb), in1=mask, op=ALU.mult), [leb, goh], [val])
            D_(lambda e: e.tensor_scalar(out=r4(pen), in0=mask, scalar1=1e30, scalar2=-1e30, op0=ALU.mult, op1=ALU.add), [goh], [pen])
            D_(lambda e: e.tensor_tensor(out=val[:, 0:nt, :], in0=val[:, 0:nt, :], in1=pen[:, 0:nt, :], op=ALU.add), [val, pen], [val])
            D_(lambda e: e.reduce_max(out=m1[:, 0:nt], in_=val[:, 0:nt, :], axis=AX.X), [val], [m1])
            D_(lambda e: e.tensor_tensor(out=oh1[:, 0:nt, :], in0=val[:, 0:nt, :], in1=m1[:, 0:nt].unsqueeze(2).to_broadcast([128, nt, 16]), op=ALU.is_ge), [val, m1], [oh1])
            D_(lambda e: e.scalar_tensor_tensor(out=val[:, 0:nt, :], in0=oh1[:, 0:nt, :], scalar=-1e30, in1=val[:, 0:nt, :], op0=ALU.mult, op1=ALU.add), [oh1, val], [val])
            D_(lambda e: e.reduce_max(out=m1[:, 0:nt], in_=val[:, 0:nt, :], axis=AX.X), [val], [m1])
            D_(lambda e: e.tensor_tensor(out=oh2[:, 0:nt, :], in0=val[:, 0:nt, :], in1=m1[:, 0:nt].unsqueeze(2).to_broadcast([128, nt, 16]), op=ALU.is_ge), [val, m1], [oh2])
            D_(lambda e: e.tensor_tensor(out=t16[:, 0:nt, :], in0=le, in1=oh1[:, 0:nt, :], op=ALU.mult), [lgle, oh1], [t16])
            D_(lambda e: e.reduce_sum(out=l1[:, 0:nt], in_=t16[:, 0:nt, :], axis=AX.X), [t16], [l1])
            D_(lambda e: e.tensor_tensor(out=t16[:, 0:nt, :], in0=le, in1=oh2[:, 0:nt, :], op=ALU.mult), [lgle, oh2], [t16])
            D_(lambda e: e.reduce_sum(out=l2[:, 0:nt], in_=t16[:, 0:nt, :], axis=AX.X), [t16], [l2])
            D_(lambda e: e.tensor_tensor(out=l1[:, 0:nt], in0=l1[:, 0:nt], in1=l2[:, 0:nt], op=ALU.subtract), [l1, l2], [l1])
            P.op("act", lambda e: e.activation(out=l1[:, 0:nt], in_=l1[:, 0:nt], func=AF.Sigmoid), reads=[l1], writes=[l1])
            D_(lambda e: e.tensor_tensor(out=w1p[:, 0:nt], in0=l1[:, 0:nt], in1=pg[:, 0:nt], op=ALU.mult), [l1, pg], [w1p])
            D_(lambda e: e.tensor_tensor(out=w2p[:, 0:nt], in0=pg[:, 0:nt], in1=w1p[:, 0:nt], op=ALU.subtract), [pg, w1p], [w2p])
            D_(lambda e: e.tensor_tensor(out=comb[:, 0:nt, :], in0=oh1[:, 0:nt, :], in1=w1p[:, 0:nt].unsqueeze(2).to_broadcast([128, nt, 16]), op=ALU.mult), [oh1, w1p], [comb])
            D_(lambda e: e.tensor_tensor(out=t16[:, 0:nt, :], in0=oh2[:, 0:nt, :], in1=w2p[:, 0:nt].unsqueeze(2).to_broadcast([128, nt, 16]), op=ALU.mult), [oh2, w2p], [t16])
            D_(lambda e: e.tensor_tensor(out=comb[:, 0:nt, :], in0=comb[:, 0:nt, :], in1=t16[:, 0:nt, :], op=ALU.add), [comb, t16], [comb])

        def router_b():
            for j in range(nt):
                P.op("pe", lambda e, j=j: e.transpose(psmisc[0:16, j * 128:(j + 1) * 128], comb[:, j, :], g.identf[:]), reads=[comb, g.identf], writes=[psmisc])
            P.op("dve", lambda e: e.tensor_copy(out=combT[:, 0:n], in_=psmisc[0:16, 0:n]), reads=[psmisc], writes=[combT])

        steps.append(router_a)
        steps.append(router_b)
        return cx, steps

    def phase2(cx, nxt_steps):
        t0, n, cond, nt, xt4, hT, combT = cx.t0, cx.n, cx.cond, cx.nt, cx.xt4, cx.hT, cx.combT
        sched = {1: 0, 3: 1, 5: 2, 7: 3, 9: 4, 12: 5}
        hid = hidr.next()
        for e_ in range(16):
            if nxt_steps is not None and e_ in sched:
                nxt_steps[sched[e_]]()
            w13 = w13r.next()
            P.dma("sp", w13[:], d.W13B[e_], writes=[w13])
            P.op("pe", lambda e, e_=e_: e.matmul(psbc[:, 0:n], lhsT=sel[0:16, e_, :], rhs=combT[0:16, 0:n], start=True, stop=True), reads=[sel, combT], writes=[psbc])
            bcs = bcsr.next()
            P.op("act", lambda e, bcs=bcs: e.copy(out=bcs[:, 0:n], in_=psbc[:, 0:n]), reads=[psbc], writes=[bcs])
            for hh in range(2):
                pa = psa.next()
                pb = psb.next()
                for k in range(8):
                    P.op("pe", lambda e, pa=pa, k=k, hh=hh, w13=w13, hT=hT: e.matmul(pa[:, 0:n], lhsT=w13[:, k, hh * 128:(hh + 1) * 128], rhs=hT[:, k, 0:n], start=(k == 0), stop=(k == 7)),
                         reads=[w13, hT], writes=[pa])
                for k in range(8):
                    P.op("pe", lambda e, pb=pb, k=k, hh=hh, w13=w13, hT=hT: e.matmul(pb[:, 0:n], lhsT=w13[:, k, 256 + hh * 128:256 + (hh + 1) * 128], rhs=hT[:, k, 0:n], start=(k == 0), stop=(k == 7)),
                         reads=[w13, hT], writes=[pb])
                sa = sar.next()
                P.op("act", lambda e, sa=sa, pa=pa: e.activation(out=sa[:, 0:n], in_=pa[:, 0:n], func=AF.Silu), reads=[pa], writes=[sa])
                tm = tmr.next()
                P.op("dve", lambda e, tm=tm, sa=sa, pb=pb: e.tensor_tensor(out=tm[:, 0:n], in0=pb[:, 0:n], in1=sa[:, 0:n], op=ALU.mult), reads=[pb, sa], writes=[tm])
                P.op("pool", lambda e, tm=tm, bcs=bcs, e_=e_, hh=hh, hid=hid: e.tensor_tensor(out=hid[:, 2 * e_ + hh, 0:n], in0=tm[:, 0:n], in1=bcs[:, 0:n], op=ALU.mult), reads=[tm, bcs], writes=[hid])
        for j in range(nt):
            xt = xt4[j]
            tt = t0 + j * 128
            for hf in range(2):
                for kc in range(32):
                    P.op("pe", lambda e, kc=kc, j=j, hf=hf, hid=hid: e.matmul(pso[:], lhsT=hid[:, kc, j * 128:(j + 1) * 128], rhs=w2all[:, kc, hf * 512:(hf + 1) * 512], start=(kc == 0), stop=(kc == 31)),
                         reads=[hid, w2all], writes=[pso])
                tm = tmr.next()
                P.op("dve", lambda e, tm=tm, hf=hf, cond=cond: e.tensor_tensor(out=tm[:], in0=pso[:], in1=g.gbc[:, 1, cond, hf * 512:(hf + 1) * 512], op=ALU.mult), reads=[pso, g.gbc], writes=[tm])
                P.op("pool", lambda e, xt=xt, tm=tm, hf=hf: e.tensor_tensor(out=xt[:, hf * 512:(hf + 1) * 512], in0=xt[:, hf * 512:(hf + 1) * 512], in1=tm[:], op=ALU.add), reads=[xt, tm], writes=[xt])
            if last:
                P.dma("pool", d.out[tt - CT:tt - CT + 128, :], xt[:], reads=[xt])
            else:
                P.dma("pool", d.XCUR[tt:tt + 128, :], xt[:], reads=[xt])

    cx, steps = make_steps(0)
    for st in steps:
        st()
    for bi in range(len(blocks)):
        if bi + 1 < len(blocks):
            ncx, nsteps = make_steps(bi + 1)
        else:
            ncx, nsteps = None, None
        phase2(cx, nsteps)
        cx = ncx
    P.end_stage()


def stage_D(P, l, d, g):
    P.begin_stage("D%d" % l)
    wuq = P.sb("wuq", [128, 2, 768], BF16)
    wukv = P.sb("wukv", [128, 1024], BF16)
    P.dma("pool", wuq[:], d.mla_w_uq[l].rearrange("(k p) n -> p k n", p=128), writes=[wuq])
    P.dma("pool", wukv[:], d.mla_w_ukv[l], writes=[wukv])
    nrm = P.sb("nrm", [128, 3], F32)
    P.dma("sp", nrm[:, 0:2], d.mla_q_norm[l].rearrange("(k p) -> p k", p=128), writes=[nrm], allow_slow_non_contiguous=True)
    P.dma("sp", nrm[:, 2:3], d.mla_kv_norm[l].rearrange("(k p) -> p k", p=128), writes=[nrm], allow_slow_non_contiguous=True)
    gqk = P.sb("gqk", [128, 16, 96], F32)
    def _dst(ps):
        P.op("dve", lambda e: e.tensor_copy(out=gqk[:, 0:8, :], in_=ps[:, 0:96].unsqueeze(1).to_broadcast([128, 8, 96])), reads=[ps], writes=[gqk])
        P.op("dve", lambda e: e.tensor_copy(out=gqk[:, 8:16, :], in_=ps[:, 96:192].unsqueeze(1).to_broadcast([128, 8, 96])), reads=[ps], writes=[gqk])
    bcast_rows(P, _dst, [d.mla_qk_gain_q[l:l + 1, :], d.mla_qk_gain_k[l:l + 1, :]], [96, 96])
    invn = P.sb("invn", [128, 2], F32)
    P.op("dve", lambda e: e.memset(invn[:, 0:1], 1.0 / 256.0), writes=[invn])
    P.op("dve", lambda e: e.memset(invn[:, 1:2], 1.0 / 128.0), writes=[invn])
    epst = P.sb("epst", [128, 1], F32)
    P.op("dve", lambda e: e.memset(epst[:], EPS), writes=[epst])
    tmr = Ring([P.sb("tm%d" % i, [128, 416], F32) for i in range(2)])
    junk = P.sb("junk", [128, 256], F32)
    ssr = Ring([P.sb("ss%d" % i, [128, 2], F32) for i in range(2)])
    dsr = Ring([P.sb("ds%d" % i, [128, 384], BF16) for i in range(2)])
    dTr = Ring([P.sb("dT%d" % i, [128, 3, 128], BF16) for i in range(2)])
    qkr = Ring([P.sb("qk%d" % i, [128, 16, 96], F32) for i in range(2)])
    sqr = Ring([P.sb("sq%d" % i, [128, 16, 96], F32) for i in range(1)])
    sshr = Ring([P.sb("ssh%d" % i, [128, 16], F32) for i in range(2)])
    qkbr = Ring([P.sb("qkb%d" % i, [128, 16, 96], BF16) for i in range(2)])
    vxr = Ring([P.sb("vx%d" % i, [128, 8, 65], BF16) for i in range(2)])
    for b in vxr.bufs:
        P.op("dve", lambda e, b=b: e.memset(b[:], 1.0), writes=[b])
    csr = Ring([P.sb("cs%d" % i, [128, 2, 16], F32) for i in range(2)])
    rtr = Ring([P.sb("rt%d" % i, [128, 4, 16, 16], F32) for i in range(2)])
    qTr = Ring([P.sb("qT%d" % i, [96, 8, 512], BF16) for i in range(2)])
    kTr = Ring([P.sb("kT%d" % i, [96, 8, 512], BF16) for i in range(2)])
    pst = P.ps("pst", [128, 3, 128], BF16)
    psq = [P.ps("psq%d" % i, [128, 384]) for i in range(2)]
    pskv = [P.ps("pskv%d" % i, [128, 512]) for i in range(2)]
    psT = [P.ps("psT%d" % i, [96, 8, 128], BF16) for i in range(2)]
    import os
    nblk = int(os.environ.get("MK_D_NBLK", "99"))
    skip = os.environ.get("MK_D_SKIP", "").split(",")
    tiles = []
    for (t0, n) in BLOCKS[:nblk]:
        for j in range(n // 128):
            tiles.append((t0, n, j))

    def phase1(tile):
        t0, n, j = tile
        tt = t0 + j * 128
        tm = tmr.next()
        P.dma("sp", tm[:], d.TM[tt:tt + 128, :], writes=[tm])
        ss = ssr.next()
        P.op("pool", lambda e, ss=ss: e.memset(ss[:], 0.0), writes=[ss])
        P.op("act", lambda e, tm=tm, ss=ss: e.activation(out=junk[:, 0:256], in_=tm[:, 0:256], func=AF.Square, accum_out=ss[:, 0:1]), reads=[tm, ss], writes=[junk, ss])
        P.op("act", lambda e, tm=tm, ss=ss: e.activation(out=junk[:, 0:128], in_=tm[:, 256:384], func=AF.Square, accum_out=ss[:, 1:2]), reads=[tm, ss], writes=[junk, ss])
        P.op("dve", lambda e, ss=ss: e.tensor_tensor(out=ss[:], in0=ss[:], in1=invn[:], op=ALU.mult), reads=[ss, invn], writes=[ss])
        P.op("act", lambda e, ss=ss: e.activation(out=ss[:], in_=ss[:], func=AF.Ln, bias=epst[:, 0:1]), reads=[ss, epst], writes=[ss])
        P.op("act", lambda e, ss=ss: e.activation(out=ss[:], in_=ss[:], func=AF.Exp, scale=-0.5), reads=[ss], writes=[ss])
        ds = dsr.next()
        P.op("dve", lambda e, ds=ds, tm=tm, ss=ss: e.tensor_scalar(out=ds[:, 0:256], in0=tm[:, 0:256], scalar1=ss[:, 0:1], scalar2=None, op0=ALU.mult), reads=[tm, ss], writes=[ds])
        P.op("dve", lambda e, ds=ds, tm=tm, ss=ss: e.tensor_scalar(out=ds[:, 256:384], in0=tm[:, 256:384], scalar1=ss[:, 1:2], scalar2=None, op0=ALU.mult), reads=[tm, ss], writes=[ds])
        for k in range(3):
            P.op("pe", lambda e, k=k, ds=ds: e.transpose(pst[:, k, :], ds[:, k * 128:(k + 1) * 128], g.identb[:]), reads=[ds, g.identb], writes=[pst])
        dT = dTr.next()
        for k in range(3):
            P.op("act", lambda e, k=k, dT=dT: e.activation(out=dT[:, k, :], in_=pst[:, k, :], func=AF.Copy, scale=nrm[:, k:k + 1]), reads=[pst, nrm], writes=[dT])
        return tm, dT

    blkbuf = {}

    def phase2(tile, tm, dT):
        t0, n, j = tile
        tt = t0 + j * 128
        is_ctx = t0 < CT
        if j == 0:
            blkbuf["qT"] = qTr.next()
            blkbuf["kT"] = kTr.next()
        qT, kT = blkbuf["qT"], blkbuf["kT"]
        for hf in range(2):
            for k in range(2):
                P.op("pe", lambda e, hf=hf, k=k, dT=dT: e.matmul(psq[hf][:], lhsT=dT[:, k, :], rhs=wuq[:, k, hf * 384:(hf + 1) * 384], start=(k == 0), stop=(k == 1)),
                     reads=[dT, wuq], writes=[psq[hf]])
            P.op("pe", lambda e, hf=hf, dT=dT: e.matmul(pskv[hf][:], lhsT=dT[:, 2, :], rhs=wukv[:, hf * 512:(hf + 1) * 512], start=True, stop=True),
                 reads=[dT, wukv], writes=[pskv[hf]])
        qk = qkr.next()
        vx = vxr.next()
        for hf in range(2):
            P.op("act", lambda e, hf=hf, qk=qk: e.copy(out=qk[:, 4 * hf:4 * hf + 4, :], in_=psq[hf][:].rearrange("p (h c) -> p h c", c=96)), reads=[psq[hf]], writes=[qk])
            kvv = pskv[hf][:].rearrange("p (h c) -> p h c", c=128)
            P.op("dve", lambda e, hf=hf, qk=qk, kvv=kvv: e.tensor_copy(out=qk[:, 8 + 4 * hf:8 + 4 * hf + 4, 0:64], in_=kvv[:, :, 0:64]), reads=[pskv[hf]], writes=[qk])
            P.op("act", lambda e, hf=hf, vx=vx, kvv=kvv: e.copy(out=vx[:, 4 * hf:4 * hf + 4, 0:64], in_=kvv[:, :, 64:128]), reads=[pskv[hf]], writes=[vx])
        P.op("dve", lambda e, qk=qk, tm=tm: e.tensor_copy(out=qk[:, 8:16, 64:96], in_=tm[:, 384:416].unsqueeze(1).to_broadcast([128, 8, 32])), reads=[tm], writes=[qk])
        P.dma("pool", d.VX[tt:tt + 128, :, :], vx[:], reads=[vx])
        sq = sqr.next()
        ssh = sshr.next()
        P.op("dve", lambda e, sq=sq, qk=qk: e.tensor_tensor(out=sq[:], in0=qk[:], in1=qk[:], op=ALU.mult), reads=[qk], writes=[sq])
        P.op("dve", lambda e, sq=sq, ssh=ssh: e.reduce_sum(out=ssh[:], in_=sq[:], axis=AX.X), reads=[sq], writes=[ssh])
        P.op("dve", lambda e, ssh=ssh: e.tensor_scalar(out=ssh[:], in0=ssh[:], scalar1=1.0 / 96.0, scalar2=EPS, op0=ALU.mult, op1=ALU.add), reads=[ssh], writes=[ssh])
        P.op("act", lambda e, ssh=ssh: e.activation(out=ssh[:], in_=ssh[:], func=AF.Ln), reads=[ssh], writes=[ssh])
        P.op("act", lambda e, ssh=ssh: e.activation(out=ssh[:], in_=ssh[:], func=AF.Exp, scale=-0.5), reads=[ssh], writes=[ssh])
        P.op("dve", lambda e, qk=qk, ssh=ssh: e.tensor_tensor(out=qk[:], in0=qk[:], in1=ssh[:].unsqueeze(2).to_broadcast([128, 16, 96]), op=ALU.mult), reads=[qk, ssh], writes=[qk])
        qkb = qkbr.next()
        if is_ctx or "rope" in skip:
            P.op("dve", lambda e, qk=qk, qkb=qkb: e.tensor_tensor(out=qkb[:], in0=qk[:], in1=gqk[:], op=ALU.mult), reads=[qk, gqk], writes=[qkb])
        else:
            P.op("dve", lambda e, qk=qk: e.tensor_tensor(out=qk[:], in0=qk[:], in1=gqk[:], op=ALU.mult), reads=[qk, gqk], writes=[qk])
            cs = csr.next()
            P.dma("sp", cs[:, 0, :], d.k_cos[tt - CT:tt - CT + 128, :], writes=[cs])
            P.dma("sp", cs[:, 1, :], d.k_sin[tt - CT:tt - CT + 128, :], writes=[cs])
            rt = rtr.next()
            cosb = cs[:, 0, :].unsqueeze(1).to_broadcast([128, 16, 16])
            sinb = cs[:, 1, :].unsqueeze(1).to_broadcast([128, 16, 16])
            r1 = qk[:, :, 64:80]
            r2 = qk[:, :, 80:96]
            P.op("dve", lambda e, rt=rt, r1=r1, cosb=cosb: e.tensor_tensor(out=rt[:, 0, :, :], in0=r1, in1=cosb, op=ALU.mult), reads=[qk, cs], writes=[rt])
            P.op("dve", lambda e, rt=rt, r2=r2, sinb=sinb: e.tensor_tensor(out=rt[:, 1, :, :], in0=r2, in1=sinb, op=ALU.mult), reads=[qk, cs], writes=[rt])
            P.op("dve", lambda e, rt=rt, r2=r2, cosb=cosb: e.tensor_tensor(out=rt[:, 2, :, :], in0=r2, in1=cosb, op=ALU.mult), reads=[qk, cs], writes=[rt])
            P.op("dve", lambda e, rt=rt, r1=r1, sinb=sinb: e.tensor_tensor(out=rt[:, 3, :, :], in0=r1, in1=sinb, op=ALU.mult), reads=[qk, cs], writes=[rt])
            P.op("act", lambda e, qk=qk, qkb=qkb: e.copy(out=qkb[:, :, 0:64], in_=qk[:, :, 0:64]), reads=[qk], writes=[qkb])
            P.op("dve", lambda e, rt=rt, qkb=qkb: e.tensor_tensor(out=qkb[:, :, 64:80], in0=rt[:, 0, :, :], in1=rt[:, 1, :, :], op=ALU.subtract), reads=[rt], writes=[qkb])
            P.op("dve", lambda e, rt=rt, qkb=qkb: e.tensor_tensor(out=qkb[:, :, 80:96], in0=rt[:, 2, :, :], in1=rt[:, 3, :, :], op=ALU.add), reads=[rt], writes=[qkb])
        for i in range(16):
            P.op("pe", lambda e, i=i, qkb=qkb: e.transpose(psT[i // 8][:, i % 8, :], qkb[:, i, :], g.identb[:]), reads=[qkb, g.identb], writes=[psT[i // 8]])
        P.op("act", lambda e, qT=qT, j=j: e.copy(out=qT[:, :, j * 128:(j + 1) * 128], in_=psT[0][:]), reads=[psT[0]], writes=[qT])
        P.op("dve", lambda e, kT=kT, j=j: e.tensor_copy(out=kT[:, :, j * 128:(j + 1) * 128], in_=psT[1][:]), reads=[psT[1]], writes=[kT])
        if j == n // 128 - 1:
            if "stores" not in skip:
                P.dma("sp", d.QT[:, :, t0:t0 + n].rearrange("h c t -> c h t"), qT[:, :, 0:n], reads=[qT])
                P.dma("pool", d.KT[:, :, t0:t0 + n].rearrange("h c t -> c h t"), kT[:, :, 0:n], reads=[kT])


    cur = phase1(tiles[0])
    for ti_ in range(len(tiles)):
        nxt = phase1(tiles[ti_ + 1]) if ti_ + 1 < len(tiles) else None
        phase2(tiles[ti_], *cur)
        cur = nxt
    P.end_stage()


def body_E(P, l, d, g, last, merged=False):
    scale = 96.0 ** -0.5
    onesf = P.sb("onesf", [128, 64], F32)
    P.op("dve", lambda e: e.memset(onesf[:], 1.0), writes=[onesf])
    kTr = Ring([P.sb("kT%d" % i, [96, NT], BF16) for i in range(1 if merged else 2)])
    qbr = Ring([P.sb("qb%d" % i, [96, 512], BF16) for i in range(3)])
    vxr = Ring([P.sb("vx%d" % i, [128, 66, 65], BF16) for i in range(1 if merged else 2)])
    ptr = Ring([P.sb("pt%d" % i, [128, 512], BF16) for i in range(4)])
    osr = Ring([P.sb("os%d" % i, [65, 512], F32) for i in range(2)])
    rcr = Ring([P.sb("rc%d" % i, [64, 512], F32) for i in range(2)])
    obr = Ring([P.sb("ob%d" % i, [64, 512], BF16) for i in range(2)])
    pss = Ring([P.ps("pss%d" % i, [128, 512]) for i in range(3 if merged else 4)])
    pso = Ring([P.ps("pso%d" % i, [128, 512]) for i in range(1 if merged else 2)])
    psb_own = None if merged else P.ps("psb", [64, 512])
    yield
    for h in range(8):
        kT = kTr.next()
        vx = vxr.next()
        P.dma("sp", kT[:], d.KT[h], writes=[kT])
        P.dma("pool", vx[:], d.VX[:, h, :].rearrange("(kt p) c -> p kt c", p=128), writes=[vx])
        for (t0, n) in BLOCKS:
            is_ctx = t0 < CT
            if is_ctx and last:
                continue
            kts = [0, 1] if is_ctx else list(range(66))
            po = pso.next()
            qb = qbr.next()
            P.dma("sp", qb[:, 0:n], d.QT[h, :, t0:t0 + n], writes=[qb])
            nk = len(kts)
            LOOK = 2
            pss_q = []

            def issue_s(ki):
                kt = kts[ki]
                ps = pss.next()
                P.op("pe", lambda e: e.matmul(ps[:, 0:n], lhsT=kT[:, kt * 128:(kt + 1) * 128], rhs=qb[:, 0:n], start=True, stop=True),
                     reads=[kT, qb], writes=[ps])
                pss_q.append(ps)
            for ki in range(min(LOOK, nk)):
                issue_s(ki)
            for ki, kt in enumerate(kts):
                ps = pss_q[ki]
                pt = ptr.next()
                P.op("act", lambda e: e.activation(out=pt[:, 0:n], in_=ps[:, 0:n], func=AF.Exp, scale=scale), reads=[ps], writes=[pt])
                if ki + LOOK < nk:
                    issue_s(ki + LOOK)
                P.op("pe", lambda e: e.matmul(po[0:65, 0:n], lhsT=vx[:, kt, :], rhs=pt[:, 0:n], start=(ki == 0), stop=(ki == nk - 1)),
                     reads=[vx, pt], writes=[po])
            psb = psb_own if psb_own is not None else pss.next()
            osb = osr.next()
            P.op("dve", lambda e, osb=osb, po=po, n=n: e.tensor_copy(out=osb[:, 0:n], in_=po[0:65, 0:n]), reads=[po], writes=[osb])
            P.op("pe", lambda e, osb=osb, n=n: e.matmul(psb[0:64, 0:n], lhsT=onesf[64:65, 0:64], rhs=osb[64:65, 0:n], start=True, stop=True), reads=[onesf, osb], writes=[psb])
            rc = rcr.next()
            P.op("dve", lambda e, rc=rc, n=n: e.reciprocal(out=rc[:, 0:n], in_=psb[0:64, 0:n]), reads=[psb], writes=[rc])
            ob = obr.next()
            P.op("pool", lambda e, ob=ob, osb=osb, rc=rc, n=n: e.tensor_tensor(out=ob[:, 0:n], in0=osb[0:64, 0:n], in1=rc[:, 0:n], op=ALU.mult), reads=[osb, rc], writes=[ob])
            P.dma("pool", d.OMLA[h * 64:(h + 1) * 64, t0:t0 + n], ob[:, 0:n], reads=[ob])
            yield


def stage_E(P, l, d, g, last):
    P.begin_stage("E%d" % l)
    for _ in body_E(P, l, d, g, last):
        pass
    P.end_stage()


def body_B(P, l, d, g, merged=False):
    NB = 256
    NCH = NB // 32
    tri = P.sb("tri", [32, 2, 32], F32)
    P.dma("sp", tri[:], d.k_tri[:, :, :], writes=[tri])
    lb = P.sb("lb", [128, 4, 2], F32)
    oml = P.sb("oml", [128, 4, 2], F32)
    if l > 0:
        lb0 = P.sb("lb0", [128, 4, 2], F32)
        for r_ in range(2):
            P.dma("sp", lb[:, :, r_], d.hg_lb[l, r_].rearrange("(t p) -> p t", p=128), writes=[lb], allow_slow_non_contiguous=True)
            P.dma("sp", lb0[:, :, r_], d.hg_lb[0, r_].rearrange("(t p) -> p t", p=128), writes=[lb0], allow_slow_non_contiguous=True)
        P.op("dve", lambda e: e.tensor_tensor(out=lb[:], in0=lb[:], in1=lb0[:], op=ALU.subtract), reads=[lb, lb0], writes=[lb])
        P.op("act", lambda e: e.activation(out=lb[:], in_=lb[:], func=AF.Sigmoid), reads=[lb], writes=[lb])
        P.op("dve", lambda e: e.tensor_scalar(out=oml[:], in0=lb[:], scalar1=-1.0, scalar2=1.0, op0=ALU.mult, op1=ALU.add), reads=[lb], writes=[oml])
    masks = []
    for dr in range(2):
        m = P.sb("mask%d" % dr, [128, 4 * NB], F32)
        P.op("pool", lambda e: e.memset(m[:], 1.0), writes=[m])
        mv = m[:].rearrange("p (c j) -> p c j", j=32)
        pos = 0 if dr == 0 else 31
        P.op("pool", lambda e: e.memset(mv[:, :, pos:pos + 1], 0.0), writes=[m])
        masks.append(m)

    class St:
        pass
    sts = []
    if merged is True:
        bankKA = P.ps_bank("psKA")
        sh_psK = bankKA.alias(bankKA.t[0:32, 0:256].bitcast(BF16), "psK")
        sh_psA = bankKA.alias(bankKA.t[0:32, 256:512], "psA")
        sh_psO = P.ps("psO", [32, 512])
        sh_psS = P.ps("psS", [128, 4, 128])
    cp = "dve"
    for dr in range(2):
        s = St()
        s.zt = P.sb("zt%d" % dr, [128, 4, NB], F32)
        s.kk = P.sb("kk%d" % dr, [128, 4, NB], F32)
        s.b = P.sb("b%d" % dr, [128, 4, NB], F32)
        s.eb = P.sb("eb%d" % dr, [128, 4, NB], F32)
        s.ebt_r = Ring([P.sb("ebt%d_%d" % (dr, i), [128, 4, NCH], F32) for i in range(2)])
        s.qt_r = Ring([P.sb("qt%d_%d" % (dr, i), [128, 4, NB], BF16) for i in range(2)])
        s.Qt_r = Ring([P.sb("Qt%d_%d" % (dr, i), [128, 4, NB], BF16) for i in range(2)])
        s.Kt_r = Ring([P.sb("Kt%d_%d" % (dr, i), [128, 4, NB], BF16) for i in range(2)])
        s.Kh_r = Ring([P.sb("Kh%d_%d" % (dr, i), [128, 4, NB], BF16) for i in range(2)])
        s.V_r = Ring([P.sb("V%d_%d" % (dr, i), [32, NCH, 512], BF16) for i in range(2)])
        s.ost = Ring([P.sb("ost%d_%d" % (dr, i), [32, NCH, 512], F32) for i in range(1 if merged else 2)])
        s.Sp = [P.sb("S%d_%d" % (dr, i), [128, 4, 64], F32) for i in range(2)]
        s.Sbp = [P.sb("Sb%d_%d" % (dr, i), [128, 4, 64], BF16) for i in range(2)]
        s.step = [0]
        s.tmpS = P.sb("tmpS%d" % dr, [128, 4, 64], F32)
        s.khat = Ring([P.sb("khat%d_%d" % (dr, i), [32, 512], BF16) for i in range(2)])
        s.AT = Ring([P.sb("AT%d_%d" % (dr, i), [32, 8, 32], BF16) for i in range(2)])
        if merged is True:
            s.psK, s.psA, s.psO, s.psS = sh_psK, sh_psA, sh_psO, sh_psS
        elif merged == "ka":
            bk = P.ps_bank("psKA%d" % dr)
            s.psK = bk.alias(bk.t[0:32, 0:256].bitcast(BF16), "psK")
            s.psA = bk.alias(bk.t[0:32, 256:512], "psA")
            s.psO = P.ps("psO%d" % dr, [32, 512])
            s.psS = P.ps("psS%d" % dr, [128, 4, 128])
        else:
            s.psK = P.ps("psK%d" % dr, [32, 512], BF16)
            s.psA = P.ps("psA%d" % dr, [32, 256])
            s.psO = P.ps("psO%d" % dr, [32, 512])
            s.psS = P.ps("psS%d" % dr, [128, 4, 128])
        for i_ in range(2):
            P.op("dve", lambda e: e.memset(s.Sp[i_][:], 0.0), writes=[s.Sp[i_]])
            P.op("dve", lambda e: e.memset(s.Sbp[i_][:], 0.0), writes=[s.Sbp[i_]])
        sts.append(s)

    blocks_f = [(i * NB, NB) for i in range(NT // NB)]
    blocks_b = [(0, NB)] + [(i * NB, NB) for i in reversed(range(1, NT // NB))]

    def prep_steps(dr, t0, n):
        s = sts[dr]

        class Cx:
            pass
        cx = Cx()
        cx.ebt, cx.qt, cx.Qt, cx.Kt, cx.Kh, cx.V = s.ebt_r.next(), s.qt_r.next(), s.Qt_r.next(), s.Kt_r.next(), s.Kh_r.next(), s.V_r.next()
        cx.t0, cx.n = t0, n
        s_ = s
        s = St()
        s.__dict__.update(s_.__dict__)
        s.__dict__.update(cx.__dict__)
        zrow = dr * 512
        fl = lambda t: t[:].rearrange("p a n -> p (a n)")

        def st0():
            P.dma("sp", s.zt[:], d.FZ[zrow:zrow + 512, t0:t0 + n].rearrange("(a p) t -> p a t", p=128), writes=[s.zt])
            P.dma("sp", s.qt[:], d.FQ[:, t0:t0 + n].rearrange("(a p) t -> p a t", p=128), writes=[s.qt])
            P.dma("pool", s.V[:], d.TV[t0:t0 + n, 0:512].rearrange("(c p) f -> p c f", p=32), writes=[s.V])

        def st1():
            P.op("act", lambda e: e.activation(out=fl(s.zt), in_=fl(s.zt), func=AF.Exp, scale=-1.0), reads=[s.zt], writes=[s.zt])

        def st2():
            P.op("dve", lambda e: e.tensor_scalar(out=fl(s.zt), in0=fl(s.zt), scalar1=1.0, scalar2=None, op0=ALU.add), reads=[s.zt], writes=[s.zt])
            P.op("dve", lambda e: e.reciprocal(out=fl(s.zt), in_=fl(s.zt)), reads=[s.zt], writes=[s.zt])
            if l > 0:
                P.op("dve", lambda e: e.tensor_tensor(out=s.zt[:], in0=s.zt[:], in1=oml[:, :, dr].unsqueeze(2).to_broadcast([128, 4, n]), op=ALU.mult), reads=[s.zt, oml], writes=[s.zt])
                P.op("dve", lambda e: e.tensor_tensor(out=s.zt[:], in0=s.zt[:], in1=lb[:, :, dr].unsqueeze(2).to_broadcast([128, 4, n]), op=ALU.add), reads=[s.zt, lb], writes=[s.zt])
            P.op("dve", lambda e: e.tensor_scalar(out=fl(s.kk), in0=fl(s.zt), scalar1=-1.0, scalar2=1.0, op0=ALU.mult, op1=ALU.add), reads=[s.zt], writes=[s.kk])

        def st3():
            P.op("act", lambda e: e.activation(out=fl(s.zt), in_=fl(s.zt), func=AF.Ln), reads=[s.zt], writes=[s.zt])

        def st4():
            if dr == 0:
                P.op("dve", lambda e: e.tensor_tensor_scan(out=fl(s.b), data0=masks[0][:], data1=fl(s.zt), initial=0.0, op0=ALU.mult, op1=ALU.add), reads=[masks[0], s.zt], writes=[s.b])
            else:
                P.op("dve", lambda e: e.tensor_tensor_scan(out=fl(s.b)[:, ::-1], data0=masks[1][:][:, ::-1], data1=fl(s.zt)[:, ::-1], initial=0.0, op0=ALU.mult, op1=ALU.add),
                     reads=[masks[1], s.zt], writes=[s.b])

        def st5():
            P.op("act", lambda e: e.activation(out=fl(s.eb), in_=fl(s.b), func=AF.Exp), reads=[s.b], writes=[s.eb])
            P.op("act", lambda e: e.activation(out=fl(s.b), in_=fl(s.b), func=AF.Exp, scale=-1.0), reads=[s.b], writes=[s.b])

        def st6():
            P.op("dve", lambda e: e.tensor_tensor(out=fl(s.Qt), in0=fl(s.qt), in1=fl(s.eb), op=ALU.mult), reads=[s.qt, s.eb], writes=[s.Qt])
            P.op("dve", lambda e: e.tensor_tensor(out=fl(s.Kt), in0=fl(s.kk), in1=fl(s.b), op=ALU.mult), reads=[s.kk, s.b], writes=[s.Kt])
            lastpos = 31 if dr == 0 else 0
            eb4 = s.eb[:].rearrange("p a (c j) -> p a c j", j=32)
            P.op("dve", lambda e: e.tensor_copy(out=s.ebt[:], in_=eb4[:, :, :, lastpos]), reads=[s.eb], writes=[s.ebt])
            P.op("dve", lambda e: e.tensor_tensor(out=s.Kh[:].rearrange("p a (c j) -> p a c j", j=32), in0=s.Kt[:].rearrange("p a (c j) -> p a c j", j=32),
                                                  in1=s.ebt[:].unsqueeze(3).to_broadcast([128, 4, NCH, 32]), op=ALU.mult), reads=[s.Kt, s.ebt], writes=[s.Kh])
        return cx, [st0, st1, st2, st3, st4, st5, st6]

    def prep(dr, t0, n):
        cx, steps = prep_steps(dr, t0, n)
        for f_ in steps:
            f_()
        return cx

    def chunks(dr, cx, nxt):
        s_ = sts[dr]
        s = St()
        s.__dict__.update(s_.__dict__)
        s.__dict__.update(cx.__dict__)
        t0, n = cx.t0, cx.n
        ost = s_.ost.next()
        order = list(range(NCH) if dr == 0 else reversed(range(NCH)))
        res = [None]
        nsteps = []
        if nxt is not None:
            res[0], nsteps = prep_steps(dr, *nxt)
        for ci, c in enumerate(order):
            if ci < len(nsteps):
                nsteps[ci]()
            o = c * 32
            khat = s.khat.next()
            for a in range(4):
                P.op("pe", lambda e: e.transpose(s.psK[0:32, a * 128:(a + 1) * 128], s.Kh[:, a, o:o + 32], g.identb[:]), reads=[s.Kh, g.identb], writes=[s.psK])
            P.op("act", lambda e: e.copy(out=khat[:], in_=s.psK[:]), reads=[s.psK], writes=[khat])
            for h in (0, 2, 4, 6, 1, 3, 5, 7):
                a, pr = h // 2, (h % 2) * 64
                P.op("pe", lambda e: e.matmul(s.psA[0:32, h * 32:(h + 1) * 32], lhsT=s.Kt[pr:pr + 64, a, o:o + 32], rhs=s.Qt[pr:pr + 64, a, o:o + 32], start=True, stop=True),
                     reads=[s.Kt, s.Qt], writes=[s.psA], rg=(pr // 32, pr // 32 + 1))
            AT = s.AT.next()
            P.op("dve", lambda e: e.tensor_tensor(out=AT[:], in0=s.psA[:].rearrange("p (h t) -> p h t", t=32), in1=tri[:, dr, :].unsqueeze(1).to_broadcast([32, 8, 32]), op=ALU.mult),
                 reads=[s.psA, tri], writes=[AT])
            k_ = s_.step[0]
            s_.step[0] += 1
            Sprev, Snew = s.Sp[k_ % 2], s.Sp[(k_ + 1) % 2]
            Sbprev, Sbnew = s.Sbp[k_ % 2], s.Sbp[(k_ + 1) % 2]
            for a in range(4):
                P.op("pe", lambda e: e.matmul(s.psS[:, a, :], lhsT=khat[0:32, a * 128:(a + 1) * 128], rhs=s.V[0:32, c, a * 128:(a + 1) * 128], start=True, stop=True),
                     reads=[khat, s.V], writes=[s.psS], rg=(0,))
            for hf in range(2):
                ph = slice(hf * 64, (hf + 1) * 64)
                P.op("dve", lambda e: e.tensor_tensor(out=s.tmpS[ph, :, :], in0=Sprev[ph, :, :], in1=s.ebt[ph, :, c].unsqueeze(2).to_broadcast([64, 4, 64]), op=ALU.mult),
                     reads=[Sprev, s.ebt], writes=[s.tmpS])
                P.op("dve", lambda e: e.tensor_tensor(out=Snew[ph, :, :], in0=s.tmpS[ph, :, :], in1=s.psS[ph, :, hf * 64:(hf + 1) * 64], op=ALU.add),
                     reads=[s.tmpS, s.psS], writes=[Snew])
            P.op("act", lambda e: e.copy(out=Sbnew[:], in_=Snew[:]), reads=[Snew], writes=[Sbnew])
            for hi, h in enumerate((0, 2, 4, 6, 1, 3, 5, 7)):
                a, pr = h // 2, (h % 2) * 64
                P.op("pe", lambda e: e.matmul(s.psO[0:32, h * 64:(h + 1) * 64], lhsT=s.Qt[pr:pr + 64, a, o:o + 32], rhs=Sbprev[pr:pr + 64, a, :], start=(hi == 0), stop=False, skip_group_check=True),
                     reads=[s.Qt, Sbprev], writes=[s.psO], rg=(pr // 32, pr // 32 + 1))
            for h in range(8):
                P.op("pe", lambda e: e.matmul(s.psO[0:32, h * 64:(h + 1) * 64], lhsT=AT[0:32, h, :], rhs=s.V[0:32, c, h * 64:(h + 1) * 64], start=False, stop=True, skip_group_check=True),
                     reads=[AT, s.V], writes=[s.psO], rg=(0,))
            P.op("act", lambda e: e.copy(out=ost[:, c, :], in_=s.psO[:]), reads=[s.psO], writes=[ost])
            yield
        P.dma("pool", d.ODIR[dr, t0:t0 + n, :].rearrange("(c p) f -> p c f", p=32), ost[:], reads=[ost])
        return res[0]

    def dirgen(dr, blocks):
        cx = prep(dr, *blocks[0])
        for i in range(len(blocks)):
            nxt = blocks[i + 1] if i + 1 < len(blocks) else None
            cx = yield from chunks(dr, cx, nxt)

    yield
    gf, gb = dirgen(0, blocks_f), dirgen(1, blocks_b)
    alive = [True, True]
    while any(alive):
        for gi, gen in enumerate((gf, gb)):
            if alive[gi]:
                try:
                    next(gen)
                except StopIteration:
                    alive[gi] = False
        yield


def stage_B(P, l, d, g):
    P.begin_stage("B%d" % l)
    for _ in body_B(P, l, d, g):
        pass
    P.end_stage()


def stage_BC(P, l, d, g):
    P.begin_stage("BC%d" % l)
    gb = body_B(P, l, d, g, "ka")
    gc = body_C(P, l, d, g, True)
    next(gb)
    next(gc)
    alive = [True, True]
    while any(alive):
        for gi, gen in enumerate((gb, gc)):
            if alive[gi]:
                try:
                    next(gen)
                except StopIteration:
                    alive[gi] = False
    P.end_stage()


def stage_BCE(P, l, d, g, last):
    P.begin_stage("BCE%d" % l)
    gens = [body_E(P, l, d, g, last, True), body_B(P, l, d, g, True), body_C(P, l, d, g, True)]
    ne = 8 * (16 if last else 17)
    totals = [ne + 1, 66 * 10 + 1, 4 * 2 * 33 + 1]
    done = [0, 0, 0]
    alive = [True, True, True]
    for gi, gen in enumerate(gens):
        next(gen)
        done[gi] += 1
    while any(alive):
        cand = [gi for gi in range(3) if alive[gi]]
        gi = min(cand, key=lambda i: done[i] / totals[i])
        try:
            next(gens[gi])
            done[gi] += 1
        except StopIteration:
            alive[gi] = False
    P.end_stage()


def stage_B2(P, l, d, g, last):
    P.begin_stage("B2_%d" % l)
    gn = P.sb("gn", [128, 64], F32)
    bcast_rows(P, lambda ps: P.op("dve", lambda e: e.tensor_copy(out=gn[:], in_=ps[:]), reads=[ps], writes=[gn]), [d.hg_norm[l:l + 1, :]], [64])
    o0r = Ring([P.sb("o0_%d" % i, [128, 512], F32) for i in range(2)])
    o1r = Ring([P.sb("o1_%d" % i, [128, 512], F32) for i in range(2)])
    gtr = Ring([P.sb("gt%d" % i, [128, 512], BF16) for i in range(2)])
    sgr = Ring([P.sb("sg%d" % i, [128, 512], F32) for i in range(2)])
    sqr = Ring([P.sb("sq%d" % i, [128, 512], F32) for i in range(2)])
    ssr = Ring([P.sb("ss%d" % i, [128, 8], F32) for i in range(2)])
    obr = Ring([P.sb("ob%d" % i, [128, 512], BF16) for i in range(2)])
    oTr = Ring([P.sb("oT%d" % i, [128, 4, 512], BF16) for i in range(2)])
    pst = Ring([P.ps("pst%d" % i, [128, 4, 128], BF16) for i in range(2)])
    for (t0, n) in BLOCKS:
        if last and t0 < CT:
            continue
        oT = oTr.next()
        for j in range(n // 128):
            tt = t0 + j * 128
            o0 = o0r.next(); o1 = o1r.next(); gt = gtr.next()
            P.dma("sp", o0[:], d.ODIR[0, tt:tt + 128, :], writes=[o0])
            P.dma("sp", o1[:], d.ODIR[1, tt:tt + 128, :], writes=[o1])
            P.dma("sp", gt[:], d.TV[tt:tt + 128, 512:1024], writes=[gt])
            sg = sgr.next(); sq = sqr.next(); ss = ssr.next(); ob = obr.next()
            P.op("act", lambda e: e.activation(out=sg[:], in_=gt[:], func=AF.Sigmoid), reads=[gt], writes=[sg])
            P.op("dve", lambda e: e.tensor_tensor(out=o0[:], in0=o0[:], in1=o1[:], op=ALU.add), reads=[o0, o1], writes=[o0])
            P.op("dve", lambda e: e.tensor_tensor(out=sq[:], in0=o0[:], in1=o0[:], op=ALU.mult), reads=[o0], writes=[sq])
            P.op("dve", lambda e: e.reduce_sum(out=ss[:], in_=sq[:].rearrange("p (h v) -> p h v", v=64), axis=AX.X), reads=[sq], writes=[ss])
            P.op("dve", lambda e: e.tensor_scalar(out=ss[:], in0=ss[:], scalar1=1.0 / 64.0, scalar2=EPS, op0=ALU.mult, op1=ALU.add), reads=[ss], writes=[ss])
            P.op("act", lambda e: e.activation(out=ss[:], in_=ss[:], func=AF.Ln), reads=[ss], writes=[ss])
            P.op("act", lambda e: e.activation(out=ss[:], in_=ss[:], func=AF.Exp, scale=-0.5), reads=[ss], writes=[ss])
            v3 = lambda t: t[:].rearrange("p (h v) -> p h v", v=64)
            P.op("dve", lambda e: e.tensor_tensor(out=v3(o0), in0=v3(o0), in1=ss[:].unsqueeze(2).to_broadcast([128, 8, 64]), op=ALU.mult), reads=[o0, ss], writes=[o0])
            P.op("dve", lambda e: e.tensor_tensor(out=v3(o0), in0=v3(o0), in1=gn[:].unsqueeze(1).to_broadcast([128, 8, 64]), op=ALU.mult), reads=[o0, gn], writes=[o0])
            P.op("dve", lambda e: e.tensor_tensor(out=ob[:], in0=o0[:], in1=sg[:], op=ALU.mult), reads=[o0, sg], writes=[ob])
            ps = pst.next()
            for a in range(4):
                P.op("pe", lambda e: e.transpose(ps[:, a, :], ob[:, a * 128:(a + 1) * 128], g.identb[:]), reads=[ob, g.identb], writes=[ps])
            P.op("act", lambda e: e.copy(out=oT[:, :, j * 128:(j + 1) * 128], in_=ps[:]), reads=[ps], writes=[oT])
        P.dma("pool", d.OHG[:, t0:t0 + n].rearrange("(a p) t -> p a t", p=128), oT[:, :, 0:n], reads=[oT])
    P.end_stage()


from concourse.bass_utils import run_bass_kernel_spmd


def build_program():
    nc = bass.Bass("TRN2", target_bir_lowering=False)
    d = declare(nc)
    P = Prog(nc)
    g = alloc_globals(P)
    stage_init(P, d, g)
    for l in range(2):
        last = (l == 1)
        stage_P(P, l, d, g)
        stage_A(P, l, d, g)
        stage_B(P, l, d, g)
        stage_B2(P, l, d, g, last)
        stage_C(P, l, d, g)
        stage_D(P, l, d, g)
        stage_E(P, l, d, g, last)
        stage_F(P, l, d, g, last)
        stage_G0(P, l, d, g)
        stage_G(P, l, d, g, last)
    P.finish()
    return nc


def kernel(**inputs):
    nc = build_program()
    consts = host_constants()
    shared = {k: np.ascontiguousarray(np.asarray(v, dtype=np.float32)) for k, v in inputs.items() if k not in ("x", "c", "ctx")}
    shared.update(consts)
    zeros = {k: np.zeros_like(v) for k, v in shared.items()}
    zx = np.zeros(tuple(inputs["x"].shape[1:]), np.float32)
    zc = np.zeros(tuple(inputs["c"].shape[1:]), np.float32)
    zctx = np.zeros(tuple(inputs["ctx"].shape[1:]), np.float32)
    active = {0: 0, 1: 1, 4: 2, 5: 3}
    in_maps = []
    for core in range(8):
        if core in active:
            b = active[core]
            m = dict(shared)
            m["x"] = np.ascontiguousarray(np.asarray(inputs["x"][b], dtype=np.float32))
            m["c"] = np.ascontiguousarray(np.asarray(inputs["c"][b], dtype=np.float32))
            m["ctx"] = np.ascontiguousarray(np.asarray(inputs["ctx"][b], dtype=np.float32))
        else:
            m = dict(zeros)
            m["x"], m["c"], m["ctx"] = zx, zc, zctx
        in_maps.append(m)
    res = run_bass_kernel_spmd(nc, in_maps, core_ids=list(range(8)))
    cores = [0, 1, 4, 5]
    out = np.stack([np.asarray(res.results[c]["out"], dtype=np.float32) for c in cores], axis=0)
    return out
```
